# Optimizing a Trainium2 kernel written in Bass

```python
import math
import jax, jax.numpy as jnp
from jax import lax
import numpy as np

D_MODEL = 2048
BATCH = 8
SEQ = 2048
DEPTH = 4

CTX_LEN = 256
GRID_W = 64
EPS = 1e-6
N_ADA = 6

GLA_HEADS = 4
GLA_DK = D_MODEL // 2
GLA_DV = D_MODEL
GLA_DK_HEAD = GLA_DK // GLA_HEADS
GLA_DV_HEAD = GLA_DV // GLA_HEADS
GLA_GATE_RANK = 16
GLA_GATE_TEMP = 16.0
GLA_CHUNK = 64
GLA_IN = 2 * GLA_DK + 2 * GLA_DV

HY_BANDS = 16
HY_EMB_DIM = 1 + 2 * HY_BANDS
HY_FILTER_HIDDEN = 64
HY_SIN_FREQ = 1.0
HY_FAST_DECAY_PCT = 0.3
HY_SLOW_DECAY_PCT = 1.5
HY_DECAY_TARGET = 1e-2

N_EXPERTS = 16
EXPERT_FF = D_MODEL // 2
CAPACITY_FACTOR = 2

N_GLA = (DEPTH + 1) // 2
N_HYENA = DEPTH // 2

kernel_name = "hybrid_gla_hyena_ecmoe_dit"


def rms_norm(x, g):
    xf = x.astype(jnp.float32)
    y = xf * lax.rsqrt(jnp.mean(xf * xf, axis=-1, keepdims=True) + EPS)
    return (y * g.astype(jnp.float32)).astype(x.dtype)


def modulate(h, shift, scale):
    return h * (1 + scale) + shift


def gla_project(h, w_in, w_a1, w_a2, b_a):
    B, L, _ = h.shape
    proj = h @ w_in
    q, k, v, g_out = jnp.split(proj, [GLA_DK, 2 * GLA_DK, 2 * GLA_DK + GLA_DV], axis=-1)

    def heads(t, d):
        return t.reshape(B, L, GLA_HEADS, d).transpose(0, 2, 1, 3).astype(jnp.float32)

    low = jnp.einsum('bld,zdr->zblr', h, w_a1)
    logit = jnp.einsum('zblr,zrk->zblk', low, w_a2) + b_a[:, None, None, :]
    log_alpha = jax.nn.log_sigmoid(logit.astype(jnp.float32)) / GLA_GATE_TEMP
    q = heads(q, GLA_DK_HEAD) * (GLA_DK_HEAD ** -0.5)
    return (q, heads(k, GLA_DK_HEAD), heads(v, GLA_DV_HEAD),
            heads(log_alpha[0], GLA_DK_HEAD), heads(log_alpha[1], GLA_DK_HEAD), g_out)


def gla_scan(q, k, v, la, s0):
    B, H, L, _ = q.shape
    dv = v.shape[-1]
    n = L // GLA_CHUNK

    def to_chunks(t):
        return t.reshape(B, H, n, GLA_CHUNK, t.shape[-1]).transpose(2, 0, 1, 3, 4)

    mask = jnp.tril(jnp.ones((GLA_CHUNK, GLA_CHUNK), dtype=bool))[:, :, None]

    def step(S, inp):
        qc, kc, vc, gc = inp
        b = jnp.cumsum(gc, axis=-2)
        o_inter = jnp.einsum('bhcd,bhde->bhce', qc * jnp.exp(b), S)
        diff = b[:, :, :, None, :] - b[:, :, None, :, :]
        decay = jnp.exp(jnp.where(mask, diff, -jnp.inf))
        A = jnp.einsum('bhid,bhjd,bhijd->bhij', qc, kc, decay)
        o_intra = jnp.einsum('bhij,bhje->bhie', A, vc)
        b_last = b[:, :, -1:, :]
        S_new = jnp.exp(b_last[:, :, 0, :])[..., None] * S + jnp.einsum(
            'bhjd,bhje->bhde', kc * jnp.exp(b_last - b), vc)
        return S_new, o_inter + o_intra

    s_fin, o = lax.scan(step, s0, (to_chunks(q), to_chunks(k), to_chunks(v), to_chunks(la)))
    return s_fin, o.transpose(1, 2, 0, 3, 4).reshape(B, H, L, dv)


def gla_bidir(q, k, v, la_f, la_b, s0_f, s0_b):
    s_f, o_f = gla_scan(q, k, v, la_f, s0_f)
    fl = lambda t: jnp.flip(t, axis=2)
    s_b, o_b = gla_scan(fl(q), fl(k), fl(v), fl(la_b), s0_b)
    return o_f + fl(o_b), s_f, s_b


def gla_output(o, g_out, head_norm, w_out, dtype):
    B, H, L, _ = o.shape
    o = o * lax.rsqrt(jnp.mean(o * o, axis=-1, keepdims=True) + EPS) * head_norm.astype(jnp.float32)
    o = o.transpose(0, 2, 1, 3).reshape(B, L, H * GLA_DV_HEAD).astype(dtype)
    return (o * jax.nn.silu(g_out)) @ w_out


def gla_mixer(h_lat, h_ctx, w_in, w_a1, w_a2, b_a, head_norm, w_out, need_ctx):
    B = h_lat.shape[0]
    zeros = jnp.zeros((B, GLA_HEADS, GLA_DK_HEAD, GLA_DV_HEAD), jnp.float32)
    qc, kc, vc, lfc, lbc, gc = gla_project(h_ctx, w_in, w_a1, w_a2, b_a)
    o_ctx, s_f, s_b = gla_bidir(qc, kc, vc, lfc, lbc, zeros, zeros)
    ql, kl, vl, lfl, lbl, gl = gla_project(h_lat, w_in, w_a1, w_a2, b_a)
    o_lat, _, _ = gla_bidir(ql, kl, vl, lfl, lbl, s_f, s_b)
    out_lat = gla_output(o_lat, gl, head_norm, w_out, h_lat.dtype)
    out_ctx = gla_output(o_ctx, gc, head_norm, w_out, h_ctx.dtype) if need_ctx else None
    return out_lat, out_ctx


def conv3_centred(u, w, b):
    up = jnp.pad(u, [(0, 0)] * (u.ndim - 2) + [(1, 1), (0, 0)])
    return up[..., :-2, :] * w[0] + up[..., 1:-1, :] * w[1] + up[..., 2:, :] * w[2] + b


def hyena_filter(L, f_w1, f_b1, f_w2, f_b2, f_w3, f_b3, f_w4):
    f32 = jnp.float32
    t = jnp.linspace(0.0, 1.0, L, dtype=f32)[:, None]
    w = 2 * math.pi * jnp.arange(L, dtype=f32)[:, None] / L
    f = jnp.linspace(1e-4, HY_BANDS - 1, HY_BANDS, dtype=f32)[None, :]
    z = jnp.concatenate([t, jnp.cos(f * w), -jnp.sin(f * w)], axis=-1)
    hdn = jnp.sin(HY_SIN_FREQ * (z @ f_w1.astype(f32) + f_b1.astype(f32)))
    hdn = jnp.sin(HY_SIN_FREQ * (hdn @ f_w2.astype(f32) + f_b2.astype(f32)))
    hdn = jnp.sin(HY_SIN_FREQ * (hdn @ f_w3.astype(f32) + f_b3.astype(f32)))
    filt = hdn @ f_w4.astype(f32)
    D = filt.shape[-1] // 2
    max_decay = math.log(HY_DECAY_TARGET) / HY_FAST_DECAY_PCT
    min_decay = math.log(HY_DECAY_TARGET) / HY_SLOW_DECAY_PCT
    deltas = jnp.abs(jnp.linspace(min_decay, max_decay, D, dtype=f32))
    decay = jnp.exp(-t * deltas[None, :])
    h_fwd = filt[:, :D] * decay
    h_bwd = filt[:, D:] * decay
    k_full = jnp.concatenate([h_fwd, jnp.zeros((1, D), f32), h_bwd[1:][::-1]], axis=0)
    return k_full / jnp.sum(jnp.abs(k_full), axis=0, keepdims=True)


def fft_long_conv(u, k_full, skip):
    L = u.shape[1]
    uf = u.astype(jnp.float32)
    U = jnp.fft.rfft(uf, n=2 * L, axis=1)
    K = jnp.fft.rfft(k_full, n=2 * L, axis=0)
    y = jnp.fft.irfft(U * K[None], n=2 * L, axis=1)[:, :L]
    return (y + uf * skip.astype(jnp.float32)).astype(u.dtype)


def hyena_mixer(h, w_in, b_in, conv_w, conv_b, f_w1, f_b1, f_w2, f_b2, f_w3, f_b3, f_w4, skip,
                w_out, b_out, grid):
    B, L, D = h.shape
    proj = h @ w_in + b_in
    if grid:
        rows = L // GRID_W
        proj = conv3_centred(proj.reshape(B, rows, GRID_W, 3 * D), conv_w, conv_b).reshape(B, L, 3 * D)
    else:
        proj = conv3_centred(proj, conv_w, conv_b)
    x0, x1, v = jnp.split(proj, 3, axis=-1)
    k_full = hyena_filter(L, f_w1, f_b1, f_w2, f_b2, f_w3, f_b3, f_w4)
    y = x0 * fft_long_conv(x1 * v, k_full, skip)
    return y @ w_out + b_out


def ec_moe(h, w_router, w1, w3, w2):
    B, L, D = h.shape
    cap = CAPACITY_FACTOR * L // N_EXPERTS
    s = jax.nn.softmax((h @ w_router).astype(jnp.float32), axis=-1)
    top_s, idx = lax.top_k(jnp.swapaxes(s, 1, 2), cap)
    xs = jax.vmap(lambda hb, ib: hb[ib])(h, idx)
    a = jax.nn.silu(jnp.einsum('becd,edf->becf', xs, w1)) * jnp.einsum('becd,edf->becf', xs, w3)
    y = jnp.einsum('becf,efd->becd', a, w2) * top_s[..., None].astype(h.dtype)
    return jax.vmap(lambda ib, yb: jnp.zeros((L, D), yb.dtype).at[ib.reshape(-1)].add(yb.reshape(-1, D)))(idx, y)


def setup_inputs(seed: int = 0) -> dict:
    key = jax.random.key(seed)
    it = iter(jax.random.split(key, 40))
    D, F, E = D_MODEL, EXPERT_FF, N_EXPERTS
    nrm = lambda shape, scale: jax.random.normal(next(it), shape, jnp.float32) * scale
    return {
        "x": nrm((BATCH, SEQ, D), 1.0),
        "c": nrm((BATCH, D), 1.0),
        "ctx": nrm((BATCH, CTX_LEN, D), 1.0),
        "c_ctx": nrm((D,), 1.0),
        "w_ada": nrm((DEPTH, D, N_ADA * D), 0.5 * D ** -0.5),
        "b_ada": nrm((DEPTH, N_ADA * D), 0.02),
        "norm_mix": 1.0 + nrm((DEPTH, D), 0.02),
        "norm_ffn": 1.0 + nrm((DEPTH, D), 0.02),
        "norm_final": 1.0 + nrm((D,), 0.02),
        "gla_w_in": nrm((N_GLA, D, GLA_IN), D ** -0.5),
        "gla_w_a1": nrm((N_GLA, 2, D, GLA_GATE_RANK), D ** -0.5),
        "gla_w_a2": nrm((N_GLA, 2, GLA_GATE_RANK, GLA_DK), GLA_GATE_RANK ** -0.5),
        "gla_b_a": nrm((N_GLA, 2, GLA_DK), 0.1),
        "gla_head_norm": 1.0 + nrm((N_GLA, GLA_DV_HEAD), 0.02),
        "gla_w_out": nrm((N_GLA, GLA_DV, D), GLA_DV ** -0.5),
        "hy_w_in": nrm((N_HYENA, D, 3 * D), D ** -0.5),
        "hy_b_in": nrm((N_HYENA, 3 * D), 0.02),
        "hy_conv_w": nrm((N_HYENA, 3, 3 * D), 3 ** -0.5),
        "hy_conv_b": nrm((N_HYENA, 3 * D), 0.02),
        "hy_f_w1": nrm((N_HYENA, HY_EMB_DIM, HY_FILTER_HIDDEN), HY_EMB_DIM ** -0.5),
        "hy_f_b1": nrm((N_HYENA, HY_FILTER_HIDDEN), 0.1),
        "hy_f_w2": nrm((N_HYENA, HY_FILTER_HIDDEN, HY_FILTER_HIDDEN), HY_FILTER_HIDDEN ** -0.5),
        "hy_f_b2": nrm((N_HYENA, HY_FILTER_HIDDEN), 0.1),
        "hy_f_w3": nrm((N_HYENA, HY_FILTER_HIDDEN, HY_FILTER_HIDDEN), HY_FILTER_HIDDEN ** -0.5),
        "hy_f_b3": nrm((N_HYENA, HY_FILTER_HIDDEN), 0.1),
        "hy_f_w4": nrm((N_HYENA, HY_FILTER_HIDDEN, 2 * D), HY_FILTER_HIDDEN ** -0.5),
        "hy_skip": nrm((N_HYENA, D), 0.1),
        "hy_w_out": nrm((N_HYENA, D, D), D ** -0.5),
        "hy_b_out": nrm((N_HYENA, D), 0.02),
        "moe_router": nrm((DEPTH, D, E), D ** -0.5),
        "moe_w1": nrm((DEPTH, E, D, F), D ** -0.5),
        "moe_w3": nrm((DEPTH, E, D, F), D ** -0.5),
        "moe_w2": nrm((DEPTH, E, F, D), F ** -0.5),
    }


def reference(x, c, ctx, c_ctx, w_ada, b_ada, norm_mix, norm_ffn, norm_final,
              gla_w_in, gla_w_a1, gla_w_a2, gla_b_a, gla_head_norm, gla_w_out,
              hy_w_in, hy_b_in, hy_conv_w, hy_conv_b, hy_f_w1, hy_f_b1, hy_f_w2, hy_f_b2,
              hy_f_w3, hy_f_b3, hy_f_w4, hy_skip, hy_w_out, hy_b_out,
              moe_router, moe_w1, moe_w3, moe_w2):
    silu_c = jax.nn.silu(c)
    silu_cc = jax.nn.silu(c_ctx)
    for i in range(DEPTH):
        need_ctx = i < DEPTH - 1
        mod = silu_c @ w_ada[i] + b_ada[i]
        mod_c = silu_cc @ w_ada[i] + b_ada[i]
        sh1, sc1, g1, sh2, sc2, g2 = jnp.split(mod[:, None, :], N_ADA, axis=-1)
        csh1, csc1, cg1, csh2, csc2, cg2 = jnp.split(mod_c, N_ADA, axis=-1)

        h = modulate(rms_norm(x, norm_mix[i]), sh1, sc1)
        hc = modulate(rms_norm(ctx, norm_mix[i]), csh1, csc1)
        j = i // 2
        if i % 2 == 0:
            o_lat, o_ctx = gla_mixer(h, hc, gla_w_in[j], gla_w_a1[j], gla_w_a2[j], gla_b_a[j],
                                     gla_head_norm[j], gla_w_out[j], need_ctx)
        else:
            hy = (hy_w_in[j], hy_b_in[j], hy_conv_w[j], hy_conv_b[j], hy_f_w1[j], hy_f_b1[j],
                  hy_f_w2[j], hy_f_b2[j], hy_f_w3[j], hy_f_b3[j], hy_f_w4[j], hy_skip[j],
                  hy_w_out[j], hy_b_out[j])
            o_lat = hyena_mixer(h, *hy, grid=True)
            o_ctx = hyena_mixer(hc, *hy, grid=False) if need_ctx else None
        x = x + g1 * o_lat
        if need_ctx:
            ctx = ctx + cg1 * o_ctx

        h = modulate(rms_norm(x, norm_ffn[i]), sh2, sc2)
        x = x + g2 * ec_moe(h, moe_router[i], moe_w1[i], moe_w3[i], moe_w2[i])
        if need_ctx:
            hc = modulate(rms_norm(ctx, norm_ffn[i]), csh2, csc2)
            ctx = ctx + cg2 * ec_moe(hc, moe_router[i], moe_w1[i], moe_w3[i], moe_w2[i])
    return rms_norm(x, norm_final)
```

```python
import math
import numpy as np
from contextlib import ExitStack
import concourse.bass as bass
import concourse.mybir as mybir
from concourse.bass_utils import run_bass_kernel_spmd

F32 = mybir.dt.float32
F32R = mybir.dt.float32r
I32 = mybir.dt.int32
U32 = mybir.dt.uint32
AF = mybir.ActivationFunctionType
ALU = mybir.AluOpType
AX = mybir.AxisListType

D = 2048
L = 2048
CL = 256
NT = L + CL
DEPTH = 4
EPS = 1e-6
NE = 16
FF = 1024
HEADS = 4
DK = 256
DV = 512
NCORES = 8

SEM_LIMIT = 16000
NDMA = 8


class Res:
    __slots__ = ("w", "r")

    def __init__(self):
        self.w = {}
        self.r = {}


class Tile:
    def __init__(self, h, nblk=1, bs=None, is_dram=False, persist=False):
        self.h = h
        self.res = [Res() for _ in range(nblk)]
        self.bs = bs
        self.is_dram = is_dram
        self.persist = persist

    def __getitem__(self, key):
        if self.is_dram:
            return self.h.ap()[key]
        return self.h[key]

    def ap(self):
        return self.h.ap() if self.is_dram else self.h[:]

    def rows(self, r0, r1):
        if self.bs is None:
            return list(self.res)
        return self.res[r0 // self.bs:(r1 - 1) // self.bs + 1]


def _flat(lst):
    out = []
    for x in lst:
        if isinstance(x, Res):
            out.append(x)
        elif isinstance(x, Tile):
            out.extend(x.res)
        else:
            out.extend(_flat(x))
    return out


class K:
    COMPUTE = ("pe", "act", "dve", "pool")
    ALL = ("pe", "act", "dve", "pool", "sp")

    def __init__(self, nc):
        self.nc = nc
        self.ges = ExitStack()
        self.sems = {}
        self.cs = {}
        self.nsem = 0
        for e in self.COMPUTE:
            self._new_csem(e)
        self.ds = {q: [[self._new_sem("d_%s%d" % (q, i)), 0] for i in range(NDMA)]
                   for q in ("sp", "pool", "act")}
        self.dn = {q: 0 for q in self.ds}
        self.ops = {e: [] for e in self.ALL}
        self.waited = {e: {} for e in self.ALL}
        self.tiles = []
        self.pes = None
        self.ntile = 0
        self.ninst = 0
        self.ccsem = None

    def _new_sem(self, name):
        name = "%s_%d" % (name, self.nsem)
        self.nsem += 1
        self.sems[name] = self.ges.enter_context(self.nc.semaphore(name))
        return name

    def _new_csem(self, e):
        self.cs[e] = [self._new_sem("c_" + e), 0]

    def dram(self, name, shape, dtype=F32, kind="Internal", bs=None, persist=False):
        h = self.nc.dram_tensor(name, list(shape), dtype, kind=kind)
        nblk = 1 if bs is None else (shape[0] + bs - 1) // bs
        t = Tile(h, nblk=nblk, bs=bs, is_dram=True, persist=persist)
        self.tiles.append(t)
        return t

    def tile(self, shape, dtype=F32, name=None):
        self.ntile += 1
        name = "%s_%d" % (name or "t", self.ntile)
        h = self.pes.enter_context(self.nc.sbuf_tensor(name, list(shape), dtype))
        t = Tile(h)
        self.tiles.append(t)
        return t

    def psum(self, shape=(128, 512), dtype=F32, name=None):
        self.ntile += 1
        name = "%s_%d" % (name or "ps", self.ntile)
        h = self.pes.enter_context(self.nc.psum_tensor(name, list(shape), dtype))
        t = Tile(h)
        self.tiles.append(t)
        return t

    def _deps(self, eng, r, w):
        deps = {}
        for x in _flat(r):
            for s, v in x.w.items():
                if deps.get(s, 0) < v:
                    deps[s] = v
        for x in _flat(w):
            for d in (x.w, x.r):
                for s, v in d.items():
                    if deps.get(s, 0) < v:
                        deps[s] = v
        waits = []
        wd = self.waited[eng]
        for s, v in deps.items():
            if eng == "pe" and s.startswith("c_pe"):
                continue
            if wd.get(s, 0) < v:
                waits.append((s, v))
                wd[s] = v
        return waits

    def _mark(self, r, w, s, v):
        for x in _flat(r):
            if x.r.get(s, 0) < v:
                x.r[s] = v
        for x in _flat(w):
            x.w = {s: v}
            x.r = {}

    def op(self, eng, fn, r=(), w=()):
        waits = self._deps(eng, r, w)
        c = self.cs[eng]
        if c[1] >= SEM_LIMIT:
            self._new_csem(eng)
            c = self.cs[eng]
        c[1] += 1
        self.ops[eng].append((waits, fn, c[0], 1))
        self._mark(r, w, c[0], c[1])
        self.ninst += 1 + len(waits)

    def dma(self, q, out, in_, r=(), w=(), fn=None, **kw):
        waits = self._deps(q, r, w)
        j = self.dn[q] % NDMA
        self.dn[q] += 1
        slot = self.ds[q][j]
        if slot[1] + 16 > SEM_LIMIT:
            slot[0] = self._new_sem("d_%s%d" % (q, j))
            slot[1] = 0
        if slot[1] > 0 and self.waited[q].get(slot[0], 0) < slot[1]:
            waits.append((slot[0], slot[1]))
            self.waited[q][slot[0]] = slot[1]
        slot[1] += 16
        if fn is None:
            def fn(e, out=out, in_=in_, kw=kw):
                return e.dma_start(out=out, in_=in_, **kw)
        self.ops[q].append((waits, fn, slot[0], 16))
        self._mark(r, w, slot[0], slot[1])
        self.ninst += 1 + len(waits)

    def coll(self, fn, r=(), w=()):
        if self.ccsem is None:
            self.ccsem = [self._new_sem("cc"), 0]
        waits = self._deps("pool", r, w)
        self.ccsem[1] += 1
        self.ops["pool"].append((waits, fn, self.ccsem[0], 1))
        self._mark(r, w, self.ccsem[0], self.ccsem[1])

    def barrier(self):
        cur = {}
        for e in self.COMPUTE:
            n, c = self.cs[e]
            if c > 0:
                cur[n] = c
        for q in self.ds:
            for n, v in self.ds[q]:
                if v > 0:
                    cur[n] = v
        for e in self.ALL:
            wd = self.waited[e]
            waits = []
            for s, v in cur.items():
                if wd.get(s, 0) < v:
                    waits.append((s, v))
                    wd[s] = v
            if waits:
                self.ops[e].append((waits, None, None, 0))
        for t in self.tiles:
            if t.persist:
                continue
            for x in t.res:
                x.w = {}
                x.r = {}

    def begin(self):
        self.pes = ExitStack()

    def end(self):
        self.barrier()
        nc = self.nc
        ops = self.ops
        sems = self.sems

        def replay(name, e):
            for waits, fn, s, inc in ops[name]:
                for ws, wv in waits:
                    e.wait_ge(sems[ws], wv)
                if fn is not None:
                    ins = fn(e)
                    ins.then_inc(sems[s], inc)

        with nc.Block() as blk:
            @blk.sync
            def _(e):
                replay("sp", e)

            @blk.scalar
            def _(e):
                replay("act", e)

            @blk.vector
            def _(e):
                replay("dve", e)

            @blk.gpsimd
            def _(e):
                replay("pool", e)

            @blk.tensor
            def _(e):
                replay("pe", e)
        self.ops = {e: [] for e in self.ALL}
        self.pes.close()
        self.pes = None
        self.tiles = [t for t in self.tiles if t.is_dram]

    def finish(self):
        self.ges.close()


class Rot:
    def __init__(self, tiles):
        self.t = tiles
        self.i = 0

    def next(self):
        t = self.t[self.i % len(self.t)]
        self.i += 1
        return t


SHARDED = {
    "w_ada": (6 * D, D, 4, 1024),
    "gla_w_in": (6144, D, 2, 2048),
    "gla_w_out": (D, D, 2, 2048),
    "hy_w_in": (3 * D, D, 2, 2048),
    "hy_w_out": (D, D, 2, 2048),
    "moe_w1": (FF, D, 64, 8192),
    "moe_w3": (FF, D, 64, 8192),
    "moe_w2": (D, FF, 64, 4096),
    "cf_l": (16 * 128, 128, 17, 2176),
    "sf_l": (16 * 128, 128, 17, 2176),
    "ci_l": (4 * 512, 128, 16, 2048),
    "nsi_l": (4 * 512, 128, 16, 2048),
}


GROUP = {"w_ada": 1, "moe_w1": 16, "moe_w3": 16, "moe_w2": 16}


def all_units():
    return {n: list(range(v[2])) for n, v in SHARDED.items()}


class Prog:
    def __init__(self, ncores=NCORES, units=None, debug_dump=None, direct=None):
        self.ncores = ncores
        self.direct = (ncores == 1) if direct is None else direct
        self.units = units if units is not None else all_units()
        self.debug_dump = debug_dump
        nc = bass.Bass("TRN2", target_bir_lowering=False)
        nc.dge_precook = False
        self.nc = nc
        self.k = K(nc)
        self.evn = 0
        self.decl()

    def decl(self):
        k = self.k
        I = lambda name, shape, dt=F32: k.dram(name, shape, dt, kind="ExternalInput")
        S = lambda name, shape, dt=F32, bs=None: k.dram(name, shape, dt, kind="Internal", bs=bs)
        self.x_in = I("x", [L, D])
        self.ctx_in = I("ctx", [CL, D])
        self.cc = I("cc", [128, 32])
        self.b_ada = I("b_ada", [DEPTH, 6 * D])
        self.norm_mix = I("norm_mix", [DEPTH, D])
        self.norm_ffn = I("norm_ffn", [DEPTH, D])
        self.norm_final = I("norm_final", [1, D])
        self.gla_wa1 = I("gla_wa1", [2, D, 32])
        self.gla_wa2 = I("gla_wa2", [2, 33, 2048])
        self.gla_hn = I("gla_hn", [2, DV])
        self.hy_cols = I("hy_cols", [2, 128, 48 * 5])
        self.hy_f_w1 = I("hy_f_w1", [2, 33, 64])
        self.hy_fb = I("hy_fb", [2, 64, 3])
        self.hy_f_w2 = I("hy_f_w2", [2, 64, 64])
        self.hy_f_w3 = I("hy_f_w3", [2, 64, 64])
        self.hy_f_w4 = I("hy_f_w4", [2, 64, 2 * D])
        self.hy_skipc = I("hy_skipc", [2, 128, 16])
        self.hy_b_out = I("hy_b_out", [2, D])
        self.moe_router = I("moe_router", [DEPTH, D, NE])
        self.wts = {}
        self.wsh = {}
        self.wbn = {}
        for n, (C, rpu, nu, CH) in SHARDED.items():
            if self.direct:
                nuse = max(1, len(self.units[n]))
                self.wts[n] = k.dram(n, [nuse * rpu, C], F32, kind="ExternalInput")
            else:
                R = nu * rpu
                gu = GROUP.get(n, nu)
                self.wsh[n] = k.dram(n, [R // NCORES, C], F32, kind="ExternalInput")
                self.wbn[n] = k.dram(n + "_bn", [R // NCORES, C], F32, kind="Internal")
                self.wts[n] = [k.dram("%s_g%d" % (n, gi), [gu * rpu, C], F32, kind="Internal", bs=CH, persist=True)
                               for gi in range(nu // gu)]
        self.ident = I("ident", [128, 128])
        self.padf = I("padf", [16, 128])
        self.tri = I("tri", [2, 128, 128])
        self.msk = I("msk", [2, 128, 128])
        self.zT = {L: I("zT_l", [33, L]), CL: I("zT_c", [33, CL])}
        self.negt = {L: I("negt_l", [128, L // 128]), CL: I("negt_c", [128, CL // 128])}
        self.delta = I("delta", [1, D])
        self.cf_c = I("cf_c", [3 * 128, 2 * 128])
        self.sf_c = I("sf_c", [3 * 128, 2 * 128])
        self.ci_c = I("ci_c", [128, 2 * 256])
        self.nsi_c = I("nsi_c", [128, 2 * 256])
        self.cin = {L: I("cin_l", [1, L]), CL: I("cin_c", [1, CL])}
        self.wn = {L: I("wn_l", [128, 17]), CL: I("wn_c", [128, 3])}
        if self.debug_dump is None:
            self.out = k.dram("out", [L, D], F32, kind="ExternalOutput", bs=128)
        else:
            self.out = k.dram("out", list(self.debug_dump[1]), F32, kind="ExternalOutput")
        self.X = S("X", [NT + 128, D], bs=128)
        self.MOD = S("MOD", [DEPTH * 2, 6 * D])
        self.HT = S("HT", [D, NT])
        self.H = S("H", [NT + 128, D])
        self.QT = S("QT", [1024, NT])
        self.KT = S("KT", [1024, NT])
        self.V = S("V", [NT, D])
        self.G = S("G", [NT, D])
        self.U = S("U", [NT, D])
        self.OG = S("OG", [NT, D])
        self.X0T = S("X0T", [D, NT])
        self.UT = S("UT", [D, NT])
        self.UTOK = S("UTOK", [NT, D])
        self.HS = S("HS", [NT, D])
        self.HD = S("HD", [NT, D])
        FR = 17 * 128 + 3 * 128
        self.KRE = S("KRE", [FR, D])
        self.KIM = S("KIM", [FR, D])
        self.URE = S("URE", [FR, D])
        self.UIM = S("UIM", [FR, D])
        self.YHT = S("YHT", [D, NT])
        self.RN = S("RN", [2 * 128, 16])
        self.IDXD = S("IDXD", [3 * 128, NE], I32)
        self.TSD = S("TSD", [3 * 128, NE])

    def wsl(self, name, unit):
        C, rpu, nu, CH = SHARDED[name]
        if self.direct:
            r0 = self.units[name].index(unit) * rpu
            return self.wts[name].ap()[r0:r0 + rpu, :], []
        gu = GROUP.get(name, nu)
        t = self.wts[name][unit // gu]
        r0 = (unit % gu) * rpu
        return t.ap()[r0:r0 + rpu, :], t.rows(r0, r0 + rpu)

    def ph_gather(self):
        k = self.k
        k.begin()
        order = []
        moe = lambda l: [(n, 4 * l + c) for c in range(4) for n in ("moe_w1", "moe_w3", "moe_w2")]
        order += [("w_ada", 0), ("w_ada", 1), ("gla_w_in", 0), ("gla_w_out", 0)] + moe(0)
        order += [("w_ada", 2), ("w_ada", 3), ("hy_w_in", 0), ("cf_l", 0), ("sf_l", 0), ("ci_l", 0), ("nsi_l", 0),
                  ("hy_w_out", 0)] + moe(1)
        order += [("w_ada", 4), ("w_ada", 5), ("gla_w_in", 1), ("gla_w_out", 1)] + moe(2)
        order += [("w_ada", 6), ("w_ada", 7), ("hy_w_in", 1), ("hy_w_out", 1)] + moe(3)
        qn = 0
        for (n, c) in order:
            C, rpu, nu, CH = SHARDED[n]
            sh, bn = self.wsh[n], self.wbn[n]
            grows = GROUP.get(n, nu) * rpu
            gt = self.wts[n][(c * CH) // grows]
            g0 = (c * CH) % grows
            pr = CH // NCORES
            qn += 1
            bres = Res()
            k.dma("sp" if qn % 2 else "act", bn[c * pr:(c + 1) * pr, :], sh[c * pr:(c + 1) * pr, :], r=[], w=[bres])
            k.coll(lambda e, bn=bn, gt=gt, c=c, pr=pr, CH=CH, g0=g0: e.collective_compute(
                "AllGather", ALU.bypass, replica_groups=[list(range(NCORES))],
                ins=[bn.ap()[c * pr:(c + 1) * pr, :]], outs=[gt.ap()[g0:g0 + CH, :]]),
                r=[bres], w=gt.rows(g0, g0 + CH))
        k.end()

    def mm(self, ps, lhsT, rhs, start, stop, r, w):
        self.k.op("pe", lambda e: e.matmul(ps, lhsT, rhs, start=start, stop=stop), r=r, w=w)

    def tr(self, ps, in_, ident, r, w):
        self.k.op("pe", lambda e: e.transpose(ps, in_, ident), r=r, w=w)

    def act(self, out, in_, func, r, w, bias=None, scale=None, accum=None):
        kw = {}
        if bias is not None:
            kw["bias"] = bias
        if scale is not None:
            kw["scale"] = scale
        if accum is not None:
            kw["accum_out"] = accum
        self.k.op("act", lambda e: e.activation(out, in_, func, **kw), r=r, w=w)

    def cp(self, eng, out, in_, r, w):
        if eng == "act":
            self.k.op("act", lambda e: e.copy(out, in_), r=r, w=w)
        else:
            self.k.op(eng, lambda e: e.tensor_copy(out, in_), r=r, w=w)

    def evac(self, out, in_, r, w):
        self.evn += 1
        self.cp("act" if self.evn % 2 else "dve", out, in_, r, w)

    def tt(self, eng, out, a, b, op, r, w):
        self.k.op(eng, lambda e: e.tensor_tensor(out=out, in0=a, in1=b, op=op), r=r, w=w)

    def ts(self, eng, out, in0, s1, s2, op0, op1, r, w):
        if s2 is None:
            self.k.op(eng, lambda e: e.tensor_scalar(out, in0, s1, None, op0=op0), r=r, w=w)
        else:
            self.k.op(eng, lambda e: e.tensor_scalar(out, in0, s1, s2, op0=op0, op1=op1), r=r, w=w)

    def stt(self, eng, out, in0, scalar, in1, op0, op1, r, w):
        self.k.op(eng, lambda e: e.scalar_tensor_tensor(out=out, in0=in0, scalar=scalar, in1=in1, op0=op0, op1=op1),
                  r=r, w=w)

    def memset(self, eng, ap, val, w):
        self.k.op(eng, lambda e: e.memset(ap, val), w=w)

    def recip(self, out, in_, r, w):
        self.k.op("dve", lambda e: e.reciprocal(out, in_), r=r, w=w)

    def load(self, q, out, in_, r, w, **kw):
        self.k.dma(q, out, in_, r=r, w=w, **kw)

    def bcload(self, tile_, dram_row_ap, r, npart=128):
        self.k.dma("pool", tile_[0:npart], dram_row_ap.partition_broadcast(npart), r=r, w=[tile_])

    def psums(self, n=8):
        return Rot([self.k.psum(name="ps%d" % i) for i in range(n)])

    def rstd(self, ss, rs, n, r):
        self.act(rs, ss, AF.Sqrt, r=r, w=r, scale=1.0 / n, bias=EPS)
        self.recip(rs, rs, r=r, w=r)

    def ph_init(self):
        k = self.k
        k.begin()
        k.dma("sp", self.X[0:L, :], self.x_in.ap(), r=[self.x_in], w=self.X.rows(0, L))
        k.dma("act", self.X[L:NT, :], self.ctx_in.ap(), r=[self.ctx_in], w=self.X.rows(L, NT))
        k.end()

    def ph_mods(self, i):
        k = self.k
        k.begin()
        ps = self.psums(4)
        cct = k.tile([128, 32], name="cct")
        sc = k.tile([128, 32], F32R, name="sc")
        self.load("sp", cct[:], self.cc.ap(), r=[self.cc], w=[cct])
        self.act(sc[:], cct[:], AF.Silu, r=[cct], w=[sc])
        scv = sc[:].rearrange("p (kc r) -> p kc r", r=2)
        wp = Rot([k.tile([128, 16, 512], F32R, name="wada") for _ in range(3)])
        bp = Rot([k.tile([2, 512], name="bada") for _ in range(2)])
        mp = Rot([k.tile([2, 512], name="modo") for _ in range(2)])
        if True:
            wap, wres = self.wsl("w_ada", i)
            wv = wap.rearrange("(kc p) n -> p kc n", p=128)
            for nb in range(24):
                wt = wp.next()
                bt = bp.next()
                mt = mp.next()
                self.load("sp" if nb % 2 == 0 else "act", wt[:], wv[:, :, nb * 512:(nb + 1) * 512].bitcast(F32R),
                          r=wres, w=[wt])
                self.bcload(bt, self.b_ada[i:i + 1, nb * 512:(nb + 1) * 512], r=[self.b_ada], npart=2)
                p = ps.next()
                for kc in range(16):
                    self.mm(p[0:2, :], scv[:, kc, :], wt[:, kc, :], kc == 0, kc == 15, r=[sc, wt], w=[p])
                self.tt("dve", mt[:], p[0:2, :], bt[:], ALU.add, r=[p, bt], w=[mt])
                k.dma("pool", self.MOD[2 * i:2 * i + 2, nb * 512:(nb + 1) * 512], mt[:], r=[mt], w=[self.MOD])
        k.end()

    def mod_row(self, i, r, chunk):
        return self.MOD[2 * i + r:2 * i + r + 1, chunk * D:(chunk + 1) * D]

    def tok_tiles(self, with_ctx):
        return list(range(18 if with_ctx else 16))

    def blocks(self, with_ctx):
        b = [(t0, 512) for t0 in range(0, L, 512)]
        if with_ctx:
            b.append((L, CL))
        return b

    def ph_norm(self, i, kind, with_ctx):
        k = self.k
        k.begin()
        ps = self.psums(6)
        ident = k.tile([128, 128], name="ident")
        self.load("sp", ident[:], self.ident.ap(), r=[self.ident], w=[ident])
        gsrc = self.norm_mix if kind == "mix" else self.norm_ffn
        shc, scc = (0, 1) if kind == "mix" else (3, 4)
        gam = k.tile([128, D], name="gam")
        self.bcload(gam, gsrc[i:i + 1, :], r=[gsrc])
        gm, sh = [], []
        for r_ in range(2 if with_ctx else 1):
            g_ = k.tile([128, D], name="gm")
            s_ = k.tile([128, D], name="sh")
            self.bcload(g_, self.mod_row(i, r_, scc), r=[self.MOD])
            self.stt("dve", g_[:], g_[:], 1.0, gam[:], ALU.add, ALU.mult, r=[g_, gam], w=[g_])
            self.bcload(s_, self.mod_row(i, r_, shc), r=[self.MOD])
            gm.append(g_)
            sh.append(s_)
        xp = Rot([k.tile([128, D], name="xt") for _ in range(2)])
        hp = Rot([k.tile([128, D], name="ht") for _ in range(2)])
        junk = k.tile([128, D], name="junk")
        ssp = Rot([k.tile([128, 2], name="ss") for _ in range(3)])
        if kind == "mix":
            hTb = Rot([k.tile([128, 16, 512], name="hTb") for _ in range(2)])
        else:
            hTt = Rot([k.tile([128, 16, 128], name="hTt") for _ in range(2)])
            wr = k.tile([128, 16, NE], name="wr")
            self.load("sp", wr[:], self.moe_router[i].rearrange("(kc p) e -> p kc e", p=128),
                      r=[self.moe_router], w=[wr])
            ST = k.tile([16, NT], name="ST")
            smp = Rot([k.tile([128, 40], name="sm") for _ in range(2)])
        tiles = self.tok_tiles(with_ctx)
        cur = None
        for tt_ in tiles:
            r_ = 1 if tt_ >= 16 else 0
            r0 = tt_ * 128
            xt = xp.next()
            ht = hp.next()
            ss = ssp.next()
            self.load("sp", xt[:], self.X[r0:r0 + 128, :], r=self.X.rows(r0, r0 + 128), w=[xt])
            self.memset("pool", ss[:], 0.0, w=[ss])
            self.act(junk[:], xt[:], AF.Square, r=[xt], w=[junk, ss], accum=ss[:, 0:1])
            self.rstd(ss[:, 0:1], ss[:, 1:2], D, r=[ss])
            self.stt("dve", ht[:], xt[:], ss[:, 1:2], gm[r_][:], ALU.mult, ALU.mult, r=[xt, ss, gm[r_]], w=[ht])
            self.tt("pool", ht[:], ht[:], sh[r_][:], ALU.add, r=[ht, sh[r_]], w=[ht])
            if kind == "mix":
                bi = tt_ // 4
                tl = tt_ % 4
                if tl == 0:
                    cur = hTb.next()
                for g in range(4):
                    p = ps.next()
                    for q in range(4):
                        kc = g * 4 + q
                        self.tr(p[:, q * 128:(q + 1) * 128], ht[:, kc * 128:(kc + 1) * 128], ident[:],
                                r=[ht, ident], w=[p])
                    self.evac(cur[:, g * 4:(g + 1) * 4, tl * 128:(tl + 1) * 128],
                              p[:].rearrange("p (a b) -> p a b", b=128), r=[p], w=[cur])
                last_in_block = (tl == 3) or (tt_ == tiles[-1])
                if last_in_block:
                    t0 = bi * 512
                    nt = (tl + 1) * 128
                    k.dma("pool", self.HT.ap().rearrange("(kc p) t -> p kc t", p=128)[:, :, t0:t0 + nt],
                          cur[:, :, 0:nt], r=[cur], w=[self.HT])
            else:
                k.dma("pool", self.H[r0:r0 + 128, :], ht[:], r=[ht], w=[self.H])
                hT = hTt.next()
                for g in range(4):
                    p = ps.next()
                    for q in range(4):
                        kc = g * 4 + q
                        self.tr(p[:, q * 128:(q + 1) * 128], ht[:, kc * 128:(kc + 1) * 128], ident[:],
                                r=[ht, ident], w=[p])
                    self.evac(hT[:, g * 4:(g + 1) * 4, :], p[:].rearrange("p (a b) -> p a b", b=128), r=[p], w=[hT])
                p = ps.next()
                for kc in range(16):
                    self.mm(p[:, 0:NE], hT[:, kc, :], wr[:, kc, :], kc == 0, kc == 15, r=[hT, wr], w=[p])
                sm = smp.next()
                k.op("dve", lambda e, sm=sm, p=p: e.tensor_reduce(out=sm[:, 32:33], in_=p[:, 0:NE], axis=AX.X,
                                                                  op=ALU.max), r=[p], w=[sm])
                self.ts("dve", sm[:, 33:34], sm[:, 32:33], -1.0, None, ALU.mult, None, r=[sm], w=[sm])
                self.memset("pool", sm[:, 34:35], 0.0, w=[sm])
                self.act(sm[:, 0:NE], p[:, 0:NE], AF.Exp, r=[p, sm], w=[sm], bias=sm[:, 33:34], accum=sm[:, 34:35])
                self.recip(sm[:, 35:36], sm[:, 34:35], r=[sm], w=[sm])
                self.ts("dve", sm[:, 16:32], sm[:, 0:NE], sm[:, 35:36], None, ALU.mult, None, r=[sm], w=[sm])
                p2 = ps.next()
                self.tr(p2[0:NE, 0:128], sm[:, 16:32], ident[:], r=[sm, ident], w=[p2])
                self.evac(ST[:, r0:r0 + 128], p2[0:NE, 0:128], r=[p2], w=[ST])
        if kind == "ffn":
            self.topk(ST, ident, ps, with_ctx)
        k.end()

    def topk(self, ST, ident, ps, with_ctx):
        k = self.k
        work = k.tile([16, L], name="work")
        vals = k.tile([16, 384], name="vals")
        idxu = k.tile([16, 384], U32, name="idxu")
        idxf = k.tile([16, 384], name="idxf")
        self.memset("pool", vals[:], 0.0, w=[vals])
        self.load("sp", idxf[:, 256:384], self.padf.ap(), r=[], w=[idxf])
        self.cp("dve", work[:], ST[:, 0:L], r=[ST], w=[work])

        def run(wk, n, off):
            for it in range(n // 8):
                sl = slice(off + it * 8, off + it * 8 + 8)
                k.op("dve", lambda e, sl=sl: e.max(out=vals[:, sl], in_=wk), r=[work], w=[vals])
                k.op("dve", lambda e, sl=sl: e.max_index(out=idxu[:, sl], in_max=vals[:, sl], in_values=wk),
                     r=[work, vals], w=[idxu])
                k.op("dve", lambda e, sl=sl: e.match_replace(out=wk, in_to_replace=vals[:, sl], in_values=wk,
                                                             imm_value=-1.0), r=[work, vals], w=[work])

        run(work[:, 0:L], 256, 0)
        self.cp("dve", idxf[:, 0:256], idxu[:, 0:256], r=[idxu], w=[idxf])
        if with_ctx:
            self.cp("dve", work[:, 0:CL], ST[:, L:NT], r=[ST, work], w=[work])
            run(work[:, 0:CL], 32, 256)
            self.cp("dve", idxf[:, 256:288], idxu[:, 256:288], r=[idxu], w=[idxf])
            self.ts("dve", idxf[:, 256:288], idxf[:, 256:288], float(L), None, ALU.add, None, r=[idxf], w=[idxf])
        for c in range(3):
            p = ps.next()
            self.tr(p[:, 0:NE], idxf[:, c * 128:(c + 1) * 128], ident[0:16, 0:16], r=[idxf, ident], w=[p])
            it_ = k.tile([128, NE], I32, name="idxi")
            self.cp("dve", it_[:], p[:, 0:NE], r=[p], w=[it_])
            k.dma("pool", self.IDXD[c * 128:(c + 1) * 128, :], it_[:], r=[it_], w=[self.IDXD])
            p = ps.next()
            self.tr(p[:, 0:NE], vals[:, c * 128:(c + 1) * 128], ident[0:16, 0:16], r=[vals, ident], w=[p])
            vt = k.tile([128, NE], name="valt")
            self.cp("dve", vt[:], p[:, 0:NE], r=[p], w=[vt])
            k.dma("pool", self.TSD[c * 128:(c + 1) * 128, :], vt[:], r=[vt], w=[self.TSD])

    def ph_experts(self, i, with_ctx):
        k = self.k
        k.begin()
        ps = self.psums(8)
        ident = k.tile([128, 128], name="ident")
        self.load("sp", ident[:], self.ident.ap(), r=[self.ident], w=[ident])
        nct = 3 if with_ctx else 2
        NC_ = 288 if with_ctx else 256
        idx = [k.tile([128, NE], I32, name="idx") for _ in range(nct)]
        tsv = [k.tile([128, NE], name="tsv") for _ in range(nct)]
        for c in range(nct):
            self.load("sp", idx[c][:], self.IDXD[c * 128:(c + 1) * 128, :], r=[self.IDXD], w=[idx[c]])
            self.load("sp", tsv[c][:], self.TSD[c * 128:(c + 1) * 128, :], r=[self.TSD], w=[tsv[c]])
        g2 = []
        for r_ in range(2 if with_ctx else 1):
            g_ = k.tile([128, D], name="g2")
            self.bcload(g_, self.mod_row(i, r_, 5), r=[self.MOD])
            g2.append(g_)
        xs = [k.tile([128, D], name="xs") for _ in range(nct)]
        yo = [k.tile([128, D], name="yo") for _ in range(nct)]
        xsT = k.tile([128, 16, NC_], F32R, name="xsT")
        aT = k.tile([128, 8, NC_], F32R, name="aT")
        tmp = Rot([k.tile([128, NC_], name="tmp") for _ in range(2)])
        wp = Rot([k.tile([128, 4096], F32R, name="wexp") for _ in range(5)])
        csz = [128, 128, 32]
        if with_ctx:
            self.memset("pool", xs[2][:], 0.0, w=[xs[2]])
            self.memset("pool", yo[2][:], 0.0, w=[yo[2]])
        qi = 0
        for e in range(NE):
            for c in range(nct):
                k.dma("pool", None, None, r=[idx[c], self.H], w=[xs[c]],
                      fn=lambda eng, c=c, e=e: eng.indirect_dma_start(
                          out=xs[c][:], out_offset=None, in_=self.H.ap(),
                          in_offset=bass.IndirectOffsetOnAxis(ap=idx[c][:, e:e + 1], axis=0)))
            for kc in range(16):
                p = ps.next()
                for c in range(nct):
                    if c < 2:
                        self.tr(p[:, c * 128:(c + 1) * 128], xs[c][:, kc * 128:(kc + 1) * 128], ident[:],
                                r=[xs[c], ident], w=[p])
                    else:
                        self.tr(p[:, 256:288], xs[c][0:32, kc * 128:(kc + 1) * 128], ident[0:32, 0:32],
                                r=[xs[c], ident], w=[p])
                self.evac(xsT[:, kc, :], p[:, 0:NC_], r=[p], w=[xsT])
            w1ap, w1res = self.wsl("moe_w1", i * NE + e)
            w3ap, w3res = self.wsl("moe_w3", i * NE + e)
            w2ap, w2res = self.wsl("moe_w2", i * NE + e)
            w1v = w1ap.rearrange("(kc p) f -> p kc f", p=128)
            w3v = w3ap.rearrange("(kc p) f -> p kc f", p=128)
            w2v = w2ap.rearrange("(fc p) d -> p fc d", p=128)
            for fq in range(4):
                w1q = wp.next()
                w3q = wp.next()
                w1a = w1q[:].rearrange("p (kc f) -> p kc f", f=256)
                w3a = w3q[:].rearrange("p (kc f) -> p kc f", f=256)
                qi += 1
                self.load("sp" if qi % 2 else "act", w1a, w1v[:, :, fq * 256:(fq + 1) * 256].bitcast(F32R),
                          r=w1res, w=[w1q])
                self.load("act" if qi % 2 else "sp", w3a, w3v[:, :, fq * 256:(fq + 1) * 256].bitcast(F32R),
                          r=w3res, w=[w3q])
                for fl in range(2):
                    fc = fq * 2 + fl
                    p1 = ps.next()
                    p3 = ps.next()
                    for kc in range(16):
                        self.mm(p1[:, 0:NC_], w1a[:, kc, fl * 128:(fl + 1) * 128], xsT[:, kc, :], kc == 0, kc == 15,
                                r=[w1q, xsT], w=[p1])
                    for kc in range(16):
                        self.mm(p3[:, 0:NC_], w3a[:, kc, fl * 128:(fl + 1) * 128], xsT[:, kc, :], kc == 0, kc == 15,
                                r=[w3q, xsT], w=[p3])
                    t_ = tmp.next()
                    self.act(t_[:], p1[:, 0:NC_], AF.Silu, r=[p1], w=[t_])
                    self.tt("dve", aT[:, fc, :], t_[:], p3[:, 0:NC_], ALU.mult, r=[t_, p3], w=[aT])
            for dq in range(4):
                w2q = wp.next()
                w2a = w2q[:].rearrange("p (fc d) -> p fc d", d=512)
                qi += 1
                self.load("sp" if qi % 2 else "act", w2a, w2v[:, :, dq * 512:(dq + 1) * 512].bitcast(F32R),
                          r=w2res, w=[w2q])
                for c in range(nct):
                    cs_ = csz[c]
                    p = ps.next()
                    for fc in range(8):
                        self.mm(p[0:cs_, :], aT[:, fc, c * 128:c * 128 + cs_], w2a[:, fc, :], fc == 0, fc == 7,
                                r=[aT, w2q], w=[p])
                    g_ = g2[1 if c == 2 else 0]
                    self.stt("dve", yo[c][0:cs_, dq * 512:(dq + 1) * 512], p[0:cs_, :], tsv[c][0:cs_, e:e + 1],
                             g_[0:cs_, dq * 512:(dq + 1) * 512], ALU.mult, ALU.mult, r=[p, tsv[c], g_], w=[yo[c]])
            for c in range(nct):
                k.dma("pool", None, None, r=[idx[c], yo[c]], w=[self.X],
                      fn=lambda eng, c=c, e=e: eng.indirect_dma_start(
                          out=self.X.ap(), out_offset=bass.IndirectOffsetOnAxis(ap=idx[c][:, e:e + 1], axis=0),
                          in_=yo[c][:], in_offset=None, compute_op=ALU.add))
        k.end()

    def ph_gla_proj(self, j, with_ctx=True):
        k = self.k
        k.begin()
        ps = self.psums(8)
        hTp = Rot([k.tile([128, 16, 512], F32R, name="hT") for _ in range(2)])
        wp = Rot([k.tile([128, 16, 512], F32R, name="w") for _ in range(2)])
        op_ = Rot([k.tile([128, 512], name="o") for _ in range(4)])
        wa1 = k.tile([128, 16, 32], F32R, name="wa1")
        wa2 = k.tile([33, 2048], name="wa2")
        lowT = k.tile([33, 512], name="lowT")
        self.load("sp", wa1[:], self.gla_wa1[j].rearrange("(kc p) r -> p kc r", p=128).bitcast(F32R),
                  r=[self.gla_wa1], w=[wa1])
        self.load("sp", wa2[:], self.gla_wa2[j], r=[self.gla_wa2], w=[wa2])
        self.memset("pool", lowT[:], 1.0, w=[lowT])
        wap, wres = self.wsl("gla_w_in", j)
        wv = wap.rearrange("(kc p) n -> p kc n", p=128)
        HTv = self.HT.ap().rearrange("(kc p) t -> p kc t", p=128)
        qn = 0
        for (t0, TB) in self.blocks(with_ctx):
            hT = hTp.next()
            self.load("act", hT[:, :, 0:TB], HTv[:, :, t0:t0 + TB].bitcast(F32R), r=[self.HT], w=[hT])
            for nb in range(12):
                wt = wp.next()
                qn += 1
                self.load("sp" if qn % 2 else "act", wt[:], wv[:, :, nb * 512:(nb + 1) * 512].bitcast(F32R),
                          r=wres, w=[wt])
                if nb < 4:
                    dst = self.QT if nb < 2 else self.KT
                    for nl in range(4):
                        p = ps.next()
                        for kc in range(16):
                            self.mm(p[:, 0:TB], wt[:, kc, nl * 128:(nl + 1) * 128], hT[:, kc, 0:TB], kc == 0, kc == 15,
                                    r=[wt, hT], w=[p])
                        o = op_.next()
                        if nb < 2:
                            self.act(o[:, 0:TB], p[:, 0:TB], AF.Copy, r=[p], w=[o], scale=DK ** -0.5)
                        else:
                            self.evac(o[:, 0:TB], p[:, 0:TB], r=[p], w=[o])
                        f0 = ((nb % 2) * 4 + nl) * 128
                        k.dma("pool", dst[f0:f0 + 128, t0:t0 + TB], o[:, 0:TB], r=[o], w=[dst])
                else:
                    dst = self.V if nb < 8 else self.G
                    c0 = ((nb - 4) % 4) * 512
                    for tl in range(TB // 128):
                        p = ps.next()
                        for kc in range(16):
                            self.mm(p[:], hT[:, kc, tl * 128:(tl + 1) * 128], wt[:, kc, :], kc == 0, kc == 15,
                                    r=[wt, hT], w=[p])
                        o = op_.next()
                        if nb < 8:
                            self.evac(o[:], p[:], r=[p], w=[o])
                        else:
                            self.act(o[:], p[:], AF.Silu, r=[p], w=[o])
                        r0 = t0 + tl * 128
                        k.dma("pool", dst[r0:r0 + 128, c0:c0 + 512], o[:], r=[o], w=[dst])
            p = ps.next()
            for kc in range(16):
                self.mm(p[0:32, 0:TB], wa1[:, kc, :], hT[:, kc, 0:TB], kc == 0, kc == 15, r=[wa1, hT], w=[p])
            self.evac(lowT[0:32, 0:TB], p[0:32, 0:TB], r=[p], w=[lowT])
            for tl in range(TB // 128):
                for cb in range(4):
                    p = ps.next()
                    self.mm(p[:], lowT[0:33, tl * 128:(tl + 1) * 128], wa2[0:33, cb * 512:(cb + 1) * 512], True, True,
                            r=[lowT, wa2], w=[p])
                    o = op_.next()
                    self.act(o[:], p[:], AF.Exp, r=[p], w=[o], scale=-1.0)
                    self.act(o[:], o[:], AF.Ln, r=[o], w=[o], bias=1.0)
                    r0 = t0 + tl * 128
                    k.dma("pool", self.U[r0:r0 + 128, cb * 512:(cb + 1) * 512], o[:], r=[o], w=[self.U])
        k.end()

    def ph_gla_scan(self, j):
        k = self.k
        k.begin()
        ps = self.psums(8)
        ident = k.tile([128, 128], name="ident")
        self.load("sp", ident[:], self.ident.ap(), r=[self.ident], w=[ident])
        tri = [k.tile([128, 128], name="tri") for _ in range(2)]
        msk = [k.tile([128, 128], name="msk") for _ in range(2)]
        for z in range(2):
            self.load("sp", tri[z][:], self.tri[z], r=[self.tri], w=[tri[z]])
            self.load("sp", msk[z][:], self.msk[z], r=[self.msk], w=[msk[z]])
        hn = k.tile([128, DV], name="hn")
        self.bcload(hn, self.gla_hn[j:j + 1, :], r=[self.gla_hn])
        oacc = k.tile([128, 18, DV], name="oacc")
        S = [k.tile([128, DV], name="S") for _ in range(2)]
        qp = Rot([k.tile([128, 2, 128], name="q") for _ in range(3)])
        kp = Rot([k.tile([128, 2, 128], name="k") for _ in range(3)])
        vp = Rot([k.tile([128, DV], name="v") for _ in range(3)])
        up = Rot([k.tile([128, 256], name="u") for _ in range(3)])
        ebp = Rot([k.tile([128, 256], name="eb") for _ in range(2)])
        enp = Rot([k.tile([128, 256], name="en") for _ in range(2)])
        qsp = Rot([k.tile([128, 2, 128], name="qs") for _ in range(2)])
        ksp = Rot([k.tile([128, 2, 128], name="ks") for _ in range(2)])
        ktp = Rot([k.tile([128, 256], name="kt") for _ in range(2)])
        atp = Rot([k.tile([128, 128], name="at") for _ in range(2)])
        tsp = Rot([k.tile([128, DV], name="tS") for _ in range(2)])
        gp = Rot([k.tile([128, DV], name="g") for _ in range(2)])
        onp = Rot([k.tile([128, DV], name="on") for _ in range(2)])
        junk = k.tile([128, DV], name="junk")
        ssp = Rot([k.tile([128, 2], name="ss") for _ in range(3)])
        QTv = self.QT.ap().rearrange("(c p) t -> p c t", p=128)
        KTv = self.KT.ap().rearrange("(c p) t -> p c t", p=128)
        for h in range(HEADS):
            for z in range(2):
                order = [16, 17] + list(range(16)) if z == 0 else [17, 16] + list(range(15, -1, -1))
                last = 127 if z == 0 else 0
                for dc in range(2):
                    self.memset("pool", S[dc][:], 0.0, w=[S[dc]])
                for ti in order:
                    r0 = ti * 128
                    q_ = qp.next()
                    k_ = kp.next()
                    v_ = vp.next()
                    u_ = up.next()
                    self.load("sp", q_[:], QTv[:, 2 * h:2 * h + 2, r0:r0 + 128], r=[self.QT], w=[q_])
                    self.load("act", k_[:], KTv[:, 2 * h:2 * h + 2, r0:r0 + 128], r=[self.KT], w=[k_])
                    self.load("sp", v_[:], self.V[r0:r0 + 128, h * DV:(h + 1) * DV], r=[self.V], w=[v_])
                    c0 = z * 1024 + h * DK
                    self.load("act", u_[:], self.U[r0:r0 + 128, c0:c0 + DK], r=[self.U], w=[u_])
                    pb = ps.next()
                    for dc in range(2):
                        self.mm(pb[:, dc * 128:(dc + 1) * 128], u_[:, dc * 128:(dc + 1) * 128], tri[z][:], True, True,
                                r=[u_, tri[z]], w=[pb])
                    eb = ebp.next()
                    en = enp.next()
                    self.act(eb[:], pb[:, 0:256], AF.Exp, r=[pb], w=[eb])
                    self.act(en[:], pb[:, 0:256], AF.Exp, r=[pb], w=[en], scale=-1.0)
                    qs = qsp.next()
                    ks = ksp.next()
                    self.tt("dve", qs[:], q_[:], eb[:].rearrange("p (c t) -> p c t", t=128), ALU.mult, r=[q_, eb], w=[qs])
                    self.tt("pool", ks[:], k_[:], en[:].rearrange("p (c t) -> p c t", t=128), ALU.mult, r=[k_, en], w=[ks])
                    pt = ps.next()
                    for dc in range(2):
                        self.tr(pt[:, dc * 128:(dc + 1) * 128], ks[:, dc, :], ident[:], r=[ks, ident], w=[pt])
                    kt = ktp.next()
                    self.evac(kt[:], pt[:, 0:256], r=[pt], w=[kt])
                    pa = ps.next()
                    for dc in range(2):
                        self.mm(pa[:, 0:128], ks[:, dc, :], qs[:, dc, :], dc == 0, dc == 1, r=[ks, qs], w=[pa])
                    at = atp.next()
                    self.tt("dve", at[:], pa[:, 0:128], msk[z][:], ALU.mult, r=[pa, msk[z]], w=[at])
                    po = ps.next()
                    self.mm(po[:], at[:], v_[:], True, False, r=[at, v_], w=[po])
                    for dc in range(2):
                        self.mm(po[:], qs[:, dc, :], S[dc][:], False, dc == 1, r=[qs, S[dc]], w=[po])
                    if z == 0:
                        self.evac(oacc[:, ti, :], po[:], r=[po], w=[oacc])
                    else:
                        self.tt("dve", oacc[:, ti, :], oacc[:, ti, :], po[:], ALU.add, r=[oacc, po], w=[oacc])
                    for dc in range(2):
                        p_s = ps.next()
                        self.mm(p_s[:], kt[:, dc * 128:(dc + 1) * 128], v_[:], True, True, r=[kt, v_], w=[p_s])
                        tS = tsp.next()
                        self.tt("dve", tS[:], S[dc][:], p_s[:], ALU.add, r=[S[dc], p_s], w=[tS])
                        col = dc * 128 + last
                        self.act(S[dc][:], tS[:], AF.Copy, r=[tS, eb], w=[S[dc]], scale=eb[:, col:col + 1])
            for ti in range(18):
                r0 = ti * 128
                ss = ssp.next()
                g_ = gp.next()
                on = onp.next()
                self.load("sp", g_[:], self.G[r0:r0 + 128, h * DV:(h + 1) * DV], r=[self.G], w=[g_])
                self.memset("pool", ss[:], 0.0, w=[ss])
                self.act(junk[:], oacc[:, ti, :], AF.Square, r=[oacc], w=[junk, ss], accum=ss[:, 0:1])
                self.rstd(ss[:, 0:1], ss[:, 1:2], DV, r=[ss])
                self.stt("dve", on[:], oacc[:, ti, :], ss[:, 1:2], hn[:], ALU.mult, ALU.mult, r=[oacc, ss, hn], w=[on])
                self.tt("pool", on[:], on[:], g_[:], ALU.mult, r=[on, g_], w=[on])
                k.dma("pool", self.OG[r0:r0 + 128, h * DV:(h + 1) * DV], on[:], r=[on], w=[self.OG])
        k.end()

    def ph_outproj(self, i, wname, wunit, src, src_major, with_ctx, bias_row=None):
        k = self.k
        k.begin()
        ps = self.psums(8)
        ident = k.tile([128, 128], name="ident")
        self.load("sp", ident[:], self.ident.ap(), r=[self.ident], w=[ident])
        g1 = []
        for r_ in range(2 if with_ctx else 1):
            g_ = k.tile([128, D], name="g1")
            self.bcload(g_, self.mod_row(i, r_, 2), r=[self.MOD])
            g1.append(g_)
        bo = None
        if bias_row is not None:
            bo = k.tile([128, D], name="bo")
            self.bcload(bo, bias_row, r=[self.hy_b_out])
        sT = k.tile([128, 16, 512], F32R, name="sT")
        wp = Rot([k.tile([128, 16, 512], F32R, name="w") for _ in range(2)])
        xt = [k.tile([128, D], name="xt") for _ in range(4)]
        sp_ = Rot([k.tile([128, D], name="st") for _ in range(2)])
        tp = Rot([k.tile([128, 512], name="tp") for _ in range(3)])
        wap, wres = self.wsl(wname, wunit)
        wv = wap.rearrange("(kc p) n -> p kc n", p=128)
        qn = 0
        for (t0, TB) in self.blocks(with_ctx):
            r_ = 1 if t0 >= L else 0
            ntl = TB // 128
            if src_major == "tok":
                for tl in range(ntl):
                    st = sp_.next()
                    r0 = t0 + tl * 128
                    self.load("act", st[:], src[r0:r0 + 128, :], r=[src], w=[st])
                    for g in range(4):
                        p = ps.next()
                        for q in range(4):
                            kc = g * 4 + q
                            self.tr(p[:, q * 128:(q + 1) * 128], st[:, kc * 128:(kc + 1) * 128], ident[:],
                                    r=[st, ident], w=[p])
                        self.evac(sT[:, g * 4:(g + 1) * 4, tl * 128:(tl + 1) * 128],
                                  p[:].rearrange("p (a b) -> p a b", b=128), r=[p], w=[sT])
            else:
                self.load("act", sT[:, :, 0:TB],
                          src.ap().rearrange("(kc p) t -> p kc t", p=128)[:, :, t0:t0 + TB].bitcast(F32R),
                          r=[src], w=[sT])
            for tl in range(ntl):
                r0 = t0 + tl * 128
                self.load("sp", xt[tl][:], self.X[r0:r0 + 128, :], r=self.X.rows(r0, r0 + 128), w=[xt[tl]])
            for nb in range(4):
                wt = wp.next()
                qn += 1
                self.load("sp" if qn % 2 else "act", wt[:], wv[:, :, nb * 512:(nb + 1) * 512].bitcast(F32R),
                          r=wres, w=[wt])
                for tl in range(ntl):
                    p = ps.next()
                    for kc in range(16):
                        self.mm(p[:], sT[:, kc, tl * 128:(tl + 1) * 128], wt[:, kc, :], kc == 0, kc == 15,
                                r=[sT, wt], w=[p])
                    t_ = tp.next()
                    sl = slice(nb * 512, (nb + 1) * 512)
                    if bo is not None:
                        self.tt("dve", t_[:], p[:], bo[:, sl], ALU.add, r=[p, bo], w=[t_])
                        self.tt("pool", t_[:], t_[:], g1[r_][:, sl], ALU.mult, r=[t_, g1[r_]], w=[t_])
                    else:
                        self.tt("dve", t_[:], p[:], g1[r_][:, sl], ALU.mult, r=[p, g1[r_]], w=[t_])
                    self.tt("pool", xt[tl][:, sl], xt[tl][:, sl], t_[:], ALU.add, r=[xt[tl], t_], w=[xt[tl]])
            for tl in range(ntl):
                r0 = t0 + tl * 128
                k.dma("pool", self.X[r0:r0 + 128, :], xt[tl][:], r=[xt[tl]], w=self.X.rows(r0, r0 + 128))
        k.end()

    def ph_hy_filter(self, j, Ls, row0, rn_row0):
        k = self.k
        k.begin()
        ps = self.psums(6)
        MT = Ls // 128
        zT = k.tile([33, Ls], name="zT")
        self.load("sp", zT[:], self.zT[Ls].ap(), r=[self.zT[Ls]], w=[zT])
        w1 = k.tile([33, 64], name="fw1")
        w2 = k.tile([64, 64], name="fw2")
        w3 = k.tile([64, 64], name="fw3")
        w4 = k.tile([64, 2 * D], name="fw4")
        fb = k.tile([64, 3], name="fb")
        fb2 = k.tile([64, 3], name="fb2")
        self.load("sp", w1[:], self.hy_f_w1[j], r=[self.hy_f_w1], w=[w1])
        self.load("sp", w2[:], self.hy_f_w2[j], r=[self.hy_f_w2], w=[w2])
        self.load("sp", w3[:], self.hy_f_w3[j], r=[self.hy_f_w3], w=[w3])
        self.load("act", w4[:], self.hy_f_w4[j], r=[self.hy_f_w4], w=[w4])
        self.load("sp", fb[:], self.hy_fb[j], r=[self.hy_fb], w=[fb])
        OFF = 16.0
        self.ts("dve", fb2[:], fb[:], 1.0 / (2 * math.pi), 0.5 + OFF, ALU.mult, ALU.add, r=[fb], w=[fb2])
        hT = [k.tile([64, Ls], name="hid") for _ in range(3)]
        up = Rot([k.tile([64, 512], name="u") for _ in range(2)])
        uip = Rot([k.tile([64, 512], I32, name="ui") for _ in range(2)])
        ufp = Rot([k.tile([64, 512], name="uf") for _ in range(2)])
        SE = 1e-6
        srcs = [(zT, 33, w1), (hT[0], 64, w2), (hT[1], 64, w3)]
        for li in range(3):
            src, K_, w_ = srcs[li]
            for lb in range((Ls + 511) // 512):
                wdt = min(512, Ls - lb * 512)
                sl = slice(lb * 512, lb * 512 + wdt)
                p = ps.next()
                self.mm(p[0:64, 0:wdt], w_[0:K_, :], src[0:K_, sl], True, True, r=[w_, src], w=[p])
                u = up.next()
                ui = uip.next()
                uf = ufp.next()
                self.act(u[:, 0:wdt], p[0:64, 0:wdt], AF.Identity, r=[p, fb2], w=[u], scale=1.0 / (2 * math.pi),
                         bias=fb2[:, li:li + 1])
                self.cp("dve", ui[:, 0:wdt], u[:, 0:wdt], r=[u], w=[ui])
                self.cp("dve", uf[:, 0:wdt], ui[:, 0:wdt], r=[ui], w=[uf])
                self.tt("dve", u[:, 0:wdt], u[:, 0:wdt], uf[:, 0:wdt], ALU.subtract, r=[u, uf], w=[u])
                self.stt("dve", uf[:, 0:wdt], u[:, 0:wdt], 0.0, u[:, 0:wdt], ALU.is_lt, ALU.add, r=[u], w=[uf])
                self.act(hT[li][:, sl], uf[:, 0:wdt], AF.Sin, r=[uf], w=[hT[li]], scale=2 * math.pi * (1 - SE),
                         bias=-math.pi * (1 - SE))
        negt = k.tile([128, MT], name="negt")
        self.load("sp", negt[:], self.negt[Ls].ap(), r=[self.negt[Ls]], w=[negt])
        dl = k.tile([128, D], name="delta")
        self.bcload(dl, self.delta[0:1, :], r=[self.delta])
        ones = k.tile([128, 1], name="ones")
        self.memset("pool", ones[:], 1.0, w=[ones])
        absum = k.tile([128, D], name="absum")
        self.memset("pool", absum[:], 0.0, w=[absum])
        decp = Rot([k.tile([128, D], name="dec") for _ in range(2)])
        hfp = Rot([k.tile([128, D], name="hf") for _ in range(2)])
        hbp = Rot([k.tile([128, D], name="hb") for _ in range(2)])
        hsp = Rot([k.tile([128, D], name="hs") for _ in range(2)])
        hdp = Rot([k.tile([128, D], name="hd") for _ in range(2)])
        a1 = k.tile([128, D], name="a1")
        a2 = k.tile([128, D], name="a2")
        for mt in range(MT):
            dec = decp.next()
            hf = hfp.next()
            hb = hbp.next()
            hs = hsp.next()
            hd = hdp.next()
            self.act(dec[:], dl[:], AF.Exp, r=[dl, negt], w=[dec], scale=negt[:, mt:mt + 1])
            for nb in range(8):
                p = ps.next()
                self.mm(p[:], hT[2][0:64, mt * 128:(mt + 1) * 128], w4[0:64, nb * 512:(nb + 1) * 512], True, True,
                        r=[hT[2], w4], w=[p])
                dst = hf if nb < 4 else hb
                sl = slice((nb % 4) * 512, (nb % 4 + 1) * 512)
                self.tt("dve", dst[:, sl], p[:], dec[:, sl], ALU.mult, r=[p, dec], w=[dst])
            if mt == 0:
                self.memset("pool", hb[0:1, :], 0.0, w=[hb])
            self.tt("pool", hs[:], hf[:], hb[:], ALU.add, r=[hf, hb], w=[hs])
            self.tt("pool", hd[:], hf[:], hb[:], ALU.subtract, r=[hf, hb], w=[hd])
            r0 = row0 + mt * 128
            k.dma("pool", self.HS[r0:r0 + 128, :], hs[:], r=[hs], w=[self.HS])
            k.dma("pool", self.HD[r0:r0 + 128, :], hd[:], r=[hd], w=[self.HD])
            self.act(a1[:], hf[:], AF.Abs, r=[hf], w=[a1])
            self.act(a2[:], hb[:], AF.Abs, r=[hb], w=[a2])
            self.tt("dve", a1[:], a1[:], a2[:], ALU.add, r=[a1, a2], w=[a1])
            self.tt("dve", absum[:], absum[:], a1[:], ALU.add, r=[absum, a1], w=[absum])
        pn = ps.next()
        for dc in range(16):
            self.mm(pn[:, dc:dc + 1], absum[:, dc * 128:(dc + 1) * 128], ones[:, 0:1], True, True, r=[absum, ones], w=[pn])
        rn = k.tile([128, 16], name="rn")
        self.recip(rn[:], pn[:, 0:16], r=[pn], w=[rn])
        k.dma("pool", self.RN[rn_row0:rn_row0 + 128, :], rn[:], r=[rn], w=[self.RN])
        k.end()

    def ph_dft(self, A, a_row0, Ls, tname, OUT, out_row0, scale_wn):
        k = self.k
        k.begin()
        ps = self.psums(8)
        TC = Ls // 128
        FC = TC + 1
        wn = None
        if scale_wn:
            wn = k.tile([128, FC], name="wn")
            self.load("sp", wn[:], self.wn[Ls].ap(), r=[self.wn[Ls]], w=[wn])
        At = k.tile([128, TC, 1024], F32R, name="At")
        tbp = Rot([k.tile([128, TC, 128], F32R, name="tab") for _ in range(3)])
        otp = Rot([k.tile([128, 1024], name="ot") for _ in range(3)])
        Av = A.ap()[a_row0:a_row0 + Ls, :].rearrange("(tc p) d -> p tc d", p=128)
        qn = 0
        for db in range(2):
            self.load("act", At[:], Av[:, :, db * 1024:(db + 1) * 1024].bitcast(F32R), r=[A], w=[At])
            for fc in range(FC):
                M = 128 if fc < TC else 1
                tab = tbp.next()
                qn += 1
                if Ls == L:
                    tap, tres = self.wsl(tname + "_l", fc)
                else:
                    tc_t = self.cf_c if tname == "cf" else self.sf_c
                    tap, tres = tc_t.ap()[fc * 128:(fc + 1) * 128, :], []
                self.load("sp" if qn % 2 else "act", tab[:],
                          tap.rearrange("p (tc j) -> p tc j", j=128).bitcast(F32R), r=tres, w=[tab])
                ot = otp.next()
                for half in range(2):
                    p = ps.next()
                    for tc in range(TC):
                        self.mm(p[0:M, :], tab[:, tc, 0:M], At[:, tc, half * 512:(half + 1) * 512], tc == 0, tc == TC - 1,
                                r=[tab, At], w=[p])
                    if wn is not None:
                        self.act(ot[0:M, half * 512:(half + 1) * 512], p[0:M, :], AF.Copy, r=[p, wn], w=[ot],
                                 scale=wn[0:M, fc:fc + 1])
                    else:
                        self.evac(ot[0:M, half * 512:(half + 1) * 512], p[0:M, :], r=[p], w=[ot])
                r0 = out_row0 + fc * 128
                k.dma("pool", OUT[r0:r0 + M, db * 1024:(db + 1) * 1024], ot[0:M, :], r=[ot], w=[OUT])
        k.end()

    def ph_hy_proj(self, j, with_ctx):
        k = self.k
        k.begin()
        ps = self.psums(8)
        ident = k.tile([128, 128], name="ident")
        self.load("sp", ident[:], self.ident.ap(), r=[self.ident], w=[ident])
        cols = k.tile([128, 48, 5], name="cols")
        self.load("sp", cols[:], self.hy_cols[j].rearrange("p (n c) -> p n c", c=5), r=[self.hy_cols], w=[cols])
        hT = k.tile([128, 16, 512], F32R, name="hT")
        wp = Rot([k.tile([128, 16, 256], F32R, name="w") for _ in range(5)])
        utok = k.tile([128, 4, D], name="utok")
        pp = Rot([k.tile([128, 512], name="p") for _ in range(3)])
        yp = Rot([k.tile([128, 512], name="y") for _ in range(6)])
        uu = Rot([k.tile([128, 512], name="uu") for _ in range(2)])
        wap, wres = self.wsl("hy_w_in", j)
        wv = wap.rearrange("(kc p) n -> p kc n", p=128)
        HTv = self.HT.ap().rearrange("(kc p) t -> p kc t", p=128)
        qn = 0
        for (t0, TB) in self.blocks(with_ctx):
            ntl = TB // 128
            grid = t0 < L
            self.load("act", hT[:, :, 0:TB], HTv[:, :, t0:t0 + TB].bitcast(F32R), r=[self.HT], w=[hT])
            for jb in range(8):
                wts = []
                for sec in range(3):
                    wt = wp.next()
                    qn += 1
                    c0 = sec * D + jb * 256
                    self.load("sp" if qn % 2 else "act", wt[:], wv[:, :, c0:c0 + 256].bitcast(F32R),
                              r=wres, w=[wt])
                    wts.append(wt)
                for jl in range(2):
                    jc = jb * 2 + jl
                    ys = []
                    for sec in range(3):
                        nc_ = sec * 16 + jc
                        p = ps.next()
                        for kc in range(16):
                            self.mm(p[:, 0:TB], wts[sec][:, kc, jl * 128:(jl + 1) * 128], hT[:, kc, 0:TB], kc == 0,
                                    kc == 15, r=[wts[sec], hT], w=[p])
                        pt = pp.next()
                        y = yp.next()
                        self.act(pt[:, 0:TB], p[:, 0:TB], AF.Identity, r=[p, cols], w=[pt], bias=cols[:, nc_, 0:1])
                        self.act(y[:, 0:TB], pt[:, 0:TB], AF.Identity, r=[pt, cols], w=[y], scale=cols[:, nc_, 2:3],
                                 bias=cols[:, nc_, 4:5])
                        if grid:
                            pv = pt[:, 0:TB].rearrange("p (r w) -> p r w", w=64)
                            yv = y[:, 0:TB].rearrange("p (r w) -> p r w", w=64)
                            self.stt("dve", yv[:, :, 1:64], pv[:, :, 0:63], cols[:, nc_, 1:2], yv[:, :, 1:64], ALU.mult,
                                     ALU.add, r=[pt, y, cols], w=[y])
                            self.stt("dve", yv[:, :, 0:63], pv[:, :, 1:64], cols[:, nc_, 3:4], yv[:, :, 0:63], ALU.mult,
                                     ALU.add, r=[pt, y, cols], w=[y])
                        else:
                            self.stt("dve", y[:, 1:TB], pt[:, 0:TB - 1], cols[:, nc_, 1:2], y[:, 1:TB], ALU.mult,
                                     ALU.add, r=[pt, y, cols], w=[y])
                            self.stt("dve", y[:, 0:TB - 1], pt[:, 1:TB], cols[:, nc_, 3:4], y[:, 0:TB - 1], ALU.mult,
                                     ALU.add, r=[pt, y, cols], w=[y])
                        ys.append(y)
                    k.dma("pool", self.X0T[jc * 128:(jc + 1) * 128, t0:t0 + TB], ys[0][:, 0:TB], r=[ys[0]], w=[self.X0T])
                    u = uu.next()
                    self.tt("pool", u[:, 0:TB], ys[1][:, 0:TB], ys[2][:, 0:TB], ALU.mult, r=[ys[1], ys[2]], w=[u])
                    k.dma("pool", self.UT[jc * 128:(jc + 1) * 128, t0:t0 + TB], u[:, 0:TB], r=[u], w=[self.UT])
                    p = ps.next()
                    for tl in range(ntl):
                        self.tr(p[:, tl * 128:(tl + 1) * 128], u[:, tl * 128:(tl + 1) * 128], ident[:], r=[u, ident], w=[p])
                    self.evac(utok[:, 0:ntl, jc * 128:(jc + 1) * 128],
                              p[:, 0:TB].rearrange("p (a b) -> p a b", b=128), r=[p], w=[utok])
            k.dma("pool", self.UTOK.ap()[t0:t0 + TB, :].rearrange("(a p) d -> p a d", p=128), utok[:, 0:ntl, :],
                  r=[utok], w=[self.UTOK])
        k.end()

    def ph_hy_inv(self, j, Ls, tok0, frow0, rn_row0):
        k = self.k
        k.begin()
        ps = self.psums(8)
        TC = Ls // 128
        FC = TC + 1
        GS = min(4, TC)
        NG = TC // GS
        TBW = min(512, Ls)
        NTB = Ls // TBW
        rn = k.tile([128, 16], name="rn")
        skc = k.tile([128, 16], name="skc")
        self.load("sp", rn[:], self.RN[rn_row0:rn_row0 + 128, :], r=[self.RN], w=[rn])
        self.load("sp", skc[:], self.hy_skipc[j], r=[self.hy_skipc], w=[skc])
        YR = k.tile([128, FC, 512], F32R, name="YR")
        YI = k.tile([128, FC, 512], F32R, name="YI")
        ldp = Rot([k.tile([128, 4, 512], name="ld") for _ in range(2)])
        tmp = Rot([k.tile([128, 4, 512], name="tm") for _ in range(2)])
        cip = Rot([k.tile([128, GS, TBW], F32R, name="ci") for _ in range(2)])
        nsp = Rot([k.tile([128, GS, TBW], F32R, name="nsi") for _ in range(2)])
        cnp = Rot([k.tile([1, TBW], F32R, name="cin") for _ in range(2)])
        e1 = Rot([k.tile([128, TBW], name="e1") for _ in range(2)])
        e2 = Rot([k.tile([128, TBW], name="e2") for _ in range(2)])
        e3 = Rot([k.tile([128, TBW], name="e3") for _ in range(2)])
        e4 = Rot([k.tile([128, TBW], name="e4") for _ in range(2)])
        for db in range(4):
            dsl = slice(db * 512, (db + 1) * 512)
            for fc in range(FC):
                M = 128 if fc < TC else 1
                r0 = frow0 + fc * 128
                ld = ldp.next()
                tm = tmp.next()
                for n_, src in enumerate((self.URE, self.UIM, self.KRE, self.KIM)):
                    self.load("sp" if n_ % 2 else "act", ld[0:M, n_, :], src[r0:r0 + M, dsl], r=[src], w=[ld])
                ur, ui, kr, ki = (ld[0:M, n_, :] for n_ in range(4))
                self.tt("dve", tm[0:M, 0, :], ur, kr, ALU.mult, r=[ld], w=[tm])
                self.tt("pool", tm[0:M, 1, :], ui, ki, ALU.mult, r=[ld], w=[tm])
                self.tt("pool", tm[0:M, 2, :], ur, ki, ALU.mult, r=[ld], w=[tm])
                self.tt("dve", tm[0:M, 3, :], ui, kr, ALU.mult, r=[ld], w=[tm])
                self.tt("dve", YR[0:M, fc, :], tm[0:M, 0, :], tm[0:M, 1, :], ALU.subtract, r=[tm], w=[YR])
                self.tt("pool", YI[0:M, fc, :], tm[0:M, 2, :], tm[0:M, 3, :], ALU.add, r=[tm], w=[YI])
            for tb in range(NTB):
                pd = [ps.next() for _ in range(4)]
                for g in range(NG):
                    ci = cip.next()
                    nsi = nsp.next()
                    if Ls == L:
                        ciap, cires = self.wsl("ci_l", tb * 4 + g)
                        nsap, nsres = self.wsl("nsi_l", tb * 4 + g)
                    else:
                        ciap, cires = self.ci_c.ap(), []
                        nsap, nsres = self.nsi_c.ap(), []
                    self.load("sp", ci[:], ciap.rearrange("p (f t) -> p f t", t=TBW).bitcast(F32R), r=cires, w=[ci])
                    self.load("act", nsi[:], nsap.rearrange("p (f t) -> p f t", t=TBW).bitcast(F32R), r=nsres, w=[nsi])
                    for dc in range(4):
                        for fl in range(GS):
                            fc = g * GS + fl
                            self.mm(pd[dc][:, 0:TBW], YR[:, fc, dc * 128:(dc + 1) * 128], ci[:, fl, :],
                                    g == 0 and fl == 0, False, r=[YR, ci], w=[pd[dc]])
                            self.mm(pd[dc][:, 0:TBW], YI[:, fc, dc * 128:(dc + 1) * 128], nsi[:, fl, :], False, False,
                                    r=[YI, nsi], w=[pd[dc]])
                cn = cnp.next()
                self.load("sp", cn[:], self.cin[Ls][0:1, tb * TBW:(tb + 1) * TBW].bitcast(F32R), r=[self.cin[Ls]], w=[cn])
                for dc in range(4):
                    self.mm(pd[dc][:, 0:TBW], YR[0:1, TC, dc * 128:(dc + 1) * 128], cn[0:1, :], False, True,
                            r=[YR, cn], w=[pd[dc]])
                    dch = db * 4 + dc
                    c0 = tok0 + tb * TBW
                    x0t = e1.next()
                    ut = e2.next()
                    t1 = e3.next()
                    t2 = e4.next()
                    self.load("sp", x0t[:], self.X0T[dch * 128:(dch + 1) * 128, c0:c0 + TBW], r=[self.X0T], w=[x0t])
                    self.load("act", ut[:], self.UT[dch * 128:(dch + 1) * 128, c0:c0 + TBW], r=[self.UT], w=[ut])
                    self.act(t1[:], pd[dc][:, 0:TBW], AF.Copy, r=[pd[dc], rn], w=[t1], scale=rn[:, dch:dch + 1])
                    self.stt("dve", t2[:], ut[:], skc[:, dch:dch + 1], t1[:], ALU.mult, ALU.add, r=[ut, skc, t1], w=[t2])
                    self.tt("pool", t2[:], t2[:], x0t[:], ALU.mult, r=[t2, x0t], w=[t2])
                    k.dma("pool", self.YHT[dch * 128:(dch + 1) * 128, c0:c0 + TBW], t2[:], r=[t2], w=[self.YHT])
        k.end()

    def ph_final(self):
        k = self.k
        k.begin()
        gam = k.tile([128, D], name="gam")
        self.bcload(gam, self.norm_final[0:1, :], r=[self.norm_final])
        xp = Rot([k.tile([128, D], name="xt") for _ in range(3)])
        op_ = Rot([k.tile([128, D], name="ot") for _ in range(3)])
        junk = k.tile([128, D], name="junk")
        ssp = Rot([k.tile([128, 2], name="ss") for _ in range(3)])
        for tt_ in range(16):
            r0 = tt_ * 128
            xt = xp.next()
            ot = op_.next()
            ss = ssp.next()
            self.load("sp", xt[:], self.X[r0:r0 + 128, :], r=self.X.rows(r0, r0 + 128), w=[xt])
            self.memset("pool", ss[:], 0.0, w=[ss])
            self.act(junk[:], xt[:], AF.Square, r=[xt], w=[junk, ss], accum=ss[:, 0:1])
            self.rstd(ss[:, 0:1], ss[:, 1:2], D, r=[ss])
            self.stt("dve", ot[:], xt[:], ss[:, 1:2], gam[:], ALU.mult, ALU.mult, r=[xt, ss, gam], w=[ot])
            k.dma("act", self.out[r0:r0 + 128, :], ot[:], r=[ot], w=self.out.rows(r0, r0 + 128))
        k.end()

    def ph_dump(self, src, r0, r1):
        k = self.k
        k.begin()
        k.dma("sp", self.out.ap(), src[r0:r1, :], r=[src], w=[self.out])
        k.end()

    def layer_mixer(self, i):
        need_ctx = i < DEPTH - 1
        j = i // 2
        if i % 2 == 0:
            self.ph_norm(i, "mix", True)
            self.ph_gla_proj(j)
            self.ph_gla_scan(j)
            self.ph_outproj(i, "gla_w_out", j, self.OG, "tok", need_ctx)
        else:
            self.ph_norm(i, "mix", need_ctx)
            streams = [(L, 0, 0, 0)] + ([(CL, L, 17 * 128, 128)] if need_ctx else [])
            for (Ls, tok0, frow0, rn0) in streams:
                self.ph_hy_filter(j, Ls, tok0, rn0)
                self.ph_dft(self.HS, tok0, Ls, "cf", self.KRE, frow0, True)
                self.ph_dft(self.HD, tok0, Ls, "sf", self.KIM, frow0, True)
            self.ph_hy_proj(j, need_ctx)
            for (Ls, tok0, frow0, rn0) in streams:
                self.ph_dft(self.UTOK, tok0, Ls, "cf", self.URE, frow0, False)
                self.ph_dft(self.UTOK, tok0, Ls, "sf", self.UIM, frow0, False)
                self.ph_hy_inv(j, Ls, tok0, frow0, rn0)
            self.ph_outproj(i, "hy_w_out", j, self.YHT, "feat", need_ctx, bias_row=self.hy_b_out[j:j + 1, :])

    def layer_ffn(self, i):
        need_ctx = i < DEPTH - 1
        self.ph_norm(i, "ffn", need_ctx)
        self.ph_experts(i, need_ctx)

    def build(self, stages=None):
        self.ph_init()
        if not self.direct:
            self.ph_gather()
        for i in range(DEPTH):
            if stages is not None and ("mix", i) not in stages and ("ffn", i) not in stages:
                continue
            self.ph_mods(i)
            if stages is None or ("mix", i) in stages:
                self.layer_mixer(i)
            if stages is None or ("ffn", i) in stages:
                self.layer_ffn(i)
        if self.debug_dump is None:
            self.ph_final()
        else:
            src = getattr(self, self.debug_dump[0])
            self.ph_dump(src, self.debug_dump[2], self.debug_dump[2] + self.debug_dump[1][0])
        self.k.finish()
        return self.nc


_CONST = None


def constants():
    global _CONST
    if _CONST is not None:
        return _CONST
    f32 = np.float32
    c = {}
    c["ident"] = np.eye(128, dtype=f32)
    s = np.arange(128)
    tri_f = (s[:, None] <= s[None, :]).astype(f32)
    tri_b = (s[:, None] >= s[None, :]).astype(f32)
    c["tri"] = np.stack([tri_f, tri_b]) * f32(-1.0 / 16.0)
    c["msk"] = np.stack([tri_f, tri_b])
    for Ls, nm in ((L, "l"), (CL, "c")):
        t = np.linspace(0.0, 1.0, Ls, dtype=f32)[:, None]
        w = (2 * np.pi * np.arange(Ls, dtype=f32)[:, None] / Ls).astype(f32)
        f = np.linspace(1e-4, 15, 16, dtype=f32)[None, :]
        fw = (f * w).astype(f32)
        z = np.concatenate([t, np.cos(fw), -np.sin(fw)], axis=-1).astype(f32)
        c["zT_" + nm] = np.ascontiguousarray(z.T)
        tt_ = np.linspace(0.0, 1.0, Ls, dtype=f32)
        c["negt_" + nm] = np.ascontiguousarray((-tt_).reshape(Ls // 128, 128).T)
        N = 2 * Ls
        TC = Ls // 128
        FCn = TC + 1
        tq = np.arange(Ls, dtype=np.float64)
        fq = np.arange(FCn * 128, dtype=np.float64)
        ang = 2 * np.pi * np.outer(tq, fq) / N
        valid = (fq <= Ls)[None, :]
        C = np.where(valid, np.cos(ang), 0.0)
        Sn = np.where(valid, -np.sin(ang), 0.0)
        def fwd(T):
            T4 = T.reshape(TC, 128, FCn, 128)
            return np.ascontiguousarray(T4.transpose(2, 1, 0, 3).reshape(FCn, 128, TC * 128)).astype(f32)
        c["cf_" + nm] = fwd(C)
        c["sf_" + nm] = fwd(Sn)
        TBW = min(512, Ls)
        NTB = Ls // TBW
        GS = min(4, TC)
        NG = TC // GS
        def inv(T):
            T5 = T[:, :TC * 128].reshape(NTB, TBW, NG, GS, 128)
            return np.ascontiguousarray(T5.transpose(0, 2, 4, 3, 1).reshape(NTB, NG, 128, GS * TBW)).astype(f32)
        c["ci_" + nm] = inv(C)
        c["nsi_" + nm] = inv(Sn)
        c["cin_" + nm] = np.cos(np.pi * tq)[None, :].astype(f32)
        wf = np.full(FCn * 128, 2.0 / N)
        wf[0] = 1.0 / N
        wf[Ls] = 1.0 / N
        wf[Ls + 1:] = 0.0
        c["wn_" + nm] = np.ascontiguousarray(wf.reshape(FCn, 128).T).astype(f32)
    max_decay = math.log(1e-2) / 0.3
    min_decay = math.log(1e-2) / 1.5
    c["padf"] = np.ascontiguousarray(np.broadcast_to((NT + np.arange(128)).astype(f32)[None, :], (16, 128)))
    c["delta"] = np.abs(np.linspace(min_decay, max_decay, D, dtype=f32))[None, :].astype(f32)
    _CONST = c
    return c


def host_inputs(inp, ncores=NCORES, units=None, direct=None):
    f32 = np.float32
    g = lambda n: np.ascontiguousarray(np.asarray(inp[n], dtype=f32))
    direct = (ncores == 1) if direct is None else direct
    units = units if units is not None else all_units()
    dcache = {}
    shared = {}
    for n in ("b_ada", "norm_mix", "norm_ffn", "hy_f_w1", "hy_f_w2", "hy_f_w3", "hy_f_w4", "hy_b_out", "moe_router"):
        shared[n] = g(n)
    shared["norm_final"] = g("norm_final").reshape(1, D)
    shared["gla_hn"] = g("gla_head_norm")
    a1 = g("gla_w_a1")
    shared["gla_wa1"] = np.ascontiguousarray(a1.transpose(0, 2, 1, 3).reshape(2, D, 32))
    a2 = g("gla_w_a2")
    ba = g("gla_b_a")
    wa2 = np.zeros((2, 33, 2048), f32)
    for z in range(2):
        wa2[:, z * 16:(z + 1) * 16, z * 1024:(z + 1) * 1024] = a2[:, z]
        wa2[:, 32, z * 1024:(z + 1) * 1024] = ba[:, z]
    shared["gla_wa2"] = wa2
    cw = g("hy_conv_w")
    cols = np.stack([g("hy_b_in"), cw[:, 0], cw[:, 1], cw[:, 2], g("hy_conv_b")], axis=-1)
    shared["hy_cols"] = np.ascontiguousarray(cols.reshape(2, 48, 128, 5).transpose(0, 2, 1, 3).reshape(2, 128, 240))
    shared["hy_fb"] = np.ascontiguousarray(np.stack([g("hy_f_b1"), g("hy_f_b2"), g("hy_f_b3")], axis=-1))
    shared["hy_skipc"] = np.ascontiguousarray(g("hy_skip").reshape(2, 16, 128).transpose(0, 2, 1))
    cst = constants()
    big = {}
    for n in SHARDED:
        C, rpu, nu, CH = SHARDED[n]
        src = cst[n] if n in cst else np.asarray(inp[n], dtype=f32)
        big[n] = src.reshape(nu * rpu, C)
    for n, v in cst.items():
        if n in SHARDED:
            continue
        if n in ("cf_c", "sf_c"):
            v = v.reshape(3 * 128, 2 * 128)
        if n in ("ci_c", "nsi_c"):
            v = v.reshape(128, 2 * 256)
        shared[n] = v
    x = g("x")
    ctx = g("ctx")
    c = g("c")
    cctx = g("c_ctx")
    maps = []
    for b in range(ncores):
        m = dict(shared)
        m["x"] = x[b]
        m["ctx"] = ctx[b]
        cc = np.stack([c[b], cctx], axis=-1)
        m["cc"] = np.ascontiguousarray(cc.reshape(16, 128, 2).transpose(1, 0, 2).reshape(128, 32))
        for n in SHARDED:
            C, rpu, nu, CH = SHARDED[n]
            if direct:
                if n not in dcache:
                    us = units[n] if len(units[n]) else [0]
                    dcache[n] = big[n] if len(us) == nu else np.ascontiguousarray(
                        big[n].reshape(nu, rpu, C)[us].reshape(-1, C))
                m[n] = dcache[n]
            else:
                R = nu * rpu
                m[n] = np.ascontiguousarray(big[n].reshape(R // CH, NCORES, CH // NCORES, C)[:, b].reshape(-1, C))
        maps.append(m)
    return maps


_PROG = {}


REPLICATE = True


def get_prog():
    if "p" not in _PROG:
        p = Prog(direct=REPLICATE)
        p.build()
        _PROG["p"] = p
    return _PROG["p"]


def kernel(**inputs):
    maps = host_inputs(inputs, direct=REPLICATE)
    prog = get_prog()
    res = run_bass_kernel_spmd(prog.nc, maps, core_ids=list(range(NCORES)))
    out = np.stack([np.asarray(r["out"]) for r in res.results], axis=0)
    return out.astype(np.float32)
```

```python
import math
import numpy as np
from contextlib import ExitStack
import concourse.bass as bass
import concourse.mybir as mybir
from concourse.bass_utils import run_bass_kernel_spmd

F32 = mybir.dt.float32
F32R = mybir.dt.float32r
I32 = mybir.dt.int32
U32 = mybir.dt.uint32
AF = mybir.ActivationFunctionType
ALU = mybir.AluOpType
AX = mybir.AxisListType

D = 2048
L = 2048
CL = 256
NT = L + CL
DEPTH = 4
EPS = 1e-6
NE = 16
FF = 1024
HEADS = 4
DK = 256
DV = 512
NCORES = 8

SEM_LIMIT = 16000
NDMA = 8


class Res:
    __slots__ = ("w", "r")

    def __init__(self):
        self.w = {}
        self.r = {}


class Tile:
    def __init__(self, h, nblk=1, bs=None, is_dram=False, persist=False):
        self.h = h
        self.res = [Res() for _ in range(nblk)]
        self.bs = bs
        self.is_dram = is_dram
        self.persist = persist

    def __getitem__(self, key):
        if self.is_dram:
            return self.h.ap()[key]
        return self.h[key]

    def ap(self):
        return self.h.ap() if self.is_dram else self.h[:]

    def rows(self, r0, r1):
        if self.bs is None:
            return list(self.res)
        return self.res[r0 // self.bs:(r1 - 1) // self.bs + 1]


def _flat(lst):
    out = []
    for x in lst:
        if isinstance(x, Res):
            out.append(x)
        elif isinstance(x, Tile):
            out.extend(x.res)
        else:
            out.extend(_flat(x))
    return out


class K:
    COMPUTE = ("pe", "act", "dve", "pool")
    ALL = ("pe", "act", "dve", "pool", "sp")

    def __init__(self, nc):
        self.nc = nc
        self.ges = ExitStack()
        self.sems = {}
        self.cs = {}
        self.nsem = 0
        for e in self.COMPUTE:
            self._new_csem(e)
        self.ds = {q: [[self._new_sem("d_%s%d" % (q, i)), 0] for i in range(NDMA)]
                   for q in ("sp", "pool", "act")}
        self.dn = {q: 0 for q in self.ds}
        self.ops = {e: [] for e in self.ALL}
        self.waited = {e: {} for e in self.ALL}
        self.tiles = []
        self.pes = None
        self.ntile = 0
        self.ninst = 0
        self.ccsem = None

    def _new_sem(self, name):
        name = "%s_%d" % (name, self.nsem)
        self.nsem += 1
        self.sems[name] = self.ges.enter_context(self.nc.semaphore(name))
        return name

    def _new_csem(self, e):
        self.cs[e] = [self._new_sem("c_" + e), 0]

    def dram(self, name, shape, dtype=F32, kind="Internal", bs=None, persist=False):
        h = self.nc.dram_tensor(name, list(shape), dtype, kind=kind)
        nblk = 1 if bs is None else (shape[0] + bs - 1) // bs
        t = Tile(h, nblk=nblk, bs=bs, is_dram=True, persist=persist)
        self.tiles.append(t)
        return t

    def tile(self, shape, dtype=F32, name=None):
        self.ntile += 1
        name = "%s_%d" % (name or "t", self.ntile)
        h = self.pes.enter_context(self.nc.sbuf_tensor(name, list(shape), dtype))
        t = Tile(h)
        self.tiles.append(t)
        return t

    def psum(self, shape=(128, 512), dtype=F32, name=None):
        self.ntile += 1
        name = "%s_%d" % (name or "ps", self.ntile)
        h = self.pes.enter_context(self.nc.psum_tensor(name, list(shape), dtype))
        t = Tile(h)
        self.tiles.append(t)
        return t

    def _deps(self, eng, r, w):
        deps = {}
        for x in _flat(r):
            for s, v in x.w.items():
                if deps.get(s, 0) < v:
                    deps[s] = v
        for x in _flat(w):
            for d in (x.w, x.r):
                for s, v in d.items():
                    if deps.get(s, 0) < v:
                        deps[s] = v
        waits = []
        wd = self.waited[eng]
        for s, v in deps.items():
            if eng == "pe" and s.startswith("c_pe"):
                continue
            if wd.get(s, 0) < v:
                waits.append((s, v))
                wd[s] = v
        return waits

    def _mark(self, r, w, s, v):
        for x in _flat(r):
            if x.r.get(s, 0) < v:
                x.r[s] = v
        for x in _flat(w):
            x.w = {s: v}
            x.r = {}

    def op(self, eng, fn, r=(), w=()):
        waits = self._deps(eng, r, w)
        c = self.cs[eng]
        if c[1] >= SEM_LIMIT:
            self._new_csem(eng)
            c = self.cs[eng]
        c[1] += 1
        self.ops[eng].append((waits, fn, c[0], 1))
        self._mark(r, w, c[0], c[1])
        self.ninst += 1 + len(waits)

    def dma(self, q, out, in_, r=(), w=(), fn=None, **kw):
        waits = self._deps(q, r, w)
        j = self.dn[q] % NDMA
        self.dn[q] += 1
        slot = self.ds[q][j]
        if slot[1] + 16 > SEM_LIMIT:
            slot[0] = self._new_sem("d_%s%d" % (q, j))
            slot[1] = 0
        if slot[1] > 0 and self.waited[q].get(slot[0], 0) < slot[1]:
            waits.append((slot[0], slot[1]))
            self.waited[q][slot[0]] = slot[1]
        slot[1] += 16
        if fn is None:
            def fn(e, out=out, in_=in_, kw=kw):
                return e.dma_start(out=out, in_=in_, **kw)
        self.ops[q].append((waits, fn, slot[0], 16))
        self._mark(r, w, slot[0], slot[1])
        self.ninst += 1 + len(waits)

    def coll(self, fn, r=(), w=()):
        if self.ccsem is None:
            self.ccsem = [self._new_sem("cc"), 0]
        waits = self._deps("pool", r, w)
        self.ccsem[1] += 1
        self.ops["pool"].append((waits, fn, self.ccsem[0], 1))
        self._mark(r, w, self.ccsem[0], self.ccsem[1])

    def barrier(self):
        cur = {}
        for e in self.COMPUTE:
            n, c = self.cs[e]
            if c > 0:
                cur[n] = c
        for q in self.ds:
            for n, v in self.ds[q]:
                if v > 0:
                    cur[n] = v
        for e in self.ALL:
            wd = self.waited[e]
            waits = []
            for s, v in cur.items():
                if wd.get(s, 0) < v:
                    waits.append((s, v))
                    wd[s] = v
            if waits:
                self.ops[e].append((waits, None, None, 0))
        for t in self.tiles:
            if t.persist:
                continue
            for x in t.res:
                x.w = {}
                x.r = {}

    def begin(self):
        self.pes = ExitStack()

    def end(self):
        self.barrier()
        nc = self.nc
        ops = self.ops
        sems = self.sems

        def replay(name, e):
            for waits, fn, s, inc in ops[name]:
                for ws, wv in waits:
                    e.wait_ge(sems[ws], wv)
                if fn is not None:
                    ins = fn(e)
                    ins.then_inc(sems[s], inc)

        with nc.Block() as blk:
            @blk.sync
            def _(e):
                replay("sp", e)

            @blk.scalar
            def _(e):
                replay("act", e)

            @blk.vector
            def _(e):
                replay("dve", e)

            @blk.gpsimd
            def _(e):
                replay("pool", e)

            @blk.tensor
            def _(e):
                replay("pe", e)
        self.ops = {e: [] for e in self.ALL}
        self.pes.close()
        self.pes = None
        self.tiles = [t for t in self.tiles if t.is_dram]

    def finish(self):
        self.ges.close()


class Rot:
    def __init__(self, tiles):
        self.t = tiles
        self.i = 0

    def next(self):
        t = self.t[self.i % len(self.t)]
        self.i += 1
        return t


SHARDED = {
    "w_ada": (6 * D, D, 4, 1024),
    "gla_w_in": (6144, D, 2, 2048),
    "gla_w_out": (D, D, 2, 2048),
    "hy_w_in": (3 * D, D, 2, 2048),
    "hy_w_out": (D, D, 2, 2048),
    "moe_w1": (FF, D, 64, 8192),
    "moe_w3": (FF, D, 64, 8192),
    "moe_w2": (D, FF, 64, 4096),
    "cf_l": (16 * 128, 128, 17, 2176),
    "sf_l": (16 * 128, 128, 17, 2176),
    "ci_l": (4 * 512, 128, 16, 2048),
    "nsi_l": (4 * 512, 128, 16, 2048),
}


GROUP = {"w_ada": 1, "moe_w1": 16, "moe_w3": 16, "moe_w2": 16}


def all_units():
    return {n: list(range(v[2])) for n, v in SHARDED.items()}


class Prog:
    def __init__(self, ncores=NCORES, units=None, debug_dump=None, direct=None):
        self.ncores = ncores
        self.direct = (ncores == 1) if direct is None else direct
        self.units = units if units is not None else all_units()
        self.debug_dump = debug_dump
        nc = bass.Bass("TRN2", target_bir_lowering=False)
        nc.dge_precook = False
        self.nc = nc
        self.k = K(nc)
        self.evn = 0
        self.decl()

    def decl(self):
        k = self.k
        I = lambda name, shape, dt=F32: k.dram(name, shape, dt, kind="ExternalInput")
        S = lambda name, shape, dt=F32, bs=None: k.dram(name, shape, dt, kind="Internal", bs=bs)
        self.x_in = I("x", [L, D])
        self.ctx_in = I("ctx", [CL, D])
        self.cc = I("cc", [128, 32])
        self.b_ada = I("b_ada", [DEPTH, 6 * D])
        self.norm_mix = I("norm_mix", [DEPTH, D])
        self.norm_ffn = I("norm_ffn", [DEPTH, D])
        self.norm_final = I("norm_final", [1, D])
        self.gla_wa1 = I("gla_wa1", [2, D, 32])
        self.gla_wa2 = I("gla_wa2", [2, 33, 2048])
        self.gla_hn = I("gla_hn", [2, DV])
        self.hy_cols = I("hy_cols", [2, 128, 48 * 5])
        self.hy_f_w1 = I("hy_f_w1", [2, 33, 64])
        self.hy_fb = I("hy_fb", [2, 64, 3])
        self.hy_f_w2 = I("hy_f_w2", [2, 64, 64])
        self.hy_f_w3 = I("hy_f_w3", [2, 64, 64])
        self.hy_f_w4 = I("hy_f_w4", [2, 64, 2 * D])
        self.hy_skipc = I("hy_skipc", [2, 128, 16])
        self.hy_b_out = I("hy_b_out", [2, D])
        self.moe_router = I("moe_router", [DEPTH, D, NE])
        self.wts = {}
        self.wsh = {}
        self.wbn = {}
        for n, (C, rpu, nu, CH) in SHARDED.items():
            if self.direct:
                nuse = max(1, len(self.units[n]))
                self.wts[n] = k.dram(n, [nuse * rpu, C], F32, kind="ExternalInput")
            else:
                R = nu * rpu
                gu = GROUP.get(n, nu)
                self.wsh[n] = k.dram(n, [R // NCORES, C], F32, kind="ExternalInput")
                self.wbn[n] = k.dram(n + "_bn", [R // NCORES, C], F32, kind="Internal")
                self.wts[n] = [k.dram("%s_g%d" % (n, gi), [gu * rpu, C], F32, kind="Internal", bs=CH, persist=True)
                               for gi in range(nu // gu)]
        self.ident = I("ident", [128, 128])
        self.padf = I("padf", [16, 128])
        self.tri = I("tri", [2, 128, 128])
        self.msk = I("msk", [2, 128, 128])
        self.zT = {L: I("zT_l", [33, L]), CL: I("zT_c", [33, CL])}
        self.negt = {L: I("negt_l", [128, L // 128]), CL: I("negt_c", [128, CL // 128])}
        self.delta = I("delta", [1, D])
        self.cf_c = I("cf_c", [3 * 128, 2 * 128])
        self.sf_c = I("sf_c", [3 * 128, 2 * 128])
        self.ci_c = I("ci_c", [128, 2 * 256])
        self.nsi_c = I("nsi_c", [128, 2 * 256])
        self.cin = {L: I("cin_l", [1, L]), CL: I("cin_c", [1, CL])}
        self.wn = {L: I("wn_l", [128, 17]), CL: I("wn_c", [128, 3])}
        if self.debug_dump is None:
            self.out = k.dram("out", [L, D], F32, kind="ExternalOutput", bs=128)
        else:
            self.out = k.dram("out", list(self.debug_dump[1]), F32, kind="ExternalOutput")
        self.X = S("X", [NT + 128, D], bs=128)
        self.MOD = S("MOD", [DEPTH * 2, 6 * D])
        self.HT = S("HT", [D, NT])
        self.H = S("H", [NT + 128, D])
        self.QT = S("QT", [1024, NT])
        self.KT = S("KT", [1024, NT])
        self.V = S("V", [NT, D])
        self.G = S("G", [NT, D])
        self.U = S("U", [NT, D])
        self.OG = S("OG", [NT, D])
        self.X0T = S("X0T", [D, NT])
        self.UT = S("UT", [D, NT])
        self.UTOK = S("UTOK", [NT, D])
        self.HS = S("HS", [NT, D])
        self.HD = S("HD", [NT, D])
        FR = 17 * 128 + 3 * 128
        self.KRE = S("KRE", [FR, D])
        self.KIM = S("KIM", [FR, D])
        self.URE = S("URE", [FR, D])
        self.UIM = S("UIM", [FR, D])
        self.YHT = S("YHT", [D, NT])
        self.RN = S("RN", [2 * 128, 16])
        self.IDXD = S("IDXD", [3 * 128, NE], I32)
        self.TSD = S("TSD", [3 * 128, NE])

    def wsl(self, name, unit):
        C, rpu, nu, CH = SHARDED[name]
        if self.direct:
            r0 = self.units[name].index(unit) * rpu
            return self.wts[name].ap()[r0:r0 + rpu, :], []
        gu = GROUP.get(name, nu)
        t = self.wts[name][unit // gu]
        r0 = (unit % gu) * rpu
        return t.ap()[r0:r0 + rpu, :], t.rows(r0, r0 + rpu)

    def ph_gather(self):
        k = self.k
        k.begin()
        order = []
        moe = lambda l: [(n, 4 * l + c) for c in range(4) for n in ("moe_w1", "moe_w3", "moe_w2")]
        order += [("w_ada", 0), ("w_ada", 1), ("gla_w_in", 0), ("gla_w_out", 0)] + moe(0)
        order += [("w_ada", 2), ("w_ada", 3), ("hy_w_in", 0), ("cf_l", 0), ("sf_l", 0), ("ci_l", 0), ("nsi_l", 0),
                  ("hy_w_out", 0)] + moe(1)
        order += [("w_ada", 4), ("w_ada", 5), ("gla_w_in", 1), ("gla_w_out", 1)] + moe(2)
        order += [("w_ada", 6), ("w_ada", 7), ("hy_w_in", 1), ("hy_w_out", 1)] + moe(3)
        qn = 0
        for (n, c) in order:
            C, rpu, nu, CH = SHARDED[n]
            sh, bn = self.wsh[n], self.wbn[n]
            grows = GROUP.get(n, nu) * rpu
            gt = self.wts[n][(c * CH) // grows]
            g0 = (c * CH) % grows
            pr = CH // NCORES
            qn += 1
            bres = Res()
            k.dma("sp" if qn % 2 else "act", bn[c * pr:(c + 1) * pr, :], sh[c * pr:(c + 1) * pr, :], r=[], w=[bres])
            k.coll(lambda e, bn=bn, gt=gt, c=c, pr=pr, CH=CH, g0=g0: e.collective_compute(
                "AllGather", ALU.bypass, replica_groups=[list(range(NCORES))],
                ins=[bn.ap()[c * pr:(c + 1) * pr, :]], outs=[gt.ap()[g0:g0 + CH, :]]),
                r=[bres], w=gt.rows(g0, g0 + CH))
        k.end()

    def mm(self, ps, lhsT, rhs, start, stop, r, w):
        self.k.op("pe", lambda e: e.matmul(ps, lhsT, rhs, start=start, stop=stop), r=r, w=w)

    def tr(self, ps, in_, ident, r, w):
        self.k.op("pe", lambda e: e.transpose(ps, in_, ident), r=r, w=w)

    def act(self, out, in_, func, r, w, bias=None, scale=None, accum=None):
        kw = {}
        if bias is not None:
            kw["bias"] = bias
        if scale is not None:
            kw["scale"] = scale
        if accum is not None:
            kw["accum_out"] = accum
        self.k.op("act", lambda e: e.activation(out, in_, func, **kw), r=r, w=w)

    def cp(self, eng, out, in_, r, w):
        if eng == "act":
            self.k.op("act", lambda e: e.copy(out, in_), r=r, w=w)
        else:
            self.k.op(eng, lambda e: e.tensor_copy(out, in_), r=r, w=w)

    def evac(self, out, in_, r, w):
        self.evn += 1
        self.cp("act" if self.evn % 2 else "dve", out, in_, r, w)

    def tt(self, eng, out, a, b, op, r, w):
        self.k.op(eng, lambda e: e.tensor_tensor(out=out, in0=a, in1=b, op=op), r=r, w=w)

    def ts(self, eng, out, in0, s1, s2, op0, op1, r, w):
        if s2 is None:
            self.k.op(eng, lambda e: e.tensor_scalar(out, in0, s1, None, op0=op0), r=r, w=w)
        else:
            self.k.op(eng, lambda e: e.tensor_scalar(out, in0, s1, s2, op0=op0, op1=op1), r=r, w=w)

    def stt(self, eng, out, in0, scalar, in1, op0, op1, r, w):
        self.k.op(eng, lambda e: e.scalar_tensor_tensor(out=out, in0=in0, scalar=scalar, in1=in1, op0=op0, op1=op1),
                  r=r, w=w)

    def memset(self, eng, ap, val, w):
        self.k.op(eng, lambda e: e.memset(ap, val), w=w)

    def recip(self, out, in_, r, w):
        self.k.op("dve", lambda e: e.reciprocal(out, in_), r=r, w=w)

    def load(self, q, out, in_, r, w, **kw):
        self.k.dma(q, out, in_, r=r, w=w, **kw)

    def bcload(self, tile_, dram_row_ap, r, npart=128):
        self.k.dma("pool", tile_[0:npart], dram_row_ap.partition_broadcast(npart), r=r, w=[tile_])

    def psums(self, n=8):
        return Rot([self.k.psum(name="ps%d" % i) for i in range(n)])

    def rstd(self, ss, rs, n, r):
        self.act(rs, ss, AF.Sqrt, r=r, w=r, scale=1.0 / n, bias=EPS)
        self.recip(rs, rs, r=r, w=r)

    def ph_init(self):
        k = self.k
        k.begin()
        k.dma("sp", self.X[0:L, :], self.x_in.ap(), r=[self.x_in], w=self.X.rows(0, L))
        k.dma("act", self.X[L:NT, :], self.ctx_in.ap(), r=[self.ctx_in], w=self.X.rows(L, NT))
        k.end()

    def ph_mods(self, i):
        k = self.k
        k.begin()
        ps = self.psums(4)
        cct = k.tile([128, 32], name="cct")
        sc = k.tile([128, 32], F32R, name="sc")
        self.load("sp", cct[:], self.cc.ap(), r=[self.cc], w=[cct])
        self.act(sc[:], cct[:], AF.Silu, r=[cct], w=[sc])
        scv = sc[:].rearrange("p (kc r) -> p kc r", r=2)
        wp = Rot([k.tile([128, 16, 512], F32R, name="wada") for _ in range(3)])
        bp = Rot([k.tile([2, 512], name="bada") for _ in range(2)])
        mp = Rot([k.tile([2, 512], name="modo") for _ in range(2)])
        if True:
            wap, wres = self.wsl("w_ada", i)
            wv = wap.rearrange("(kc p) n -> p kc n", p=128)
            for nb in range(24):
                wt = wp.next()
                bt = bp.next()
                mt = mp.next()
                self.load("sp" if nb % 2 == 0 else "act", wt[:], wv[:, :, nb * 512:(nb + 1) * 512].bitcast(F32R),
                          r=wres, w=[wt])
                self.bcload(bt, self.b_ada[i:i + 1, nb * 512:(nb + 1) * 512], r=[self.b_ada], npart=2)
                p = ps.next()
                for kc in range(16):
                    self.mm(p[0:2, :], scv[:, kc, :], wt[:, kc, :], kc == 0, kc == 15, r=[sc, wt], w=[p])
                self.tt("dve", mt[:], p[0:2, :], bt[:], ALU.add, r=[p, bt], w=[mt])
                k.dma("pool", self.MOD[2 * i:2 * i + 2, nb * 512:(nb + 1) * 512], mt[:], r=[mt], w=[self.MOD])
        k.end()

    def mod_row(self, i, r, chunk):
        return self.MOD[2 * i + r:2 * i + r + 1, chunk * D:(chunk + 1) * D]

    def tok_tiles(self, with_ctx):
        return list(range(18 if with_ctx else 16))

    def blocks(self, with_ctx):
        b = [(t0, 512) for t0 in range(0, L, 512)]
        if with_ctx:
            b.append((L, CL))
        return b

    def ph_norm(self, i, kind, with_ctx):
        k = self.k
        k.begin()
        ps = self.psums(6)
        ident = k.tile([128, 128], name="ident")
        self.load("sp", ident[:], self.ident.ap(), r=[self.ident], w=[ident])
        gsrc = self.norm_mix if kind == "mix" else self.norm_ffn
        shc, scc = (0, 1) if kind == "mix" else (3, 4)
        gam = k.tile([128, D], name="gam")
        self.bcload(gam, gsrc[i:i + 1, :], r=[gsrc])
        gm, sh = [], []
        for r_ in range(2 if with_ctx else 1):
            g_ = k.tile([128, D], name="gm")
            s_ = k.tile([128, D], name="sh")
            self.bcload(g_, self.mod_row(i, r_, scc), r=[self.MOD])
            self.stt("dve", g_[:], g_[:], 1.0, gam[:], ALU.add, ALU.mult, r=[g_, gam], w=[g_])
            self.bcload(s_, self.mod_row(i, r_, shc), r=[self.MOD])
            gm.append(g_)
            sh.append(s_)
        xp = Rot([k.tile([128, D], name="xt") for _ in range(2)])
        hp = Rot([k.tile([128, D], name="ht") for _ in range(2)])
        junk = k.tile([128, D], name="junk")
        ssp = Rot([k.tile([128, 2], name="ss") for _ in range(3)])
        if kind == "mix":
            hTb = Rot([k.tile([128, 16, 512], name="hTb") for _ in range(2)])
        else:
            hTt = Rot([k.tile([128, 16, 128], name="hTt") for _ in range(2)])
            wr = k.tile([128, 16, NE], name="wr")
            self.load("sp", wr[:], self.moe_router[i].rearrange("(kc p) e -> p kc e", p=128),
                      r=[self.moe_router], w=[wr])
            ST = k.tile([16, NT], name="ST")
            smp = Rot([k.tile([128, 40], name="sm") for _ in range(2)])
        tiles = self.tok_tiles(with_ctx)
        cur = None
        for tt_ in tiles:
            r_ = 1 if tt_ >= 16 else 0
            r0 = tt_ * 128
            xt = xp.next()
            ht = hp.next()
            ss = ssp.next()
            self.load("sp", xt[:], self.X[r0:r0 + 128, :], r=self.X.rows(r0, r0 + 128), w=[xt])
            self.memset("pool", ss[:], 0.0, w=[ss])
            self.act(junk[:], xt[:], AF.Square, r=[xt], w=[junk, ss], accum=ss[:, 0:1])
            self.rstd(ss[:, 0:1], ss[:, 1:2], D, r=[ss])
            self.stt("dve", ht[:], xt[:], ss[:, 1:2], gm[r_][:], ALU.mult, ALU.mult, r=[xt, ss, gm[r_]], w=[ht])
            self.tt("pool", ht[:], ht[:], sh[r_][:], ALU.add, r=[ht, sh[r_]], w=[ht])
            if kind == "mix":
                bi = tt_ // 4
                tl = tt_ % 4
                if tl == 0:
                    cur = hTb.next()
                for g in range(4):
                    p = ps.next()
                    for q in range(4):
                        kc = g * 4 + q
                        self.tr(p[:, q * 128:(q + 1) * 128], ht[:, kc * 128:(kc + 1) * 128], ident[:],
                                r=[ht, ident], w=[p])
                    self.evac(cur[:, g * 4:(g + 1) * 4, tl * 128:(tl + 1) * 128],
                              p[:].rearrange("p (a b) -> p a b", b=128), r=[p], w=[cur])
                last_in_block = (tl == 3) or (tt_ == tiles[-1])
                if last_in_block:
                    t0 = bi * 512
                    nt = (tl + 1) * 128
                    k.dma("pool", self.HT.ap().rearrange("(kc p) t -> p kc t", p=128)[:, :, t0:t0 + nt],
                          cur[:, :, 0:nt], r=[cur], w=[self.HT])
            else:
                k.dma("pool", self.H[r0:r0 + 128, :], ht[:], r=[ht], w=[self.H])
                hT = hTt.next()
                for g in range(4):
                    p = ps.next()
                    for q in range(4):
                        kc = g * 4 + q
                        self.tr(p[:, q * 128:(q + 1) * 128], ht[:, kc * 128:(kc + 1) * 128], ident[:],
                                r=[ht, ident], w=[p])
                    self.evac(hT[:, g * 4:(g + 1) * 4, :], p[:].rearrange("p (a b) -> p a b", b=128), r=[p], w=[hT])
                p = ps.next()
                for kc in range(16):
                    self.mm(p[:, 0:NE], hT[:, kc, :], wr[:, kc, :], kc == 0, kc == 15, r=[hT, wr], w=[p])
                sm = smp.next()
                k.op("dve", lambda e, sm=sm, p=p: e.tensor_reduce(out=sm[:, 32:33], in_=p[:, 0:NE], axis=AX.X,
                                                                  op=ALU.max), r=[p], w=[sm])
                self.ts("dve", sm[:, 33:34], sm[:, 32:33], -1.0, None, ALU.mult, None, r=[sm], w=[sm])
                self.memset("pool", sm[:, 34:35], 0.0, w=[sm])
                self.act(sm[:, 0:NE], p[:, 0:NE], AF.Exp, r=[p, sm], w=[sm], bias=sm[:, 33:34], accum=sm[:, 34:35])
                self.recip(sm[:, 35:36], sm[:, 34:35], r=[sm], w=[sm])
                self.ts("dve", sm[:, 16:32], sm[:, 0:NE], sm[:, 35:36], None, ALU.mult, None, r=[sm], w=[sm])
                p2 = ps.next()
                self.tr(p2[0:NE, 0:128], sm[:, 16:32], ident[:], r=[sm, ident], w=[p2])
                self.evac(ST[:, r0:r0 + 128], p2[0:NE, 0:128], r=[p2], w=[ST])
        if kind == "ffn":
            self.topk(ST, ident, ps, with_ctx)
        k.end()

    def topk(self, ST, ident, ps, with_ctx):
        k = self.k
        work = k.tile([16, L], name="work")
        vals = k.tile([16, 384], name="vals")
        idxu = k.tile([16, 384], U32, name="idxu")
        idxf = k.tile([16, 384], name="idxf")
        self.memset("pool", vals[:], 0.0, w=[vals])
        self.load("sp", idxf[:, 256:384], self.padf.ap(), r=[], w=[idxf])
        self.cp("dve", work[:], ST[:, 0:L], r=[ST], w=[work])

        def run(wk, n, off):
            for it in range(n // 8):
                sl = slice(off + it * 8, off + it * 8 + 8)
                k.op("dve", lambda e, sl=sl: e.max(out=vals[:, sl], in_=wk), r=[work], w=[vals])
                k.op("dve", lambda e, sl=sl: e.max_index(out=idxu[:, sl], in_max=vals[:, sl], in_values=wk),
                     r=[work, vals], w=[idxu])
                k.op("dve", lambda e, sl=sl: e.match_replace(out=wk, in_to_replace=vals[:, sl], in_values=wk,
                                                             imm_value=-1.0), r=[work, vals], w=[work])

        run(work[:, 0:L], 256, 0)
        self.cp("dve", idxf[:, 0:256], idxu[:, 0:256], r=[idxu], w=[idxf])
        if with_ctx:
            self.cp("dve", work[:, 0:CL], ST[:, L:NT], r=[ST, work], w=[work])
            run(work[:, 0:CL], 32, 256)
            self.cp("dve", idxf[:, 256:288], idxu[:, 256:288], r=[idxu], w=[idxf])
            self.ts("dve", idxf[:, 256:288], idxf[:, 256:288], float(L), None, ALU.add, None, r=[idxf], w=[idxf])
        for c in range(3):
            p = ps.next()
            self.tr(p[:, 0:NE], idxf[:, c * 128:(c + 1) * 128], ident[0:16, 0:16], r=[idxf, ident], w=[p])
            it_ = k.tile([128, NE], I32, name="idxi")
            self.cp("dve", it_[:], p[:, 0:NE], r=[p], w=[it_])
            k.dma("pool", self.IDXD[c * 128:(c + 1) * 128, :], it_[:], r=[it_], w=[self.IDXD])
            p = ps.next()
            self.tr(p[:, 0:NE], vals[:, c * 128:(c + 1) * 128], ident[0:16, 0:16], r=[vals, ident], w=[p])
            vt = k.tile([128, NE], name="valt")
            self.cp("dve", vt[:], p[:, 0:NE], r=[p], w=[vt])
            k.dma("pool", self.TSD[c * 128:(c + 1) * 128, :], vt[:], r=[vt], w=[self.TSD])

    def ph_experts(self, i, with_ctx):
        k = self.k
        k.begin()
        ps = self.psums(8)
        ident = k.tile([128, 128], name="ident")
        self.load("sp", ident[:], self.ident.ap(), r=[self.ident], w=[ident])
        nct = 3 if with_ctx else 2
        NC_ = 288 if with_ctx else 256
        idx = [k.tile([128, NE], I32, name="idx") for _ in range(nct)]
        tsv = [k.tile([128, NE], name="tsv") for _ in range(nct)]
        for c in range(nct):
            self.load("sp", idx[c][:], self.IDXD[c * 128:(c + 1) * 128, :], r=[self.IDXD], w=[idx[c]])
            self.load("sp", tsv[c][:], self.TSD[c * 128:(c + 1) * 128, :], r=[self.TSD], w=[tsv[c]])
        g2 = []
        for r_ in range(2 if with_ctx else 1):
            g_ = k.tile([128, D], name="g2")
            self.bcload(g_, self.mod_row(i, r_, 5), r=[self.MOD])
            g2.append(g_)
        xs = [k.tile([128, D], name="xs") for _ in range(nct)]
        yo = [k.tile([128, D], name="yo") for _ in range(nct)]
        xsT = k.tile([128, 16, NC_], F32R, name="xsT")
        aT = k.tile([128, 8, NC_], F32R, name="aT")
        tmp = Rot([k.tile([128, NC_], name="tmp") for _ in range(2)])
        wp = Rot([k.tile([128, 4096], F32R, name="wexp") for _ in range(5)])
        csz = [128, 128, 32]
        if with_ctx:
            self.memset("pool", xs[2][:], 0.0, w=[xs[2]])
            self.memset("pool", yo[2][:], 0.0, w=[yo[2]])
        qi = 0

        def gather(e):
            for c in range(nct):
                k.dma("pool", None, None, r=[idx[c], self.H], w=[xs[c]],
                      fn=lambda eng, c=c, e=e: eng.indirect_dma_start(
                          out=xs[c][:], out_offset=None, in_=self.H.ap(),
                          in_offset=bass.IndirectOffsetOnAxis(ap=idx[c][:, e:e + 1], axis=0)))

        gather(0)
        for e in range(NE):
            for kc in range(16):
                p = ps.next()
                for c in range(nct):
                    if c < 2:
                        self.tr(p[:, c * 128:(c + 1) * 128], xs[c][:, kc * 128:(kc + 1) * 128], ident[:],
                                r=[xs[c], ident], w=[p])
                    else:
                        self.tr(p[:, 256:288], xs[c][0:32, kc * 128:(kc + 1) * 128], ident[0:32, 0:32],
                                r=[xs[c], ident], w=[p])
                self.evac(xsT[:, kc, :], p[:, 0:NC_], r=[p], w=[xsT])
            if e + 1 < NE:
                gather(e + 1)
            w1ap, w1res = self.wsl("moe_w1", i * NE + e)
            w3ap, w3res = self.wsl("moe_w3", i * NE + e)
            w2ap, w2res = self.wsl("moe_w2", i * NE + e)
            w1v = w1ap.rearrange("(kc p) f -> p kc f", p=128)
            w3v = w3ap.rearrange("(kc p) f -> p kc f", p=128)
            w2v = w2ap.rearrange("(fc p) d -> p fc d", p=128)
            for fq in range(4):
                w1q = wp.next()
                w3q = wp.next()
                w1a = w1q[:].rearrange("p (kc f) -> p kc f", f=256)
                w3a = w3q[:].rearrange("p (kc f) -> p kc f", f=256)
                qi += 1
                self.load("sp" if qi % 2 else "act", w1a, w1v[:, :, fq * 256:(fq + 1) * 256].bitcast(F32R),
                          r=w1res, w=[w1q])
                self.load("act" if qi % 2 else "sp", w3a, w3v[:, :, fq * 256:(fq + 1) * 256].bitcast(F32R),
                          r=w3res, w=[w3q])
                for fl in range(2):
                    fc = fq * 2 + fl
                    p1 = ps.next()
                    p3 = ps.next()
                    for kc in range(16):
                        self.mm(p1[:, 0:NC_], w1a[:, kc, fl * 128:(fl + 1) * 128], xsT[:, kc, :], kc == 0, kc == 15,
                                r=[w1q, xsT], w=[p1])
                    for kc in range(16):
                        self.mm(p3[:, 0:NC_], w3a[:, kc, fl * 128:(fl + 1) * 128], xsT[:, kc, :], kc == 0, kc == 15,
                                r=[w3q, xsT], w=[p3])
                    t_ = tmp.next()
                    self.act(t_[:], p1[:, 0:NC_], AF.Silu, r=[p1], w=[t_])
                    self.tt("dve", aT[:, fc, :], t_[:], p3[:, 0:NC_], ALU.mult, r=[t_, p3], w=[aT])
            for dq in range(4):
                w2q = wp.next()
                w2a = w2q[:].rearrange("p (fc d) -> p fc d", d=512)
                qi += 1
                self.load("sp" if qi % 2 else "act", w2a, w2v[:, :, dq * 512:(dq + 1) * 512].bitcast(F32R),
                          r=w2res, w=[w2q])
                for c in range(nct):
                    cs_ = csz[c]
                    p = ps.next()
                    for fc in range(8):
                        self.mm(p[0:cs_, :], aT[:, fc, c * 128:c * 128 + cs_], w2a[:, fc, :], fc == 0, fc == 7,
                                r=[aT, w2q], w=[p])
                    g_ = g2[1 if c == 2 else 0]
                    self.stt("dve", yo[c][0:cs_, dq * 512:(dq + 1) * 512], p[0:cs_, :], tsv[c][0:cs_, e:e + 1],
                             g_[0:cs_, dq * 512:(dq + 1) * 512], ALU.mult, ALU.mult, r=[p, tsv[c], g_], w=[yo[c]])
            for c in range(nct):
                k.dma("pool", None, None, r=[idx[c], yo[c]], w=[self.X],
                      fn=lambda eng, c=c, e=e: eng.indirect_dma_start(
                          out=self.X.ap(), out_offset=bass.IndirectOffsetOnAxis(ap=idx[c][:, e:e + 1], axis=0),
                          in_=yo[c][:], in_offset=None, compute_op=ALU.add))
        k.end()

    def ph_gla_proj(self, j, with_ctx=True):
        k = self.k
        k.begin()
        ps = self.psums(8)
        hTp = Rot([k.tile([128, 16, 512], F32R, name="hT") for _ in range(2)])
        wp = Rot([k.tile([128, 16, 512], F32R, name="w") for _ in range(2)])
        op_ = Rot([k.tile([128, 512], name="o") for _ in range(4)])
        wa1 = k.tile([128, 16, 32], F32R, name="wa1")
        wa2 = k.tile([33, 2048], name="wa2")
        lowT = k.tile([33, 512], name="lowT")
        self.load("sp", wa1[:], self.gla_wa1[j].rearrange("(kc p) r -> p kc r", p=128).bitcast(F32R),
                  r=[self.gla_wa1], w=[wa1])
        self.load("sp", wa2[:], self.gla_wa2[j], r=[self.gla_wa2], w=[wa2])
        self.memset("pool", lowT[:], 1.0, w=[lowT])
        wap, wres = self.wsl("gla_w_in", j)
        wv = wap.rearrange("(kc p) n -> p kc n", p=128)
        HTv = self.HT.ap().rearrange("(kc p) t -> p kc t", p=128)
        qn = 0
        for (t0, TB) in self.blocks(with_ctx):
            hT = hTp.next()
            self.load("act", hT[:, :, 0:TB], HTv[:, :, t0:t0 + TB].bitcast(F32R), r=[self.HT], w=[hT])
            for nb in range(12):
                wt = wp.next()
                qn += 1
                self.load("sp" if qn % 2 else "act", wt[:], wv[:, :, nb * 512:(nb + 1) * 512].bitcast(F32R),
                          r=wres, w=[wt])
                if nb < 4:
                    dst = self.QT if nb < 2 else self.KT
                    for nl in range(4):
                        p = ps.next()
                        for kc in range(16):
                            self.mm(p[:, 0:TB], wt[:, kc, nl * 128:(nl + 1) * 128], hT[:, kc, 0:TB], kc == 0, kc == 15,
                                    r=[wt, hT], w=[p])
                        o = op_.next()
                        if nb < 2:
                            self.act(o[:, 0:TB], p[:, 0:TB], AF.Copy, r=[p], w=[o], scale=DK ** -0.5)
                        else:
                            self.evac(o[:, 0:TB], p[:, 0:TB], r=[p], w=[o])
                        f0 = ((nb % 2) * 4 + nl) * 128
                        k.dma("pool", dst[f0:f0 + 128, t0:t0 + TB], o[:, 0:TB], r=[o], w=[dst])
                else:
                    dst = self.V if nb < 8 else self.G
                    c0 = ((nb - 4) % 4) * 512
                    for tl in range(TB // 128):
                        p = ps.next()
                        for kc in range(16):
                            self.mm(p[:], hT[:, kc, tl * 128:(tl + 1) * 128], wt[:, kc, :], kc == 0, kc == 15,
                                    r=[wt, hT], w=[p])
                        o = op_.next()
                        if nb < 8:
                            self.evac(o[:], p[:], r=[p], w=[o])
                        else:
                            self.act(o[:], p[:], AF.Silu, r=[p], w=[o])
                        r0 = t0 + tl * 128
                        k.dma("pool", dst[r0:r0 + 128, c0:c0 + 512], o[:], r=[o], w=[dst])
            p = ps.next()
            for kc in range(16):
                self.mm(p[0:32, 0:TB], wa1[:, kc, :], hT[:, kc, 0:TB], kc == 0, kc == 15, r=[wa1, hT], w=[p])
            self.evac(lowT[0:32, 0:TB], p[0:32, 0:TB], r=[p], w=[lowT])
            for tl in range(TB // 128):
                for cb in range(4):
                    p = ps.next()
                    self.mm(p[:], lowT[0:33, tl * 128:(tl + 1) * 128], wa2[0:33, cb * 512:(cb + 1) * 512], True, True,
                            r=[lowT, wa2], w=[p])
                    o = op_.next()
                    self.act(o[:], p[:], AF.Exp, r=[p], w=[o], scale=-1.0)
                    self.act(o[:], o[:], AF.Ln, r=[o], w=[o], bias=1.0)
                    r0 = t0 + tl * 128
                    k.dma("pool", self.U[r0:r0 + 128, cb * 512:(cb + 1) * 512], o[:], r=[o], w=[self.U])
        k.end()

    def ph_gla_scan(self, j):
        k = self.k
        k.begin()
        ps = self.psums(8)
        ident = k.tile([128, 128], name="ident")
        self.load("sp", ident[:], self.ident.ap(), r=[self.ident], w=[ident])
        tri = [k.tile([128, 128], name="tri") for _ in range(2)]
        msk = [k.tile([128, 128], name="msk") for _ in range(2)]
        for z in range(2):
            self.load("sp", tri[z][:], self.tri[z], r=[self.tri], w=[tri[z]])
            self.load("sp", msk[z][:], self.msk[z], r=[self.msk], w=[msk[z]])
        hn = k.tile([128, DV], name="hn")
        self.bcload(hn, self.gla_hn[j:j + 1, :], r=[self.gla_hn])
        oacc = k.tile([128, 18, DV], name="oacc")
        S = [k.tile([128, DV], F32R, name="S") for _ in range(2)]
        zer = k.tile([128, DV], name="zer")
        self.memset("pool", zer[:], 0.0, w=[zer])
        qp = Rot([k.tile([128, 2, 128], name="q") for _ in range(3)])
        kp = Rot([k.tile([128, 2, 128], name="k") for _ in range(3)])
        vp = Rot([k.tile([128, DV], F32R, name="v") for _ in range(3)])
        up = Rot([k.tile([128, 256], name="u") for _ in range(3)])
        ebp = Rot([k.tile([128, 256], name="eb") for _ in range(2)])
        enp = Rot([k.tile([128, 256], name="en") for _ in range(2)])
        qsp = Rot([k.tile([128, 2, 128], F32R, name="qs") for _ in range(2)])
        ksp = Rot([k.tile([128, 2, 128], F32R, name="ks") for _ in range(2)])
        ktp = Rot([k.tile([128, 256], F32R, name="kt") for _ in range(2)])
        atp = Rot([k.tile([128, 128], F32R, name="at") for _ in range(2)])
        tsp = Rot([k.tile([128, DV], name="tS") for _ in range(2)])
        gp = Rot([k.tile([128, DV], name="g") for _ in range(2)])
        onp = Rot([k.tile([128, DV], name="on") for _ in range(2)])
        junk = k.tile([128, DV], name="junk")
        ssp = Rot([k.tile([128, 2], name="ss") for _ in range(3)])
        QTv = self.QT.ap().rearrange("(c p) t -> p c t", p=128)
        KTv = self.KT.ap().rearrange("(c p) t -> p c t", p=128)
        for h in range(HEADS):
            for z in range(2):
                order = [16, 17] + list(range(16)) if z == 0 else [17, 16] + list(range(15, -1, -1))
                last = 127 if z == 0 else 0
                for dc in range(2):
                    self.cp("dve", S[dc][:], zer[:], r=[zer], w=[S[dc]])
                for ti in order:
                    r0 = ti * 128
                    q_ = qp.next()
                    k_ = kp.next()
                    v_ = vp.next()
                    u_ = up.next()
                    self.load("sp", q_[:], QTv[:, 2 * h:2 * h + 2, r0:r0 + 128], r=[self.QT], w=[q_])
                    self.load("act", k_[:], KTv[:, 2 * h:2 * h + 2, r0:r0 + 128], r=[self.KT], w=[k_])
                    self.load("sp", v_[:], self.V[r0:r0 + 128, h * DV:(h + 1) * DV].bitcast(F32R), r=[self.V], w=[v_])
                    c0 = z * 1024 + h * DK
                    self.load("act", u_[:], self.U[r0:r0 + 128, c0:c0 + DK], r=[self.U], w=[u_])
                    pb = ps.next()
                    for dc in range(2):
                        self.mm(pb[:, dc * 128:(dc + 1) * 128], u_[:, dc * 128:(dc + 1) * 128], tri[z][:], True, True,
                                r=[u_, tri[z]], w=[pb])
                    eb = ebp.next()
                    en = enp.next()
                    self.act(eb[:], pb[:, 0:256], AF.Exp, r=[pb], w=[eb])
                    self.act(en[:], pb[:, 0:256], AF.Exp, r=[pb], w=[en], scale=-1.0)
                    qs = qsp.next()
                    ks = ksp.next()
                    self.tt("dve", qs[:], q_[:], eb[:].rearrange("p (c t) -> p c t", t=128), ALU.mult, r=[q_, eb], w=[qs])
                    self.tt("pool", ks[:], k_[:], en[:].rearrange("p (c t) -> p c t", t=128), ALU.mult, r=[k_, en], w=[ks])
                    pt = ps.next()
                    for dc in range(2):
                        self.tr(pt[:, dc * 128:(dc + 1) * 128], ks[:, dc, :].bitcast(F32), ident[:], r=[ks, ident], w=[pt])
                    kt = ktp.next()
                    self.evac(kt[:], pt[:, 0:256], r=[pt], w=[kt])
                    pa = ps.next()
                    for dc in range(2):
                        self.mm(pa[:, 0:128], ks[:, dc, :], qs[:, dc, :], dc == 0, dc == 1, r=[ks, qs], w=[pa])
                    at = atp.next()
                    self.tt("dve", at[:], pa[:, 0:128], msk[z][:], ALU.mult, r=[pa, msk[z]], w=[at])
                    po = ps.next()
                    self.mm(po[:], at[:], v_[:], True, False, r=[at, v_], w=[po])
                    for dc in range(2):
                        self.mm(po[:], qs[:, dc, :], S[dc][:], False, dc == 1, r=[qs, S[dc]], w=[po])
                    if z == 0:
                        self.evac(oacc[:, ti, :], po[:], r=[po], w=[oacc])
                    else:
                        self.tt("dve", oacc[:, ti, :], oacc[:, ti, :], po[:], ALU.add, r=[oacc, po], w=[oacc])
                    for dc in range(2):
                        p_s = ps.next()
                        self.mm(p_s[:], kt[:, dc * 128:(dc + 1) * 128], v_[:], True, True, r=[kt, v_], w=[p_s])
                        tS = tsp.next()
                        self.tt("dve", tS[:], S[dc][:].bitcast(F32), p_s[:], ALU.add, r=[S[dc], p_s], w=[tS])
                        col = dc * 128 + last
                        self.act(S[dc][:], tS[:], AF.Copy, r=[tS, eb], w=[S[dc]], scale=eb[:, col:col + 1])
            for ti in range(18):
                r0 = ti * 128
                ss = ssp.next()
                g_ = gp.next()
                on = onp.next()
                self.load("sp", g_[:], self.G[r0:r0 + 128, h * DV:(h + 1) * DV], r=[self.G], w=[g_])
                self.memset("pool", ss[:], 0.0, w=[ss])
                self.act(junk[:], oacc[:, ti, :], AF.Square, r=[oacc], w=[junk, ss], accum=ss[:, 0:1])
                self.rstd(ss[:, 0:1], ss[:, 1:2], DV, r=[ss])
                self.stt("dve", on[:], oacc[:, ti, :], ss[:, 1:2], hn[:], ALU.mult, ALU.mult, r=[oacc, ss, hn], w=[on])
                self.tt("pool", on[:], on[:], g_[:], ALU.mult, r=[on, g_], w=[on])
                k.dma("pool", self.OG[r0:r0 + 128, h * DV:(h + 1) * DV], on[:], r=[on], w=[self.OG])
        k.end()

    def ph_outproj(self, i, wname, wunit, src, src_major, with_ctx, bias_row=None):
        k = self.k
        k.begin()
        ps = self.psums(8)
        ident = k.tile([128, 128], name="ident")
        self.load("sp", ident[:], self.ident.ap(), r=[self.ident], w=[ident])
        g1 = []
        for r_ in range(2 if with_ctx else 1):
            g_ = k.tile([128, D], name="g1")
            self.bcload(g_, self.mod_row(i, r_, 2), r=[self.MOD])
            g1.append(g_)
        bo = None
        if bias_row is not None:
            bo = k.tile([128, D], name="bo")
            self.bcload(bo, bias_row, r=[self.hy_b_out])
        sT = k.tile([128, 16, 512], F32R, name="sT")
        wp = Rot([k.tile([128, 16, 512], F32R, name="w") for _ in range(2)])
        xt = [k.tile([128, D], name="xt") for _ in range(4)]
        sp_ = Rot([k.tile([128, D], name="st") for _ in range(2)])
        tp = Rot([k.tile([128, 512], name="tp") for _ in range(3)])
        wap, wres = self.wsl(wname, wunit)
        wv = wap.rearrange("(kc p) n -> p kc n", p=128)
        qn = 0
        for (t0, TB) in self.blocks(with_ctx):
            r_ = 1 if t0 >= L else 0
            ntl = TB // 128
            if src_major == "tok":
                for tl in range(ntl):
                    st = sp_.next()
                    r0 = t0 + tl * 128
                    self.load("act", st[:], src[r0:r0 + 128, :], r=[src], w=[st])
                    for g in range(4):
                        p = ps.next()
                        for q in range(4):
                            kc = g * 4 + q
                            self.tr(p[:, q * 128:(q + 1) * 128], st[:, kc * 128:(kc + 1) * 128], ident[:],
                                    r=[st, ident], w=[p])
                        self.evac(sT[:, g * 4:(g + 1) * 4, tl * 128:(tl + 1) * 128],
                                  p[:].rearrange("p (a b) -> p a b", b=128), r=[p], w=[sT])
            else:
                self.load("act", sT[:, :, 0:TB],
                          src.ap().rearrange("(kc p) t -> p kc t", p=128)[:, :, t0:t0 + TB].bitcast(F32R),
                          r=[src], w=[sT])
            for tl in range(ntl):
                r0 = t0 + tl * 128
                self.load("sp", xt[tl][:], self.X[r0:r0 + 128, :], r=self.X.rows(r0, r0 + 128), w=[xt[tl]])
            for nb in range(4):
                wt = wp.next()
                qn += 1
                self.load("sp" if qn % 2 else "act", wt[:], wv[:, :, nb * 512:(nb + 1) * 512].bitcast(F32R),
                          r=wres, w=[wt])
                for tl in range(ntl):
                    p = ps.next()
                    for kc in range(16):
                        self.mm(p[:], sT[:, kc, tl * 128:(tl + 1) * 128], wt[:, kc, :], kc == 0, kc == 15,
                                r=[sT, wt], w=[p])
                    t_ = tp.next()
                    sl = slice(nb * 512, (nb + 1) * 512)
                    if bo is not None:
                        self.tt("dve", t_[:], p[:], bo[:, sl], ALU.add, r=[p, bo], w=[t_])
                        self.tt("pool", t_[:], t_[:], g1[r_][:, sl], ALU.mult, r=[t_, g1[r_]], w=[t_])
                    else:
                        self.tt("dve", t_[:], p[:], g1[r_][:, sl], ALU.mult, r=[p, g1[r_]], w=[t_])
                    self.tt("pool", xt[tl][:, sl], xt[tl][:, sl], t_[:], ALU.add, r=[xt[tl], t_], w=[xt[tl]])
            for tl in range(ntl):
                r0 = t0 + tl * 128
                k.dma("pool", self.X[r0:r0 + 128, :], xt[tl][:], r=[xt[tl]], w=self.X.rows(r0, r0 + 128))
        k.end()

    def ph_hy_filter(self, j, Ls, row0, rn_row0):
        k = self.k
        k.begin()
        ps = self.psums(6)
        MT = Ls // 128
        zT = k.tile([33, Ls], name="zT")
        self.load("sp", zT[:], self.zT[Ls].ap(), r=[self.zT[Ls]], w=[zT])
        w1 = k.tile([33, 64], name="fw1")
        w2 = k.tile([64, 64], name="fw2")
        w3 = k.tile([64, 64], name="fw3")
        w4 = k.tile([64, 2 * D], name="fw4")
        fb = k.tile([64, 3], name="fb")
        fb2 = k.tile([64, 3], name="fb2")
        self.load("sp", w1[:], self.hy_f_w1[j], r=[self.hy_f_w1], w=[w1])
        self.load("sp", w2[:], self.hy_f_w2[j], r=[self.hy_f_w2], w=[w2])
        self.load("sp", w3[:], self.hy_f_w3[j], r=[self.hy_f_w3], w=[w3])
        self.load("act", w4[:], self.hy_f_w4[j], r=[self.hy_f_w4], w=[w4])
        self.load("sp", fb[:], self.hy_fb[j], r=[self.hy_fb], w=[fb])
        OFF = 16.0
        self.ts("dve", fb2[:], fb[:], 1.0 / (2 * math.pi), 0.5 + OFF, ALU.mult, ALU.add, r=[fb], w=[fb2])
        hT = [k.tile([64, Ls], name="hid") for _ in range(3)]
        up = Rot([k.tile([64, 512], name="u") for _ in range(2)])
        uip = Rot([k.tile([64, 512], I32, name="ui") for _ in range(2)])
        ufp = Rot([k.tile([64, 512], name="uf") for _ in range(2)])
        SE = 1e-6
        srcs = [(zT, 33, w1), (hT[0], 64, w2), (hT[1], 64, w3)]
        for li in range(3):
            src, K_, w_ = srcs[li]
            for lb in range((Ls + 511) // 512):
                wdt = min(512, Ls - lb * 512)
                sl = slice(lb * 512, lb * 512 + wdt)
                p = ps.next()
                self.mm(p[0:64, 0:wdt], w_[0:K_, :], src[0:K_, sl], True, True, r=[w_, src], w=[p])
                u = up.next()
                ui = uip.next()
                uf = ufp.next()
                self.act(u[:, 0:wdt], p[0:64, 0:wdt], AF.Identity, r=[p, fb2], w=[u], scale=1.0 / (2 * math.pi),
                         bias=fb2[:, li:li + 1])
                self.cp("dve", ui[:, 0:wdt], u[:, 0:wdt], r=[u], w=[ui])
                self.cp("dve", uf[:, 0:wdt], ui[:, 0:wdt], r=[ui], w=[uf])
                self.tt("dve", u[:, 0:wdt], u[:, 0:wdt], uf[:, 0:wdt], ALU.subtract, r=[u, uf], w=[u])
                self.stt("dve", uf[:, 0:wdt], u[:, 0:wdt], 0.0, u[:, 0:wdt], ALU.is_lt, ALU.add, r=[u], w=[uf])
                self.act(hT[li][:, sl], uf[:, 0:wdt], AF.Sin, r=[uf], w=[hT[li]], scale=2 * math.pi * (1 - SE),
                         bias=-math.pi * (1 - SE))
        negt = k.tile([128, MT], name="negt")
        self.load("sp", negt[:], self.negt[Ls].ap(), r=[self.negt[Ls]], w=[negt])
        dl = k.tile([128, D], name="delta")
        self.bcload(dl, self.delta[0:1, :], r=[self.delta])
        ones = k.tile([128, 1], name="ones")
        self.memset("pool", ones[:], 1.0, w=[ones])
        absum = k.tile([128, D], name="absum")
        self.memset("pool", absum[:], 0.0, w=[absum])
        decp = Rot([k.tile([128, D], name="dec") for _ in range(2)])
        hfp = Rot([k.tile([128, D], name="hf") for _ in range(2)])
        hbp = Rot([k.tile([128, D], name="hb") for _ in range(2)])
        hsp = Rot([k.tile([128, D], name="hs") for _ in range(2)])
        hdp = Rot([k.tile([128, D], name="hd") for _ in range(2)])
        a1 = k.tile([128, D], name="a1")
        a2 = k.tile([128, D], name="a2")
        for mt in range(MT):
            dec = decp.next()
            hf = hfp.next()
            hb = hbp.next()
            hs = hsp.next()
            hd = hdp.next()
            self.act(dec[:], dl[:], AF.Exp, r=[dl, negt], w=[dec], scale=negt[:, mt:mt + 1])
            for nb in range(8):
                p = ps.next()
                self.mm(p[:], hT[2][0:64, mt * 128:(mt + 1) * 128], w4[0:64, nb * 512:(nb + 1) * 512], True, True,
                        r=[hT[2], w4], w=[p])
                dst = hf if nb < 4 else hb
                sl = slice((nb % 4) * 512, (nb % 4 + 1) * 512)
                self.tt("dve", dst[:, sl], p[:], dec[:, sl], ALU.mult, r=[p, dec], w=[dst])
            if mt == 0:
                self.memset("pool", hb[0:1, :], 0.0, w=[hb])
            self.tt("pool", hs[:], hf[:], hb[:], ALU.add, r=[hf, hb], w=[hs])
            self.tt("pool", hd[:], hf[:], hb[:], ALU.subtract, r=[hf, hb], w=[hd])
            r0 = row0 + mt * 128
            k.dma("pool", self.HS[r0:r0 + 128, :], hs[:], r=[hs], w=[self.HS])
            k.dma("pool", self.HD[r0:r0 + 128, :], hd[:], r=[hd], w=[self.HD])
            self.act(a1[:], hf[:], AF.Abs, r=[hf], w=[a1])
            self.act(a2[:], hb[:], AF.Abs, r=[hb], w=[a2])
            self.tt("dve", a1[:], a1[:], a2[:], ALU.add, r=[a1, a2], w=[a1])
            self.tt("dve", absum[:], absum[:], a1[:], ALU.add, r=[absum, a1], w=[absum])
        pn = ps.next()
        for dc in range(16):
            self.mm(pn[:, dc:dc + 1], absum[:, dc * 128:(dc + 1) * 128], ones[:, 0:1], True, True, r=[absum, ones], w=[pn])
        rn = k.tile([128, 16], name="rn")
        self.recip(rn[:], pn[:, 0:16], r=[pn], w=[rn])
        k.dma("pool", self.RN[rn_row0:rn_row0 + 128, :], rn[:], r=[rn], w=[self.RN])
        k.end()

    def ph_dft(self, A, a_row0, Ls, tname, OUT, out_row0, scale_wn):
        k = self.k
        k.begin()
        ps = self.psums(8)
        TC = Ls // 128
        FC = TC + 1
        wn = None
        if scale_wn:
            wn = k.tile([128, FC], name="wn")
            self.load("sp", wn[:], self.wn[Ls].ap(), r=[self.wn[Ls]], w=[wn])
        At = k.tile([128, TC, 1024], F32R, name="At")
        tbp = Rot([k.tile([128, TC, 128], F32R, name="tab") for _ in range(3)])
        otp = Rot([k.tile([128, 1024], name="ot") for _ in range(3)])
        Av = A.ap()[a_row0:a_row0 + Ls, :].rearrange("(tc p) d -> p tc d", p=128)
        qn = 0
        for db in range(2):
            self.load("act", At[:], Av[:, :, db * 1024:(db + 1) * 1024].bitcast(F32R), r=[A], w=[At])
            for fc in range(FC):
                M = 128 if fc < TC else 1
                tab = tbp.next()
                qn += 1
                if Ls == L:
                    tap, tres = self.wsl(tname + "_l", fc)
                else:
                    tc_t = self.cf_c if tname == "cf" else self.sf_c
                    tap, tres = tc_t.ap()[fc * 128:(fc + 1) * 128, :], []
                self.load("sp" if qn % 2 else "act", tab[:],
                          tap.rearrange("p (tc j) -> p tc j", j=128).bitcast(F32R), r=tres, w=[tab])
                ot = otp.next()
                for half in range(2):
                    p = ps.next()
                    for tc in range(TC):
                        self.mm(p[0:M, :], tab[:, tc, 0:M], At[:, tc, half * 512:(half + 1) * 512], tc == 0, tc == TC - 1,
                                r=[tab, At], w=[p])
                    if wn is not None:
                        self.act(ot[0:M, half * 512:(half + 1) * 512], p[0:M, :], AF.Copy, r=[p, wn], w=[ot],
                                 scale=wn[0:M, fc:fc + 1])
                    else:
                        self.evac(ot[0:M, half * 512:(half + 1) * 512], p[0:M, :], r=[p], w=[ot])
                r0 = out_row0 + fc * 128
                k.dma("pool", OUT[r0:r0 + M, db * 1024:(db + 1) * 1024], ot[0:M, :], r=[ot], w=[OUT])
        k.end()

    def ph_hy_proj(self, j, with_ctx):
        k = self.k
        k.begin()
        ps = self.psums(8)
        ident = k.tile([128, 128], name="ident")
        self.load("sp", ident[:], self.ident.ap(), r=[self.ident], w=[ident])
        cols = k.tile([128, 48, 5], name="cols")
        self.load("sp", cols[:], self.hy_cols[j].rearrange("p (n c) -> p n c", c=5), r=[self.hy_cols], w=[cols])
        hT = k.tile([128, 16, 512], F32R, name="hT")
        wp = Rot([k.tile([128, 16, 256], F32R, name="w") for _ in range(5)])
        utok = k.tile([128, 4, D], name="utok")
        pp = Rot([k.tile([128, 512], name="p") for _ in range(3)])
        yp = Rot([k.tile([128, 512], name="y") for _ in range(6)])
        uu = Rot([k.tile([128, 512], name="uu") for _ in range(2)])
        wap, wres = self.wsl("hy_w_in", j)
        wv = wap.rearrange("(kc p) n -> p kc n", p=128)
        HTv = self.HT.ap().rearrange("(kc p) t -> p kc t", p=128)
        qn = 0
        for (t0, TB) in self.blocks(with_ctx):
            ntl = TB // 128
            grid = t0 < L
            self.load("act", hT[:, :, 0:TB], HTv[:, :, t0:t0 + TB].bitcast(F32R), r=[self.HT], w=[hT])
            for jb in range(8):
                wts = []
                for sec in range(3):
                    wt = wp.next()
                    qn += 1
                    c0 = sec * D + jb * 256
                    self.load("sp" if qn % 2 else "act", wt[:], wv[:, :, c0:c0 + 256].bitcast(F32R),
                              r=wres, w=[wt])
                    wts.append(wt)
                for jl in range(2):
                    jc = jb * 2 + jl
                    ys = []
                    for sec in range(3):
                        nc_ = sec * 16 + jc
                        p = ps.next()
                        for kc in range(16):
                            self.mm(p[:, 0:TB], wts[sec][:, kc, jl * 128:(jl + 1) * 128], hT[:, kc, 0:TB], kc == 0,
                                    kc == 15, r=[wts[sec], hT], w=[p])
                        pt = pp.next()
                        y = yp.next()
                        self.act(pt[:, 0:TB], p[:, 0:TB], AF.Identity, r=[p, cols], w=[pt], bias=cols[:, nc_, 0:1])
                        self.act(y[:, 0:TB], pt[:, 0:TB], AF.Identity, r=[pt, cols], w=[y], scale=cols[:, nc_, 2:3],
                                 bias=cols[:, nc_, 4:5])
                        if grid:
                            pv = pt[:, 0:TB].rearrange("p (r w) -> p r w", w=64)
                            yv = y[:, 0:TB].rearrange("p (r w) -> p r w", w=64)
                            self.stt("dve", yv[:, :, 1:64], pv[:, :, 0:63], cols[:, nc_, 1:2], yv[:, :, 1:64], ALU.mult,
                                     ALU.add, r=[pt, y, cols], w=[y])
                            self.stt("dve", yv[:, :, 0:63], pv[:, :, 1:64], cols[:, nc_, 3:4], yv[:, :, 0:63], ALU.mult,
                                     ALU.add, r=[pt, y, cols], w=[y])
                        else:
                            self.stt("dve", y[:, 1:TB], pt[:, 0:TB - 1], cols[:, nc_, 1:2], y[:, 1:TB], ALU.mult,
                                     ALU.add, r=[pt, y, cols], w=[y])
                            self.stt("dve", y[:, 0:TB - 1], pt[:, 1:TB], cols[:, nc_, 3:4], y[:, 0:TB - 1], ALU.mult,
                                     ALU.add, r=[pt, y, cols], w=[y])
                        ys.append(y)
                    k.dma("pool", self.X0T[jc * 128:(jc + 1) * 128, t0:t0 + TB], ys[0][:, 0:TB], r=[ys[0]], w=[self.X0T])
                    u = uu.next()
                    self.tt("pool", u[:, 0:TB], ys[1][:, 0:TB], ys[2][:, 0:TB], ALU.mult, r=[ys[1], ys[2]], w=[u])
                    k.dma("pool", self.UT[jc * 128:(jc + 1) * 128, t0:t0 + TB], u[:, 0:TB], r=[u], w=[self.UT])
                    p = ps.next()
                    for tl in range(ntl):
                        self.tr(p[:, tl * 128:(tl + 1) * 128], u[:, tl * 128:(tl + 1) * 128], ident[:], r=[u, ident], w=[p])
                    self.evac(utok[:, 0:ntl, jc * 128:(jc + 1) * 128],
                              p[:, 0:TB].rearrange("p (a b) -> p a b", b=128), r=[p], w=[utok])
            k.dma("pool", self.UTOK.ap()[t0:t0 + TB, :].rearrange("(a p) d -> p a d", p=128), utok[:, 0:ntl, :],
                  r=[utok], w=[self.UTOK])
        k.end()

    def ph_hy_inv(self, j, Ls, tok0, frow0, rn_row0):
        k = self.k
        k.begin()
        ps = self.psums(8)
        TC = Ls // 128
        FC = TC + 1
        GS = min(4, TC)
        NG = TC // GS
        TBW = min(512, Ls)
        NTB = Ls // TBW
        rn = k.tile([128, 16], name="rn")
        skc = k.tile([128, 16], name="skc")
        self.load("sp", rn[:], self.RN[rn_row0:rn_row0 + 128, :], r=[self.RN], w=[rn])
        self.load("sp", skc[:], self.hy_skipc[j], r=[self.hy_skipc], w=[skc])
        YR = k.tile([128, FC, 512], F32R, name="YR")
        YI = k.tile([128, FC, 512], F32R, name="YI")
        ldp = Rot([k.tile([128, 4, 512], name="ld") for _ in range(2)])
        tmp = Rot([k.tile([128, 4, 512], name="tm") for _ in range(2)])
        cip = Rot([k.tile([128, GS, TBW], F32R, name="ci") for _ in range(2)])
        nsp = Rot([k.tile([128, GS, TBW], F32R, name="nsi") for _ in range(2)])
        cnp = Rot([k.tile([1, TBW], F32R, name="cin") for _ in range(2)])
        e1 = Rot([k.tile([128, TBW], name="e1") for _ in range(2)])
        e2 = Rot([k.tile([128, TBW], name="e2") for _ in range(2)])
        e3 = Rot([k.tile([128, TBW], name="e3") for _ in range(2)])
        e4 = Rot([k.tile([128, TBW], name="e4") for _ in range(2)])
        for db in range(4):
            dsl = slice(db * 512, (db + 1) * 512)
            for fc in range(FC):
                M = 128 if fc < TC else 1
                r0 = frow0 + fc * 128
                ld = ldp.next()
                tm = tmp.next()
                for n_, src in enumerate((self.URE, self.UIM, self.KRE, self.KIM)):
                    self.load("sp" if n_ % 2 else "act", ld[0:M, n_, :], src[r0:r0 + M, dsl], r=[src], w=[ld])
                ur, ui, kr, ki = (ld[0:M, n_, :] for n_ in range(4))
                self.tt("dve", tm[0:M, 0, :], ur, kr, ALU.mult, r=[ld], w=[tm])
                self.tt("pool", tm[0:M, 1, :], ui, ki, ALU.mult, r=[ld], w=[tm])
                self.tt("pool", tm[0:M, 2, :], ur, ki, ALU.mult, r=[ld], w=[tm])
                self.tt("dve", tm[0:M, 3, :], ui, kr, ALU.mult, r=[ld], w=[tm])
                self.tt("dve", YR[0:M, fc, :], tm[0:M, 0, :], tm[0:M, 1, :], ALU.subtract, r=[tm], w=[YR])
                self.tt("pool", YI[0:M, fc, :], tm[0:M, 2, :], tm[0:M, 3, :], ALU.add, r=[tm], w=[YI])
            for tb in range(NTB):
                pd = [ps.next() for _ in range(4)]
                for g in range(NG):
                    ci = cip.next()
                    nsi = nsp.next()
                    if Ls == L:
                        ciap, cires = self.wsl("ci_l", tb * 4 + g)
                        nsap, nsres = self.wsl("nsi_l", tb * 4 + g)
                    else:
                        ciap, cires = self.ci_c.ap(), []
                        nsap, nsres = self.nsi_c.ap(), []
                    self.load("sp", ci[:], ciap.rearrange("p (f t) -> p f t", t=TBW).bitcast(F32R), r=cires, w=[ci])
                    self.load("act", nsi[:], nsap.rearrange("p (f t) -> p f t", t=TBW).bitcast(F32R), r=nsres, w=[nsi])
                    for dc in range(4):
                        for fl in range(GS):
                            fc = g * GS + fl
                            self.mm(pd[dc][:, 0:TBW], YR[:, fc, dc * 128:(dc + 1) * 128], ci[:, fl, :],
                                    g == 0 and fl == 0, False, r=[YR, ci], w=[pd[dc]])
                            self.mm(pd[dc][:, 0:TBW], YI[:, fc, dc * 128:(dc + 1) * 128], nsi[:, fl, :], False, False,
                                    r=[YI, nsi], w=[pd[dc]])
                cn = cnp.next()
                self.load("sp", cn[:], self.cin[Ls][0:1, tb * TBW:(tb + 1) * TBW].bitcast(F32R), r=[self.cin[Ls]], w=[cn])
                for dc in range(4):
                    self.mm(pd[dc][:, 0:TBW], YR[0:1, TC, dc * 128:(dc + 1) * 128], cn[0:1, :], False, True,
                            r=[YR, cn], w=[pd[dc]])
                    dch = db * 4 + dc
                    c0 = tok0 + tb * TBW
                    x0t = e1.next()
                    ut = e2.next()
                    t1 = e3.next()
                    t2 = e4.next()
                    self.load("sp", x0t[:], self.X0T[dch * 128:(dch + 1) * 128, c0:c0 + TBW], r=[self.X0T], w=[x0t])
                    self.load("act", ut[:], self.UT[dch * 128:(dch + 1) * 128, c0:c0 + TBW], r=[self.UT], w=[ut])
                    self.act(t1[:], pd[dc][:, 0:TBW], AF.Copy, r=[pd[dc], rn], w=[t1], scale=rn[:, dch:dch + 1])
                    self.stt("dve", t2[:], ut[:], skc[:, dch:dch + 1], t1[:], ALU.mult, ALU.add, r=[ut, skc, t1], w=[t2])
                    self.tt("pool", t2[:], t2[:], x0t[:], ALU.mult, r=[t2, x0t], w=[t2])
                    k.dma("pool", self.YHT[dch * 128:(dch + 1) * 128, c0:c0 + TBW], t2[:], r=[t2], w=[self.YHT])
        k.end()

    def ph_final(self):
        k = self.k
        k.begin()
        gam = k.tile([128, D], name="gam")
        self.bcload(gam, self.norm_final[0:1, :], r=[self.norm_final])
        xp = Rot([k.tile([128, D], name="xt") for _ in range(3)])
        op_ = Rot([k.tile([128, D], name="ot") for _ in range(3)])
        junk = k.tile([128, D], name="junk")
        ssp = Rot([k.tile([128, 2], name="ss") for _ in range(3)])
        for tt_ in range(16):
            r0 = tt_ * 128
            xt = xp.next()
            ot = op_.next()
            ss = ssp.next()
            self.load("sp", xt[:], self.X[r0:r0 + 128, :], r=self.X.rows(r0, r0 + 128), w=[xt])
            self.memset("pool", ss[:], 0.0, w=[ss])
            self.act(junk[:], xt[:], AF.Square, r=[xt], w=[junk, ss], accum=ss[:, 0:1])
            self.rstd(ss[:, 0:1], ss[:, 1:2], D, r=[ss])
            self.stt("dve", ot[:], xt[:], ss[:, 1:2], gam[:], ALU.mult, ALU.mult, r=[xt, ss, gam], w=[ot])
            k.dma("act", self.out[r0:r0 + 128, :], ot[:], r=[ot], w=self.out.rows(r0, r0 + 128))
        k.end()

    def ph_dump(self, src, r0, r1):
        k = self.k
        k.begin()
        k.dma("sp", self.out.ap(), src[r0:r1, :], r=[src], w=[self.out])
        k.end()

    def layer_mixer(self, i):
        need_ctx = i < DEPTH - 1
        j = i // 2
        if i % 2 == 0:
            self.ph_norm(i, "mix", True)
            self.ph_gla_proj(j)
            self.ph_gla_scan(j)
            self.ph_outproj(i, "gla_w_out", j, self.OG, "tok", need_ctx)
        else:
            self.ph_norm(i, "mix", need_ctx)
            streams = [(L, 0, 0, 0)] + ([(CL, L, 17 * 128, 128)] if need_ctx else [])
            for (Ls, tok0, frow0, rn0) in streams:
                self.ph_hy_filter(j, Ls, tok0, rn0)
                self.ph_dft(self.HS, tok0, Ls, "cf", self.KRE, frow0, True)
                self.ph_dft(self.HD, tok0, Ls, "sf", self.KIM, frow0, True)
            self.ph_hy_proj(j, need_ctx)
            for (Ls, tok0, frow0, rn0) in streams:
                self.ph_dft(self.UTOK, tok0, Ls, "cf", self.URE, frow0, False)
                self.ph_dft(self.UTOK, tok0, Ls, "sf", self.UIM, frow0, False)
                self.ph_hy_inv(j, Ls, tok0, frow0, rn0)
            self.ph_outproj(i, "hy_w_out", j, self.YHT, "feat", need_ctx, bias_row=self.hy_b_out[j:j + 1, :])

    def layer_ffn(self, i):
        need_ctx = i < DEPTH - 1
        self.ph_norm(i, "ffn", need_ctx)
        self.ph_experts(i, need_ctx)

    def build(self, stages=None):
        self.ph_init()
        if not self.direct:
            self.ph_gather()
        for i in range(DEPTH):
            if stages is not None and ("mix", i) not in stages and ("ffn", i) not in stages:
                continue
            self.ph_mods(i)
            if stages is None or ("mix", i) in stages:
                self.layer_mixer(i)
            if stages is None or ("ffn", i) in stages:
                self.layer_ffn(i)
        if self.debug_dump is None:
            self.ph_final()
        else:
            src = getattr(self, self.debug_dump[0])
            self.ph_dump(src, self.debug_dump[2], self.debug_dump[2] + self.debug_dump[1][0])
        self.k.finish()
        return self.nc


_CONST = None


def constants():
    global _CONST
    if _CONST is not None:
        return _CONST
    f32 = np.float32
    c = {}
    c["ident"] = np.eye(128, dtype=f32)
    s = np.arange(128)
    tri_f = (s[:, None] <= s[None, :]).astype(f32)
    tri_b = (s[:, None] >= s[None, :]).astype(f32)
    c["tri"] = np.stack([tri_f, tri_b]) * f32(-1.0 / 16.0)
    c["msk"] = np.stack([tri_f, tri_b])
    for Ls, nm in ((L, "l"), (CL, "c")):
        t = np.linspace(0.0, 1.0, Ls, dtype=f32)[:, None]
        w = (2 * np.pi * np.arange(Ls, dtype=f32)[:, None] / Ls).astype(f32)
        f = np.linspace(1e-4, 15, 16, dtype=f32)[None, :]
        fw = (f * w).astype(f32)
        z = np.concatenate([t, np.cos(fw), -np.sin(fw)], axis=-1).astype(f32)
        c["zT_" + nm] = np.ascontiguousarray(z.T)
        tt_ = np.linspace(0.0, 1.0, Ls, dtype=f32)
        c["negt_" + nm] = np.ascontiguousarray((-tt_).reshape(Ls // 128, 128).T)
        N = 2 * Ls
        TC = Ls // 128
        FCn = TC + 1
        tq = np.arange(Ls, dtype=np.float64)
        fq = np.arange(FCn * 128, dtype=np.float64)
        ang = 2 * np.pi * np.outer(tq, fq) / N
        valid = (fq <= Ls)[None, :]
        C = np.where(valid, np.cos(ang), 0.0)
        Sn = np.where(valid, -np.sin(ang), 0.0)
        def fwd(T):
            T4 = T.reshape(TC, 128, FCn, 128)
            return np.ascontiguousarray(T4.transpose(2, 1, 0, 3).reshape(FCn, 128, TC * 128)).astype(f32)
        c["cf_" + nm] = fwd(C)
        c["sf_" + nm] = fwd(Sn)
        TBW = min(512, Ls)
        NTB = Ls // TBW
        GS = min(4, TC)
        NG = TC // GS
        def inv(T):
            T5 = T[:, :TC * 128].reshape(NTB, TBW, NG, GS, 128)
            return np.ascontiguousarray(T5.transpose(0, 2, 4, 3, 1).reshape(NTB, NG, 128, GS * TBW)).astype(f32)
        c["ci_" + nm] = inv(C)
        c["nsi_" + nm] = inv(Sn)
        c["cin_" + nm] = np.cos(np.pi * tq)[None, :].astype(f32)
        wf = np.full(FCn * 128, 2.0 / N)
        wf[0] = 1.0 / N
        wf[Ls] = 1.0 / N
        wf[Ls + 1:] = 0.0
        c["wn_" + nm] = np.ascontiguousarray(wf.reshape(FCn, 128).T).astype(f32)
    max_decay = math.log(1e-2) / 0.3
    min_decay = math.log(1e-2) / 1.5
    c["padf"] = np.ascontiguousarray(np.broadcast_to((NT + np.arange(128)).astype(f32)[None, :], (16, 128)))
    c["delta"] = np.abs(np.linspace(min_decay, max_decay, D, dtype=f32))[None, :].astype(f32)
    _CONST = c
    return c


def host_inputs(inp, ncores=NCORES, units=None, direct=None):
    f32 = np.float32
    g = lambda n: np.ascontiguousarray(np.asarray(inp[n], dtype=f32))
    direct = (ncores == 1) if direct is None else direct
    units = units if units is not None else all_units()
    dcache = {}
    shared = {}
    for n in ("b_ada", "norm_mix", "norm_ffn", "hy_f_w1", "hy_f_w2", "hy_f_w3", "hy_f_w4", "hy_b_out", "moe_router"):
        shared[n] = g(n)
    shared["norm_final"] = g("norm_final").reshape(1, D)
    shared["gla_hn"] = g("gla_head_norm")
    a1 = g("gla_w_a1")
    shared["gla_wa1"] = np.ascontiguousarray(a1.transpose(0, 2, 1, 3).reshape(2, D, 32))
    a2 = g("gla_w_a2")
    ba = g("gla_b_a")
    wa2 = np.zeros((2, 33, 2048), f32)
    for z in range(2):
        wa2[:, z * 16:(z + 1) * 16, z * 1024:(z + 1) * 1024] = a2[:, z]
        wa2[:, 32, z * 1024:(z + 1) * 1024] = ba[:, z]
    shared["gla_wa2"] = wa2
    cw = g("hy_conv_w")
    cols = np.stack([g("hy_b_in"), cw[:, 0], cw[:, 1], cw[:, 2], g("hy_conv_b")], axis=-1)
    shared["hy_cols"] = np.ascontiguousarray(cols.reshape(2, 48, 128, 5).transpose(0, 2, 1, 3).reshape(2, 128, 240))
    shared["hy_fb"] = np.ascontiguousarray(np.stack([g("hy_f_b1"), g("hy_f_b2"), g("hy_f_b3")], axis=-1))
    shared["hy_skipc"] = np.ascontiguousarray(g("hy_skip").reshape(2, 16, 128).transpose(0, 2, 1))
    cst = constants()
    big = {}
    for n in SHARDED:
        C, rpu, nu, CH = SHARDED[n]
        src = cst[n] if n in cst else np.asarray(inp[n], dtype=f32)
        big[n] = src.reshape(nu * rpu, C)
    for n, v in cst.items():
        if n in SHARDED:
            continue
        if n in ("cf_c", "sf_c"):
            v = v.reshape(3 * 128, 2 * 128)
        if n in ("ci_c", "nsi_c"):
            v = v.reshape(128, 2 * 256)
        shared[n] = v
    x = g("x")
    ctx = g("ctx")
    c = g("c")
    cctx = g("c_ctx")
    maps = []
    for b in range(ncores):
        m = dict(shared)
        m["x"] = x[b]
        m["ctx"] = ctx[b]
        cc = np.stack([c[b], cctx], axis=-1)
        m["cc"] = np.ascontiguousarray(cc.reshape(16, 128, 2).transpose(1, 0, 2).reshape(128, 32))
        for n in SHARDED:
            C, rpu, nu, CH = SHARDED[n]
            if direct:
                if n not in dcache:
                    us = units[n] if len(units[n]) else [0]
                    dcache[n] = big[n] if len(us) == nu else np.ascontiguousarray(
                        big[n].reshape(nu, rpu, C)[us].reshape(-1, C))
                m[n] = dcache[n]
            else:
                R = nu * rpu
                m[n] = np.ascontiguousarray(big[n].reshape(R // CH, NCORES, CH // NCORES, C)[:, b].reshape(-1, C))
        maps.append(m)
    return maps


_PROG = {}


REPLICATE = True


def get_prog():
    if "p" not in _PROG:
        p = Prog(direct=REPLICATE)
        p.build()
        _PROG["p"] = p
    return _PROG["p"]


def kernel(**inputs):
    maps = host_inputs(inputs, direct=REPLICATE)
    prog = get_prog()
    res = run_bass_kernel_spmd(prog.nc, maps, core_ids=list(range(NCORES)))
    out = np.stack([np.asarray(r["out"]) for r in res.results], axis=0)
    return out.astype(np.float32)
```

```python
import math
import numpy as np
from contextlib import ExitStack
import concourse.bass as bass
import concourse.mybir as mybir
from concourse.bass_utils import run_bass_kernel_spmd

F32 = mybir.dt.float32
F32R = mybir.dt.float32r
I32 = mybir.dt.int32
U32 = mybir.dt.uint32
AF = mybir.ActivationFunctionType
ALU = mybir.AluOpType
AX = mybir.AxisListType

D = 2048
L = 2048
CL = 256
NT = L + CL
DEPTH = 4
EPS = 1e-6
NE = 16
FF = 1024
HEADS = 4
DK = 256
DV = 512
NCORES = 8

SEM_LIMIT = 16000
NDMA = 8


class Res:
    __slots__ = ("w", "r")

    def __init__(self):
        self.w = {}
        self.r = {}


class Tile:
    def __init__(self, h, nblk=1, bs=None, is_dram=False, persist=False):
        self.h = h
        self.res = [Res() for _ in range(nblk)]
        self.bs = bs
        self.is_dram = is_dram
        self.persist = persist

    def __getitem__(self, key):
        if self.is_dram:
            return self.h.ap()[key]
        return self.h[key]

    def ap(self):
        return self.h.ap() if self.is_dram else self.h[:]

    def rows(self, r0, r1):
        if self.bs is None:
            return list(self.res)
        return self.res[r0 // self.bs:(r1 - 1) // self.bs + 1]


def _flat(lst):
    out = []
    for x in lst:
        if isinstance(x, Res):
            out.append(x)
        elif isinstance(x, Tile):
            out.extend(x.res)
        else:
            out.extend(_flat(x))
    return out


class K:
    COMPUTE = ("pe", "act", "dve", "pool")
    ALL = ("pe", "act", "dve", "pool", "sp")

    def __init__(self, nc):
        self.nc = nc
        self.ges = ExitStack()
        self.sems = {}
        self.cs = {}
        self.nsem = 0
        for e in self.COMPUTE:
            self._new_csem(e)
        self.ds = {q: [[self._new_sem("d_%s%d" % (q, i)), 0] for i in range(NDMA)]
                   for q in ("sp", "pool", "act")}
        self.dn = {q: 0 for q in self.ds}
        self.ops = {e: [] for e in self.ALL}
        self.waited = {e: {} for e in self.ALL}
        self.tiles = []
        self.pes = None
        self.ntile = 0
        self.ninst = 0
        self.ccsem = None

    def _new_sem(self, name):
        name = "%s_%d" % (name, self.nsem)
        self.nsem += 1
        self.sems[name] = self.ges.enter_context(self.nc.semaphore(name))
        return name

    def _new_csem(self, e):
        self.cs[e] = [self._new_sem("c_" + e), 0]

    def dram(self, name, shape, dtype=F32, kind="Internal", bs=None, persist=False):
        h = self.nc.dram_tensor(name, list(shape), dtype, kind=kind)
        nblk = 1 if bs is None else (shape[0] + bs - 1) // bs
        t = Tile(h, nblk=nblk, bs=bs, is_dram=True, persist=persist)
        self.tiles.append(t)
        return t

    def tile(self, shape, dtype=F32, name=None):
        self.ntile += 1
        name = "%s_%d" % (name or "t", self.ntile)
        h = self.pes.enter_context(self.nc.sbuf_tensor(name, list(shape), dtype))
        t = Tile(h)
        self.tiles.append(t)
        return t

    def psum(self, shape=(128, 512), dtype=F32, name=None):
        self.ntile += 1
        name = "%s_%d" % (name or "ps", self.ntile)
        h = self.pes.enter_context(self.nc.psum_tensor(name, list(shape), dtype))
        t = Tile(h)
        self.tiles.append(t)
        return t

    def _deps(self, eng, r, w):
        deps = {}
        for x in _flat(r):
            for s, v in x.w.items():
                if deps.get(s, 0) < v:
                    deps[s] = v
        for x in _flat(w):
            for d in (x.w, x.r):
                for s, v in d.items():
                    if deps.get(s, 0) < v:
                        deps[s] = v
        waits = []
        wd = self.waited[eng]
        for s, v in deps.items():
            if eng == "pe" and s.startswith("c_pe"):
                continue
            if wd.get(s, 0) < v:
                waits.append((s, v))
                wd[s] = v
        return waits

    def _mark(self, r, w, s, v):
        for x in _flat(r):
            if x.r.get(s, 0) < v:
                x.r[s] = v
        for x in _flat(w):
            x.w = {s: v}
            x.r = {}

    def op(self, eng, fn, r=(), w=()):
        waits = self._deps(eng, r, w)
        c = self.cs[eng]
        if c[1] >= SEM_LIMIT:
            self._new_csem(eng)
            c = self.cs[eng]
        c[1] += 1
        self.ops[eng].append((waits, fn, c[0], 1))
        self._mark(r, w, c[0], c[1])
        self.ninst += 1 + len(waits)

    def dma(self, q, out, in_, r=(), w=(), fn=None, **kw):
        waits = self._deps(q, r, w)
        j = self.dn[q] % NDMA
        self.dn[q] += 1
        slot = self.ds[q][j]
        if slot[1] + 16 > SEM_LIMIT:
            slot[0] = self._new_sem("d_%s%d" % (q, j))
            slot[1] = 0
        if slot[1] > 0 and self.waited[q].get(slot[0], 0) < slot[1]:
            waits.append((slot[0], slot[1]))
            self.waited[q][slot[0]] = slot[1]
        slot[1] += 16
        if fn is None:
            def fn(e, out=out, in_=in_, kw=kw):
                return e.dma_start(out=out, in_=in_, **kw)
        self.ops[q].append((waits, fn, slot[0], 16))
        self._mark(r, w, slot[0], slot[1])
        self.ninst += 1 + len(waits)

    def coll(self, fn, r=(), w=()):
        if self.ccsem is None:
            self.ccsem = [self._new_sem("cc"), 0]
        waits = self._deps("pool", r, w)
        self.ccsem[1] += 1
        self.ops["pool"].append((waits, fn, self.ccsem[0], 1))
        self._mark(r, w, self.ccsem[0], self.ccsem[1])

    def barrier(self):
        cur = {}
        for e in self.COMPUTE:
            n, c = self.cs[e]
            if c > 0:
                cur[n] = c
        for q in self.ds:
            for n, v in self.ds[q]:
                if v > 0:
                    cur[n] = v
        for e in self.ALL:
            wd = self.waited[e]
            waits = []
            for s, v in cur.items():
                if wd.get(s, 0) < v:
                    waits.append((s, v))
                    wd[s] = v
            if waits:
                self.ops[e].append((waits, None, None, 0))
        for t in self.tiles:
            if t.persist:
                continue
            for x in t.res:
                x.w = {}
                x.r = {}

    def begin(self):
        self.pes = ExitStack()

    def end(self):
        self.barrier()
        nc = self.nc
        ops = self.ops
        sems = self.sems

        def replay(name, e):
            for waits, fn, s, inc in ops[name]:
                for ws, wv in waits:
                    e.wait_ge(sems[ws], wv)
                if fn is not None:
                    ins = fn(e)
                    ins.then_inc(sems[s], inc)

        with nc.Block() as blk:
            @blk.sync
            def _(e):
                replay("sp", e)

            @blk.scalar
            def _(e):
                replay("act", e)

            @blk.vector
            def _(e):
                replay("dve", e)

            @blk.gpsimd
            def _(e):
                replay("pool", e)

            @blk.tensor
            def _(e):
                replay("pe", e)
        self.ops = {e: [] for e in self.ALL}
        self.pes.close()
        self.pes = None
        self.tiles = [t for t in self.tiles if t.is_dram]

    def finish(self):
        self.ges.close()


class Rot:
    def __init__(self, tiles):
        self.t = tiles
        self.i = 0

    def next(self):
        t = self.t[self.i % len(self.t)]
        self.i += 1
        return t


SHARDED = {
    "w_ada": (6 * D, D, 4, 1024),
    "gla_w_in": (6144, D, 2, 2048),
    "gla_w_out": (D, D, 2, 2048),
    "hy_w_in": (3 * D, D, 2, 2048),
    "hy_w_out": (D, D, 2, 2048),
    "moe_w1": (FF, D, 64, 8192),
    "moe_w3": (FF, D, 64, 8192),
    "moe_w2": (D, FF, 64, 4096),
    "cf_l": (16 * 128, 128, 17, 2176),
    "sf_l": (16 * 128, 128, 17, 2176),
    "ci_l": (4 * 512, 128, 16, 2048),
    "nsi_l": (4 * 512, 128, 16, 2048),
}


GROUP = {"w_ada": 1, "moe_w1": 16, "moe_w3": 16, "moe_w2": 16}


def all_units():
    return {n: list(range(v[2])) for n, v in SHARDED.items()}


class Prog:
    def __init__(self, ncores=NCORES, units=None, debug_dump=None, direct=None):
        self.ncores = ncores
        self.direct = (ncores == 1) if direct is None else direct
        self.units = units if units is not None else all_units()
        self.debug_dump = debug_dump
        nc = bass.Bass("TRN2", target_bir_lowering=False)
        nc.dge_precook = False
        self.nc = nc
        self.k = K(nc)
        self.evn = 0
        self.decl()

    def decl(self):
        k = self.k
        I = lambda name, shape, dt=F32: k.dram(name, shape, dt, kind="ExternalInput")
        S = lambda name, shape, dt=F32, bs=None: k.dram(name, shape, dt, kind="Internal", bs=bs)
        self.x_in = I("x", [L, D])
        self.ctx_in = I("ctx", [CL, D])
        self.cc = I("cc", [128, 32])
        self.b_ada = I("b_ada", [DEPTH, 6 * D])
        self.norm_mix = I("norm_mix", [DEPTH, D])
        self.norm_ffn = I("norm_ffn", [DEPTH, D])
        self.norm_final = I("norm_final", [1, D])
        self.gla_wa1 = I("gla_wa1", [2, D, 32])
        self.gla_wa2 = I("gla_wa2", [2, 33, 2048])
        self.gla_hn = I("gla_hn", [2, DV])
        self.hy_cols = I("hy_cols", [2, 128, 48 * 5])
        self.hy_f_w1 = I("hy_f_w1", [2, 33, 64])
        self.hy_fb = I("hy_fb", [2, 64, 3])
        self.hy_f_w2 = I("hy_f_w2", [2, 64, 64])
        self.hy_f_w3 = I("hy_f_w3", [2, 64, 64])
        self.hy_f_w4 = I("hy_f_w4", [2, 64, 2 * D])
        self.hy_skipc = I("hy_skipc", [2, 128, 16])
        self.hy_b_out = I("hy_b_out", [2, D])
        self.moe_router = I("moe_router", [DEPTH, D, NE])
        self.wts = {}
        self.wsh = {}
        self.wbn = {}
        for n, (C, rpu, nu, CH) in SHARDED.items():
            if self.direct:
                nuse = max(1, len(self.units[n]))
                self.wts[n] = k.dram(n, [nuse * rpu, C], F32, kind="ExternalInput")
            else:
                R = nu * rpu
                gu = GROUP.get(n, nu)
                self.wsh[n] = k.dram(n, [R // NCORES, C], F32, kind="ExternalInput")
                self.wbn[n] = k.dram(n + "_bn", [R // NCORES, C], F32, kind="Internal")
                self.wts[n] = [k.dram("%s_g%d" % (n, gi), [gu * rpu, C], F32, kind="Internal", bs=CH, persist=True)
                               for gi in range(nu // gu)]
        self.ident = I("ident", [128, 128])
        self.padf = I("padf", [16, 128])
        self.tri = I("tri", [2, 128, 128])
        self.msk = I("msk", [2, 128, 128])
        self.zT = {L: I("zT_l", [33, L]), CL: I("zT_c", [33, CL])}
        self.negt = {L: I("negt_l", [128, L // 128]), CL: I("negt_c", [128, CL // 128])}
        self.delta = I("delta", [1, D])
        self.cf_c = I("cf_c", [3 * 128, 2 * 128])
        self.sf_c = I("sf_c", [3 * 128, 2 * 128])
        self.ci_c = I("ci_c", [128, 2 * 256])
        self.nsi_c = I("nsi_c", [128, 2 * 256])
        self.cin = {L: I("cin_l", [1, L]), CL: I("cin_c", [1, CL])}
        self.wn = {L: I("wn_l", [128, 17]), CL: I("wn_c", [128, 3])}
        if self.debug_dump is None:
            self.out = k.dram("out", [L, D], F32, kind="ExternalOutput", bs=128)
        else:
            self.out = k.dram("out", list(self.debug_dump[1]), F32, kind="ExternalOutput")
        self.X = S("X", [NT + 128, D], bs=128)
        self.MOD = S("MOD", [DEPTH * 2, 6 * D])
        self.HT = S("HT", [D, NT])
        self.H = S("H", [NT + 128, D])
        self.QT = S("QT", [1024, NT])
        self.KT = S("KT", [1024, NT])
        self.V = S("V", [NT, D])
        self.G = S("G", [NT, D])
        self.U = S("U", [NT, D])
        self.OG = S("OG", [NT, D])
        self.X0T = S("X0T", [D, NT])
        self.UT = S("UT", [D, NT])
        self.UTOK = S("UTOK", [NT, D])
        self.HS = S("HS", [NT, D])
        self.HD = S("HD", [NT, D])
        FR = 17 * 128 + 3 * 128
        self.KRE = S("KRE", [FR, D])
        self.KIM = S("KIM", [FR, D])
        self.URE = S("URE", [FR, D])
        self.UIM = S("UIM", [FR, D])
        self.YHT = S("YHT", [D, NT])
        self.RN = S("RN", [2 * 128, 16])
        self.IDXD = S("IDXD", [3 * 128, NE], I32)
        self.TSD = S("TSD", [3 * 128, NE])

    def wsl(self, name, unit):
        C, rpu, nu, CH = SHARDED[name]
        if self.direct:
            r0 = self.units[name].index(unit) * rpu
            return self.wts[name].ap()[r0:r0 + rpu, :], []
        gu = GROUP.get(name, nu)
        t = self.wts[name][unit // gu]
        r0 = (unit % gu) * rpu
        return t.ap()[r0:r0 + rpu, :], t.rows(r0, r0 + rpu)

    def ph_gather(self):
        k = self.k
        k.begin()
        order = []
        moe = lambda l: [(n, 4 * l + c) for c in range(4) for n in ("moe_w1", "moe_w3", "moe_w2")]
        order += [("w_ada", 0), ("w_ada", 1), ("gla_w_in", 0), ("gla_w_out", 0)] + moe(0)
        order += [("w_ada", 2), ("w_ada", 3), ("hy_w_in", 0), ("cf_l", 0), ("sf_l", 0), ("ci_l", 0), ("nsi_l", 0),
                  ("hy_w_out", 0)] + moe(1)
        order += [("w_ada", 4), ("w_ada", 5), ("gla_w_in", 1), ("gla_w_out", 1)] + moe(2)
        order += [("w_ada", 6), ("w_ada", 7), ("hy_w_in", 1), ("hy_w_out", 1)] + moe(3)
        qn = 0
        for (n, c) in order:
            C, rpu, nu, CH = SHARDED[n]
            sh, bn = self.wsh[n], self.wbn[n]
            grows = GROUP.get(n, nu) * rpu
            gt = self.wts[n][(c * CH) // grows]
            g0 = (c * CH) % grows
            pr = CH // NCORES
            qn += 1
            bres = Res()
            k.dma("sp", bn[c * pr:(c + 1) * pr, :], sh[c * pr:(c + 1) * pr, :], r=[], w=[bres])
            k.coll(lambda e, bn=bn, gt=gt, c=c, pr=pr, CH=CH, g0=g0: e.collective_compute(
                "AllGather", ALU.bypass, replica_groups=[list(range(NCORES))],
                ins=[bn.ap()[c * pr:(c + 1) * pr, :]], outs=[gt.ap()[g0:g0 + CH, :]]),
                r=[bres], w=gt.rows(g0, g0 + CH))
        k.end()

    def mm(self, ps, lhsT, rhs, start, stop, r, w):
        self.k.op("pe", lambda e: e.matmul(ps, lhsT, rhs, start=start, stop=stop), r=r, w=w)

    def tr(self, ps, in_, ident, r, w):
        self.k.op("pe", lambda e: e.transpose(ps, in_, ident), r=r, w=w)

    def act(self, out, in_, func, r, w, bias=None, scale=None, accum=None):
        kw = {}
        if bias is not None:
            kw["bias"] = bias
        if scale is not None:
            kw["scale"] = scale
        if accum is not None:
            kw["accum_out"] = accum
        self.k.op("act", lambda e: e.activation(out, in_, func, **kw), r=r, w=w)

    def cp(self, eng, out, in_, r, w):
        if eng == "act":
            self.k.op("act", lambda e: e.copy(out, in_), r=r, w=w)
        else:
            self.k.op(eng, lambda e: e.tensor_copy(out, in_), r=r, w=w)

    def evac(self, out, in_, r, w):
        self.evn += 1
        self.cp("act" if self.evn % 2 else "dve", out, in_, r, w)

    def tt(self, eng, out, a, b, op, r, w):
        self.k.op(eng, lambda e: e.tensor_tensor(out=out, in0=a, in1=b, op=op), r=r, w=w)

    def ts(self, eng, out, in0, s1, s2, op0, op1, r, w):
        if s2 is None:
            self.k.op(eng, lambda e: e.tensor_scalar(out, in0, s1, None, op0=op0), r=r, w=w)
        else:
            self.k.op(eng, lambda e: e.tensor_scalar(out, in0, s1, s2, op0=op0, op1=op1), r=r, w=w)

    def stt(self, eng, out, in0, scalar, in1, op0, op1, r, w):
        self.k.op(eng, lambda e: e.scalar_tensor_tensor(out=out, in0=in0, scalar=scalar, in1=in1, op0=op0, op1=op1),
                  r=r, w=w)

    def memset(self, eng, ap, val, w):
        self.k.op(eng, lambda e: e.memset(ap, val), w=w)

    def recip(self, out, in_, r, w):
        self.k.op("dve", lambda e: e.reciprocal(out, in_), r=r, w=w)

    def load(self, q, out, in_, r, w, **kw):
        self.k.dma(q, out, in_, r=r, w=w, **kw)

    def bcload(self, tile_, dram_row_ap, r, npart=128):
        self.k.dma("pool", tile_[0:npart], dram_row_ap.partition_broadcast(npart), r=r, w=[tile_])

    def psums(self, n=8):
        return Rot([self.k.psum(name="ps%d" % i) for i in range(n)])

    def rstd(self, ss, rs, n, r):
        self.act(rs, ss, AF.Sqrt, r=r, w=r, scale=1.0 / n, bias=EPS)
        self.recip(rs, rs, r=r, w=r)

    def ph_init(self):
        k = self.k
        k.begin()
        k.dma("sp", self.X[0:L, :], self.x_in.ap(), r=[self.x_in], w=self.X.rows(0, L))
        k.dma("act", self.X[L:NT, :], self.ctx_in.ap(), r=[self.ctx_in], w=self.X.rows(L, NT))
        k.end()

    def ph_mods(self, i):
        k = self.k
        k.begin()
        ps = self.psums(4)
        cct = k.tile([128, 32], name="cct")
        sc = k.tile([128, 32], F32R, name="sc")
        self.load("sp", cct[:], self.cc.ap(), r=[self.cc], w=[cct])
        self.act(sc[:], cct[:], AF.Silu, r=[cct], w=[sc])
        scv = sc[:].rearrange("p (kc r) -> p kc r", r=2)
        wp = Rot([k.tile([128, 16, 512], F32R, name="wada") for _ in range(3)])
        bp = Rot([k.tile([2, 512], name="bada") for _ in range(2)])
        mp = Rot([k.tile([2, 512], name="modo") for _ in range(2)])
        if True:
            wap, wres = self.wsl("w_ada", i)
            wv = wap.rearrange("(kc p) n -> p kc n", p=128)
            for nb in range(24):
                wt = wp.next()
                bt = bp.next()
                mt = mp.next()
                self.load("sp", wt[:], wv[:, :, nb * 512:(nb + 1) * 512].bitcast(F32R),
                          r=wres, w=[wt])
                self.bcload(bt, self.b_ada[i:i + 1, nb * 512:(nb + 1) * 512], r=[self.b_ada], npart=2)
                p = ps.next()
                for kc in range(16):
                    self.mm(p[0:2, :], scv[:, kc, :], wt[:, kc, :], kc == 0, kc == 15, r=[sc, wt], w=[p])
                self.tt("dve", mt[:], p[0:2, :], bt[:], ALU.add, r=[p, bt], w=[mt])
                k.dma("pool", self.MOD[2 * i:2 * i + 2, nb * 512:(nb + 1) * 512], mt[:], r=[mt], w=[self.MOD])
        k.end()

    def mod_row(self, i, r, chunk):
        return self.MOD[2 * i + r:2 * i + r + 1, chunk * D:(chunk + 1) * D]

    def tok_tiles(self, with_ctx):
        return list(range(18 if with_ctx else 16))

    def blocks(self, with_ctx):
        b = [(t0, 512) for t0 in range(0, L, 512)]
        if with_ctx:
            b.append((L, CL))
        return b

    def ph_norm(self, i, kind, with_ctx):
        k = self.k
        k.begin()
        ps = self.psums(6)
        ident = k.tile([128, 128], name="ident")
        self.load("sp", ident[:], self.ident.ap(), r=[self.ident], w=[ident])
        gsrc = self.norm_mix if kind == "mix" else self.norm_ffn
        shc, scc = (0, 1) if kind == "mix" else (3, 4)
        gam = k.tile([128, D], name="gam")
        self.bcload(gam, gsrc[i:i + 1, :], r=[gsrc])
        gm, sh = [], []
        for r_ in range(2 if with_ctx else 1):
            g_ = k.tile([128, D], name="gm")
            s_ = k.tile([128, D], name="sh")
            self.bcload(g_, self.mod_row(i, r_, scc), r=[self.MOD])
            self.stt("dve", g_[:], g_[:], 1.0, gam[:], ALU.add, ALU.mult, r=[g_, gam], w=[g_])
            self.bcload(s_, self.mod_row(i, r_, shc), r=[self.MOD])
            gm.append(g_)
            sh.append(s_)
        xp = Rot([k.tile([128, D], name="xt") for _ in range(2)])
        hp = Rot([k.tile([128, D], name="ht") for _ in range(2)])
        junk = k.tile([128, D], name="junk")
        ssp = Rot([k.tile([128, 2], name="ss") for _ in range(3)])
        if kind == "mix":
            hTb = Rot([k.tile([128, 16, 512], name="hTb") for _ in range(2)])
        else:
            hTt = Rot([k.tile([128, 16, 128], name="hTt") for _ in range(2)])
            wr = k.tile([128, 16, NE], name="wr")
            self.load("sp", wr[:], self.moe_router[i].rearrange("(kc p) e -> p kc e", p=128),
                      r=[self.moe_router], w=[wr])
            ST = k.tile([16, NT], name="ST")
            smp = Rot([k.tile([128, 40], name="sm") for _ in range(2)])
        tiles = self.tok_tiles(with_ctx)
        cur = None
        for tt_ in tiles:
            r_ = 1 if tt_ >= 16 else 0
            r0 = tt_ * 128
            xt = xp.next()
            ht = hp.next()
            ss = ssp.next()
            self.load("sp", xt[:], self.X[r0:r0 + 128, :], r=self.X.rows(r0, r0 + 128), w=[xt])
            self.memset("pool", ss[:], 0.0, w=[ss])
            self.act(junk[:], xt[:], AF.Square, r=[xt], w=[junk, ss], accum=ss[:, 0:1])
            self.rstd(ss[:, 0:1], ss[:, 1:2], D, r=[ss])
            self.stt("dve", ht[:], xt[:], ss[:, 1:2], gm[r_][:], ALU.mult, ALU.mult, r=[xt, ss, gm[r_]], w=[ht])
            self.tt("pool", ht[:], ht[:], sh[r_][:], ALU.add, r=[ht, sh[r_]], w=[ht])
            if kind == "mix":
                bi = tt_ // 4
                tl = tt_ % 4
                if tl == 0:
                    cur = hTb.next()
                for g in range(4):
                    p = ps.next()
                    for q in range(4):
                        kc = g * 4 + q
                        self.tr(p[:, q * 128:(q + 1) * 128], ht[:, kc * 128:(kc + 1) * 128], ident[:],
                                r=[ht, ident], w=[p])
                    self.evac(cur[:, g * 4:(g + 1) * 4, tl * 128:(tl + 1) * 128],
                              p[:].rearrange("p (a b) -> p a b", b=128), r=[p], w=[cur])
                last_in_block = (tl == 3) or (tt_ == tiles[-1])
                if last_in_block:
                    t0 = bi * 512
                    nt = (tl + 1) * 128
                    k.dma("pool", self.HT.ap().rearrange("(kc p) t -> p kc t", p=128)[:, :, t0:t0 + nt],
                          cur[:, :, 0:nt], r=[cur], w=[self.HT])
            else:
                k.dma("pool", self.H[r0:r0 + 128, :], ht[:], r=[ht], w=[self.H])
                hT = hTt.next()
                for g in range(4):
                    p = ps.next()
                    for q in range(4):
                        kc = g * 4 + q
                        self.tr(p[:, q * 128:(q + 1) * 128], ht[:, kc * 128:(kc + 1) * 128], ident[:],
                                r=[ht, ident], w=[p])
                    self.evac(hT[:, g * 4:(g + 1) * 4, :], p[:].rearrange("p (a b) -> p a b", b=128), r=[p], w=[hT])
                p = ps.next()
                for kc in range(16):
                    self.mm(p[:, 0:NE], hT[:, kc, :], wr[:, kc, :], kc == 0, kc == 15, r=[hT, wr], w=[p])
                sm = smp.next()
                k.op("dve", lambda e, sm=sm, p=p: e.tensor_reduce(out=sm[:, 32:33], in_=p[:, 0:NE], axis=AX.X,
                                                                  op=ALU.max), r=[p], w=[sm])
                self.ts("dve", sm[:, 33:34], sm[:, 32:33], -1.0, None, ALU.mult, None, r=[sm], w=[sm])
                self.memset("pool", sm[:, 34:35], 0.0, w=[sm])
                self.act(sm[:, 0:NE], p[:, 0:NE], AF.Exp, r=[p, sm], w=[sm], bias=sm[:, 33:34], accum=sm[:, 34:35])
                self.recip(sm[:, 35:36], sm[:, 34:35], r=[sm], w=[sm])
                self.ts("dve", sm[:, 16:32], sm[:, 0:NE], sm[:, 35:36], None, ALU.mult, None, r=[sm], w=[sm])
                p2 = ps.next()
                self.tr(p2[0:NE, 0:128], sm[:, 16:32], ident[:], r=[sm, ident], w=[p2])
                self.evac(ST[:, r0:r0 + 128], p2[0:NE, 0:128], r=[p2], w=[ST])
        if kind == "ffn":
            self.topk(ST, ident, ps, with_ctx)
        k.end()

    def topk(self, ST, ident, ps, with_ctx):
        k = self.k
        work = k.tile([16, L], name="work")
        vals = k.tile([16, 384], name="vals")
        idxu = k.tile([16, 384], U32, name="idxu")
        idxf = k.tile([16, 384], name="idxf")
        self.memset("pool", vals[:], 0.0, w=[vals])
        self.load("sp", idxf[:, 256:384], self.padf.ap(), r=[], w=[idxf])
        self.cp("dve", work[:], ST[:, 0:L], r=[ST], w=[work])

        def run(wk, n, off):
            for it in range(n // 8):
                sl = slice(off + it * 8, off + it * 8 + 8)
                k.op("dve", lambda e, sl=sl: e.max(out=vals[:, sl], in_=wk), r=[work], w=[vals])
                k.op("dve", lambda e, sl=sl: e.max_index(out=idxu[:, sl], in_max=vals[:, sl], in_values=wk),
                     r=[work, vals], w=[idxu])
                k.op("dve", lambda e, sl=sl: e.match_replace(out=wk, in_to_replace=vals[:, sl], in_values=wk,
                                                             imm_value=-1.0), r=[work, vals], w=[work])

        run(work[:, 0:L], 256, 0)
        self.cp("dve", idxf[:, 0:256], idxu[:, 0:256], r=[idxu], w=[idxf])
        if with_ctx:
            self.cp("dve", work[:, 0:CL], ST[:, L:NT], r=[ST, work], w=[work])
            run(work[:, 0:CL], 32, 256)
            self.cp("dve", idxf[:, 256:288], idxu[:, 256:288], r=[idxu], w=[idxf])
            self.ts("dve", idxf[:, 256:288], idxf[:, 256:288], float(L), None, ALU.add, None, r=[idxf], w=[idxf])
        for c in range(3):
            p = ps.next()
            self.tr(p[:, 0:NE], idxf[:, c * 128:(c + 1) * 128], ident[0:16, 0:16], r=[idxf, ident], w=[p])
            it_ = k.tile([128, NE], I32, name="idxi")
            self.cp("dve", it_[:], p[:, 0:NE], r=[p], w=[it_])
            k.dma("pool", self.IDXD[c * 128:(c + 1) * 128, :], it_[:], r=[it_], w=[self.IDXD])
            p = ps.next()
            self.tr(p[:, 0:NE], vals[:, c * 128:(c + 1) * 128], ident[0:16, 0:16], r=[vals, ident], w=[p])
            vt = k.tile([128, NE], name="valt")
            self.cp("dve", vt[:], p[:, 0:NE], r=[p], w=[vt])
            k.dma("pool", self.TSD[c * 128:(c + 1) * 128, :], vt[:], r=[vt], w=[self.TSD])

    def ph_experts(self, i, with_ctx):
        k = self.k
        k.begin()
        ps = self.psums(8)
        ident = k.tile([128, 128], name="ident")
        self.load("sp", ident[:], self.ident.ap(), r=[self.ident], w=[ident])
        nct = 3 if with_ctx else 2
        NC_ = 288 if with_ctx else 256
        idx = [k.tile([128, NE], I32, name="idx") for _ in range(nct)]
        tsv = [k.tile([128, NE], name="tsv") for _ in range(nct)]
        for c in range(nct):
            self.load("sp", idx[c][:], self.IDXD[c * 128:(c + 1) * 128, :], r=[self.IDXD], w=[idx[c]])
            self.load("sp", tsv[c][:], self.TSD[c * 128:(c + 1) * 128, :], r=[self.TSD], w=[tsv[c]])
        g2 = []
        for r_ in range(2 if with_ctx else 1):
            g_ = k.tile([128, D], name="g2")
            self.bcload(g_, self.mod_row(i, r_, 5), r=[self.MOD])
            g2.append(g_)
        xs = [k.tile([128, D], name="xs") for _ in range(nct)]
        yo = [k.tile([128, D], name="yo") for _ in range(nct)]
        xsT = k.tile([128, 16, NC_], F32R, name="xsT")
        aT = k.tile([128, 8, NC_], F32R, name="aT")
        tmp = Rot([k.tile([128, NC_], name="tmp") for _ in range(2)])
        wp = Rot([k.tile([128, 4096], F32R, name="wexp") for _ in range(5)])
        csz = [128, 128, 32]
        if with_ctx:
            self.memset("pool", xs[2][:], 0.0, w=[xs[2]])
            self.memset("pool", yo[2][:], 0.0, w=[yo[2]])
        qi = 0

        def gather(e):
            for c in range(nct):
                k.dma("pool", None, None, r=[idx[c], self.H], w=[xs[c]],
                      fn=lambda eng, c=c, e=e: eng.indirect_dma_start(
                          out=xs[c][:], out_offset=None, in_=self.H.ap(),
                          in_offset=bass.IndirectOffsetOnAxis(ap=idx[c][:, e:e + 1], axis=0)))

        gather(0)
        for e in range(NE):
            for kc in range(16):
                p = ps.next()
                for c in range(nct):
                    if c < 2:
                        self.tr(p[:, c * 128:(c + 1) * 128], xs[c][:, kc * 128:(kc + 1) * 128], ident[:],
                                r=[xs[c], ident], w=[p])
                    else:
                        self.tr(p[:, 256:288], xs[c][0:32, kc * 128:(kc + 1) * 128], ident[0:32, 0:32],
                                r=[xs[c], ident], w=[p])
                self.evac(xsT[:, kc, :], p[:, 0:NC_], r=[p], w=[xsT])
            if e + 1 < NE:
                gather(e + 1)
            w1ap, w1res = self.wsl("moe_w1", i * NE + e)
            w3ap, w3res = self.wsl("moe_w3", i * NE + e)
            w2ap, w2res = self.wsl("moe_w2", i * NE + e)
            w1v = w1ap.rearrange("(kc p) f -> p kc f", p=128)
            w3v = w3ap.rearrange("(kc p) f -> p kc f", p=128)
            w2v = w2ap.rearrange("(fc p) d -> p fc d", p=128)
            for fq in range(4):
                w1q = wp.next()
                w3q = wp.next()
                w1a = w1q[:].rearrange("p (kc f) -> p kc f", f=256)
                w3a = w3q[:].rearrange("p (kc f) -> p kc f", f=256)
                qi += 1
                self.load("sp", w1a, w1v[:, :, fq * 256:(fq + 1) * 256].bitcast(F32R),
                          r=w1res, w=[w1q])
                self.load("sp", w3a, w3v[:, :, fq * 256:(fq + 1) * 256].bitcast(F32R),
                          r=w3res, w=[w3q])
                for fl in range(2):
                    fc = fq * 2 + fl
                    p1 = ps.next()
                    p3 = ps.next()
                    for kc in range(16):
                        self.mm(p1[:, 0:NC_], w1a[:, kc, fl * 128:(fl + 1) * 128], xsT[:, kc, :], kc == 0, kc == 15,
                                r=[w1q, xsT], w=[p1])
                    for kc in range(16):
                        self.mm(p3[:, 0:NC_], w3a[:, kc, fl * 128:(fl + 1) * 128], xsT[:, kc, :], kc == 0, kc == 15,
                                r=[w3q, xsT], w=[p3])
                    t_ = tmp.next()
                    self.act(t_[:], p1[:, 0:NC_], AF.Silu, r=[p1], w=[t_])
                    self.tt("dve", aT[:, fc, :], t_[:], p3[:, 0:NC_], ALU.mult, r=[t_, p3], w=[aT])
            for dq in range(4):
                w2q = wp.next()
                w2a = w2q[:].rearrange("p (fc d) -> p fc d", d=512)
                qi += 1
                self.load("sp", w2a, w2v[:, :, dq * 512:(dq + 1) * 512].bitcast(F32R),
                          r=w2res, w=[w2q])
                for c in range(nct):
                    cs_ = csz[c]
                    p = ps.next()
                    for fc in range(8):
                        self.mm(p[0:cs_, :], aT[:, fc, c * 128:c * 128 + cs_], w2a[:, fc, :], fc == 0, fc == 7,
                                r=[aT, w2q], w=[p])
                    g_ = g2[1 if c == 2 else 0]
                    self.stt("dve", yo[c][0:cs_, dq * 512:(dq + 1) * 512], p[0:cs_, :], tsv[c][0:cs_, e:e + 1],
                             g_[0:cs_, dq * 512:(dq + 1) * 512], ALU.mult, ALU.mult, r=[p, tsv[c], g_], w=[yo[c]])
            for c in range(nct):
                k.dma("pool", None, None, r=[idx[c], yo[c]], w=[self.X],
                      fn=lambda eng, c=c, e=e: eng.indirect_dma_start(
                          out=self.X.ap(), out_offset=bass.IndirectOffsetOnAxis(ap=idx[c][:, e:e + 1], axis=0),
                          in_=yo[c][:], in_offset=None, compute_op=ALU.add))
        k.end()

    def ph_gla_proj(self, j, with_ctx=True):
        k = self.k
        k.begin()
        ps = self.psums(8)
        hTp = Rot([k.tile([128, 16, 512], F32R, name="hT") for _ in range(2)])
        wp = Rot([k.tile([128, 16, 512], F32R, name="w") for _ in range(2)])
        op_ = Rot([k.tile([128, 512], name="o") for _ in range(4)])
        wa1 = k.tile([128, 16, 32], F32R, name="wa1")
        wa2 = k.tile([33, 2048], name="wa2")
        lowT = k.tile([33, 512], name="lowT")
        self.load("sp", wa1[:], self.gla_wa1[j].rearrange("(kc p) r -> p kc r", p=128).bitcast(F32R),
                  r=[self.gla_wa1], w=[wa1])
        self.load("sp", wa2[:], self.gla_wa2[j], r=[self.gla_wa2], w=[wa2])
        self.memset("pool", lowT[:], 1.0, w=[lowT])
        wap, wres = self.wsl("gla_w_in", j)
        wv = wap.rearrange("(kc p) n -> p kc n", p=128)
        HTv = self.HT.ap().rearrange("(kc p) t -> p kc t", p=128)
        qn = 0
        for (t0, TB) in self.blocks(with_ctx):
            hT = hTp.next()
            self.load("sp", hT[:, :, 0:TB], HTv[:, :, t0:t0 + TB].bitcast(F32R), r=[self.HT], w=[hT])
            for nb in range(12):
                wt = wp.next()
                qn += 1
                self.load("sp", wt[:], wv[:, :, nb * 512:(nb + 1) * 512].bitcast(F32R),
                          r=wres, w=[wt])
                if nb < 4:
                    dst = self.QT if nb < 2 else self.KT
                    for nl in range(4):
                        p = ps.next()
                        for kc in range(16):
                            self.mm(p[:, 0:TB], wt[:, kc, nl * 128:(nl + 1) * 128], hT[:, kc, 0:TB], kc == 0, kc == 15,
                                    r=[wt, hT], w=[p])
                        o = op_.next()
                        if nb < 2:
                            self.act(o[:, 0:TB], p[:, 0:TB], AF.Copy, r=[p], w=[o], scale=DK ** -0.5)
                        else:
                            self.evac(o[:, 0:TB], p[:, 0:TB], r=[p], w=[o])
                        f0 = ((nb % 2) * 4 + nl) * 128
                        k.dma("pool", dst[f0:f0 + 128, t0:t0 + TB], o[:, 0:TB], r=[o], w=[dst])
                else:
                    dst = self.V if nb < 8 else self.G
                    c0 = ((nb - 4) % 4) * 512
                    for tl in range(TB // 128):
                        p = ps.next()
                        for kc in range(16):
                            self.mm(p[:], hT[:, kc, tl * 128:(tl + 1) * 128], wt[:, kc, :], kc == 0, kc == 15,
                                    r=[wt, hT], w=[p])
                        o = op_.next()
                        if nb < 8:
                            self.evac(o[:], p[:], r=[p], w=[o])
                        else:
                            self.act(o[:], p[:], AF.Silu, r=[p], w=[o])
                        r0 = t0 + tl * 128
                        k.dma("pool", dst[r0:r0 + 128, c0:c0 + 512], o[:], r=[o], w=[dst])
            p = ps.next()
            for kc in range(16):
                self.mm(p[0:32, 0:TB], wa1[:, kc, :], hT[:, kc, 0:TB], kc == 0, kc == 15, r=[wa1, hT], w=[p])
            self.evac(lowT[0:32, 0:TB], p[0:32, 0:TB], r=[p], w=[lowT])
            for tl in range(TB // 128):
                for cb in range(4):
                    p = ps.next()
                    self.mm(p[:], lowT[0:33, tl * 128:(tl + 1) * 128], wa2[0:33, cb * 512:(cb + 1) * 512], True, True,
                            r=[lowT, wa2], w=[p])
                    o = op_.next()
                    self.act(o[:], p[:], AF.Exp, r=[p], w=[o], scale=-1.0)
                    self.act(o[:], o[:], AF.Ln, r=[o], w=[o], bias=1.0)
                    r0 = t0 + tl * 128
                    k.dma("pool", self.U[r0:r0 + 128, cb * 512:(cb + 1) * 512], o[:], r=[o], w=[self.U])
        k.end()

    def ph_gla_scan(self, j):
        k = self.k
        k.begin()
        ps = self.psums(8)
        ident = k.tile([128, 128], name="ident")
        self.load("sp", ident[:], self.ident.ap(), r=[self.ident], w=[ident])
        tri = [k.tile([128, 128], name="tri") for _ in range(2)]
        msk = [k.tile([128, 128], name="msk") for _ in range(2)]
        for z in range(2):
            self.load("sp", tri[z][:], self.tri[z], r=[self.tri], w=[tri[z]])
            self.load("sp", msk[z][:], self.msk[z], r=[self.msk], w=[msk[z]])
        hn = k.tile([128, DV], name="hn")
        self.bcload(hn, self.gla_hn[j:j + 1, :], r=[self.gla_hn])
        oacc = k.tile([128, 18, DV], name="oacc")
        S = [k.tile([128, DV], F32R, name="S") for _ in range(2)]
        zer = k.tile([128, DV], name="zer")
        self.memset("pool", zer[:], 0.0, w=[zer])
        qp = Rot([k.tile([128, 2, 128], name="q") for _ in range(3)])
        kp = Rot([k.tile([128, 2, 128], name="k") for _ in range(3)])
        vp = Rot([k.tile([128, DV], F32R, name="v") for _ in range(3)])
        up = Rot([k.tile([128, 256], name="u") for _ in range(3)])
        ebp = Rot([k.tile([128, 256], name="eb") for _ in range(2)])
        enp = Rot([k.tile([128, 256], name="en") for _ in range(2)])
        qsp = Rot([k.tile([128, 2, 128], F32R, name="qs") for _ in range(2)])
        ksp = Rot([k.tile([128, 2, 128], F32R, name="ks") for _ in range(2)])
        ktp = Rot([k.tile([128, 256], F32R, name="kt") for _ in range(2)])
        atp = Rot([k.tile([128, 128], F32R, name="at") for _ in range(2)])
        tsp = Rot([k.tile([128, DV], name="tS") for _ in range(2)])
        gp = Rot([k.tile([128, DV], name="g") for _ in range(2)])
        onp = Rot([k.tile([128, DV], name="on") for _ in range(2)])
        junk = k.tile([128, DV], name="junk")
        ssp = Rot([k.tile([128, 2], name="ss") for _ in range(3)])
        QTv = self.QT.ap().rearrange("(c p) t -> p c t", p=128)
        KTv = self.KT.ap().rearrange("(c p) t -> p c t", p=128)
        for h in range(HEADS):
            for z in range(2):
                order = [16, 17] + list(range(16)) if z == 0 else [17, 16] + list(range(15, -1, -1))
                last = 127 if z == 0 else 0
                for dc in range(2):
                    self.cp("dve", S[dc][:], zer[:], r=[zer], w=[S[dc]])
                for ti in order:
                    r0 = ti * 128
                    q_ = qp.next()
                    k_ = kp.next()
                    v_ = vp.next()
                    u_ = up.next()
                    self.load("sp", q_[:], QTv[:, 2 * h:2 * h + 2, r0:r0 + 128], r=[self.QT], w=[q_])
                    self.load("sp", k_[:], KTv[:, 2 * h:2 * h + 2, r0:r0 + 128], r=[self.KT], w=[k_])
                    self.load("sp", v_[:], self.V[r0:r0 + 128, h * DV:(h + 1) * DV].bitcast(F32R), r=[self.V], w=[v_])
                    c0 = z * 1024 + h * DK
                    self.load("sp", u_[:], self.U[r0:r0 + 128, c0:c0 + DK], r=[self.U], w=[u_])
                    pb = ps.next()
                    for dc in range(2):
                        self.mm(pb[:, dc * 128:(dc + 1) * 128], u_[:, dc * 128:(dc + 1) * 128], tri[z][:], True, True,
                                r=[u_, tri[z]], w=[pb])
                    eb = ebp.next()
                    en = enp.next()
                    self.act(eb[:], pb[:, 0:256], AF.Exp, r=[pb], w=[eb])
                    self.act(en[:], pb[:, 0:256], AF.Exp, r=[pb], w=[en], scale=-1.0)
                    qs = qsp.next()
                    ks = ksp.next()
                    self.tt("dve", qs[:], q_[:], eb[:].rearrange("p (c t) -> p c t", t=128), ALU.mult, r=[q_, eb], w=[qs])
                    self.tt("pool", ks[:], k_[:], en[:].rearrange("p (c t) -> p c t", t=128), ALU.mult, r=[k_, en], w=[ks])
                    pt = ps.next()
                    for dc in range(2):
                        self.tr(pt[:, dc * 128:(dc + 1) * 128], ks[:, dc, :].bitcast(F32), ident[:], r=[ks, ident], w=[pt])
                    kt = ktp.next()
                    self.evac(kt[:], pt[:, 0:256], r=[pt], w=[kt])
                    pa = ps.next()
                    for dc in range(2):
                        self.mm(pa[:, 0:128], ks[:, dc, :], qs[:, dc, :], dc == 0, dc == 1, r=[ks, qs], w=[pa])
                    at = atp.next()
                    self.tt("dve", at[:], pa[:, 0:128], msk[z][:], ALU.mult, r=[pa, msk[z]], w=[at])
                    po = ps.next()
                    self.mm(po[:], at[:], v_[:], True, False, r=[at, v_], w=[po])
                    for dc in range(2):
                        self.mm(po[:], qs[:, dc, :], S[dc][:], False, dc == 1, r=[qs, S[dc]], w=[po])
                    if z == 0:
                        self.evac(oacc[:, ti, :], po[:], r=[po], w=[oacc])
                    else:
                        self.tt("dve", oacc[:, ti, :], oacc[:, ti, :], po[:], ALU.add, r=[oacc, po], w=[oacc])
                    for dc in range(2):
                        p_s = ps.next()
                        self.mm(p_s[:], kt[:, dc * 128:(dc + 1) * 128], v_[:], True, True, r=[kt, v_], w=[p_s])
                        tS = tsp.next()
                        self.tt("dve", tS[:], S[dc][:].bitcast(F32), p_s[:], ALU.add, r=[S[dc], p_s], w=[tS])
                        col = dc * 128 + last
                        self.act(S[dc][:], tS[:], AF.Copy, r=[tS, eb], w=[S[dc]], scale=eb[:, col:col + 1])
            for ti in range(18):
                r0 = ti * 128
                ss = ssp.next()
                g_ = gp.next()
                on = onp.next()
                self.load("sp", g_[:], self.G[r0:r0 + 128, h * DV:(h + 1) * DV], r=[self.G], w=[g_])
                self.memset("pool", ss[:], 0.0, w=[ss])
                self.act(junk[:], oacc[:, ti, :], AF.Square, r=[oacc], w=[junk, ss], accum=ss[:, 0:1])
                self.rstd(ss[:, 0:1], ss[:, 1:2], DV, r=[ss])
                self.stt("dve", on[:], oacc[:, ti, :], ss[:, 1:2], hn[:], ALU.mult, ALU.mult, r=[oacc, ss, hn], w=[on])
                self.tt("pool", on[:], on[:], g_[:], ALU.mult, r=[on, g_], w=[on])
                k.dma("pool", self.OG[r0:r0 + 128, h * DV:(h + 1) * DV], on[:], r=[on], w=[self.OG])
        k.end()

    def ph_outproj(self, i, wname, wunit, src, src_major, with_ctx, bias_row=None):
        k = self.k
        k.begin()
        ps = self.psums(8)
        ident = k.tile([128, 128], name="ident")
        self.load("sp", ident[:], self.ident.ap(), r=[self.ident], w=[ident])
        g1 = []
        for r_ in range(2 if with_ctx else 1):
            g_ = k.tile([128, D], name="g1")
            self.bcload(g_, self.mod_row(i, r_, 2), r=[self.MOD])
            g1.append(g_)
        bo = None
        if bias_row is not None:
            bo = k.tile([128, D], name="bo")
            self.bcload(bo, bias_row, r=[self.hy_b_out])
        sT = k.tile([128, 16, 512], F32R, name="sT")
        wp = Rot([k.tile([128, 16, 512], F32R, name="w") for _ in range(2)])
        xt = [k.tile([128, D], name="xt") for _ in range(4)]
        sp_ = Rot([k.tile([128, D], name="st") for _ in range(2)])
        tp = Rot([k.tile([128, 512], name="tp") for _ in range(3)])
        wap, wres = self.wsl(wname, wunit)
        wv = wap.rearrange("(kc p) n -> p kc n", p=128)
        qn = 0
        for (t0, TB) in self.blocks(with_ctx):
            r_ = 1 if t0 >= L else 0
            ntl = TB // 128
            if src_major == "tok":
                for tl in range(ntl):
                    st = sp_.next()
                    r0 = t0 + tl * 128
                    self.load("sp", st[:], src[r0:r0 + 128, :], r=[src], w=[st])
                    for g in range(4):
                        p = ps.next()
                        for q in range(4):
                            kc = g * 4 + q
                            self.tr(p[:, q * 128:(q + 1) * 128], st[:, kc * 128:(kc + 1) * 128], ident[:],
                                    r=[st, ident], w=[p])
                        self.evac(sT[:, g * 4:(g + 1) * 4, tl * 128:(tl + 1) * 128],
                                  p[:].rearrange("p (a b) -> p a b", b=128), r=[p], w=[sT])
            else:
                self.load("sp", sT[:, :, 0:TB],
                          src.ap().rearrange("(kc p) t -> p kc t", p=128)[:, :, t0:t0 + TB].bitcast(F32R),
                          r=[src], w=[sT])
            for tl in range(ntl):
                r0 = t0 + tl * 128
                self.load("sp", xt[tl][:], self.X[r0:r0 + 128, :], r=self.X.rows(r0, r0 + 128), w=[xt[tl]])
            for nb in range(4):
                wt = wp.next()
                qn += 1
                self.load("sp", wt[:], wv[:, :, nb * 512:(nb + 1) * 512].bitcast(F32R),
                          r=wres, w=[wt])
                for tl in range(ntl):
                    p = ps.next()
                    for kc in range(16):
                        self.mm(p[:], sT[:, kc, tl * 128:(tl + 1) * 128], wt[:, kc, :], kc == 0, kc == 15,
                                r=[sT, wt], w=[p])
                    t_ = tp.next()
                    sl = slice(nb * 512, (nb + 1) * 512)
                    if bo is not None:
                        self.tt("dve", t_[:], p[:], bo[:, sl], ALU.add, r=[p, bo], w=[t_])
                        self.tt("pool", t_[:], t_[:], g1[r_][:, sl], ALU.mult, r=[t_, g1[r_]], w=[t_])
                    else:
                        self.tt("dve", t_[:], p[:], g1[r_][:, sl], ALU.mult, r=[p, g1[r_]], w=[t_])
                    self.tt("pool", xt[tl][:, sl], xt[tl][:, sl], t_[:], ALU.add, r=[xt[tl], t_], w=[xt[tl]])
            for tl in range(ntl):
                r0 = t0 + tl * 128
                k.dma("pool", self.X[r0:r0 + 128, :], xt[tl][:], r=[xt[tl]], w=self.X.rows(r0, r0 + 128))
        k.end()

    def ph_hy_filter(self, j, Ls, row0, rn_row0):
        k = self.k
        k.begin()
        ps = self.psums(6)
        MT = Ls // 128
        zT = k.tile([33, Ls], name="zT")
        self.load("sp", zT[:], self.zT[Ls].ap(), r=[self.zT[Ls]], w=[zT])
        w1 = k.tile([33, 64], name="fw1")
        w2 = k.tile([64, 64], name="fw2")
        w3 = k.tile([64, 64], name="fw3")
        w4 = k.tile([64, 2 * D], name="fw4")
        fb = k.tile([64, 3], name="fb")
        fb2 = k.tile([64, 3], name="fb2")
        self.load("sp", w1[:], self.hy_f_w1[j], r=[self.hy_f_w1], w=[w1])
        self.load("sp", w2[:], self.hy_f_w2[j], r=[self.hy_f_w2], w=[w2])
        self.load("sp", w3[:], self.hy_f_w3[j], r=[self.hy_f_w3], w=[w3])
        self.load("sp", w4[:], self.hy_f_w4[j], r=[self.hy_f_w4], w=[w4])
        self.load("sp", fb[:], self.hy_fb[j], r=[self.hy_fb], w=[fb])
        OFF = 16.0
        self.ts("dve", fb2[:], fb[:], 1.0 / (2 * math.pi), 0.5 + OFF, ALU.mult, ALU.add, r=[fb], w=[fb2])
        hT = [k.tile([64, Ls], name="hid") for _ in range(3)]
        up = Rot([k.tile([64, 512], name="u") for _ in range(2)])
        uip = Rot([k.tile([64, 512], I32, name="ui") for _ in range(2)])
        ufp = Rot([k.tile([64, 512], name="uf") for _ in range(2)])
        SE = 1e-6
        srcs = [(zT, 33, w1), (hT[0], 64, w2), (hT[1], 64, w3)]
        for li in range(3):
            src, K_, w_ = srcs[li]
            for lb in range((Ls + 511) // 512):
                wdt = min(512, Ls - lb * 512)
                sl = slice(lb * 512, lb * 512 + wdt)
                p = ps.next()
                self.mm(p[0:64, 0:wdt], w_[0:K_, :], src[0:K_, sl], True, True, r=[w_, src], w=[p])
                u = up.next()
                ui = uip.next()
                uf = ufp.next()
                self.act(u[:, 0:wdt], p[0:64, 0:wdt], AF.Identity, r=[p, fb2], w=[u], scale=1.0 / (2 * math.pi),
                         bias=fb2[:, li:li + 1])
                self.cp("dve", ui[:, 0:wdt], u[:, 0:wdt], r=[u], w=[ui])
                self.cp("dve", uf[:, 0:wdt], ui[:, 0:wdt], r=[ui], w=[uf])
                self.tt("dve", u[:, 0:wdt], u[:, 0:wdt], uf[:, 0:wdt], ALU.subtract, r=[u, uf], w=[u])
                self.stt("dve", uf[:, 0:wdt], u[:, 0:wdt], 0.0, u[:, 0:wdt], ALU.is_lt, ALU.add, r=[u], w=[uf])
                self.act(hT[li][:, sl], uf[:, 0:wdt], AF.Sin, r=[uf], w=[hT[li]], scale=2 * math.pi * (1 - SE),
                         bias=-math.pi * (1 - SE))
        negt = k.tile([128, MT], name="negt")
        self.load("sp", negt[:], self.negt[Ls].ap(), r=[self.negt[Ls]], w=[negt])
        dl = k.tile([128, D], name="delta")
        self.bcload(dl, self.delta[0:1, :], r=[self.delta])
        ones = k.tile([128, 1], name="ones")
        self.memset("pool", ones[:], 1.0, w=[ones])
        absum = k.tile([128, D], name="absum")
        self.memset("pool", absum[:], 0.0, w=[absum])
        decp = Rot([k.tile([128, D], name="dec") for _ in range(2)])
        hfp = Rot([k.tile([128, D], name="hf") for _ in range(2)])
        hbp = Rot([k.tile([128, D], name="hb") for _ in range(2)])
        hsp = Rot([k.tile([128, D], name="hs") for _ in range(2)])
        hdp = Rot([k.tile([128, D], name="hd") for _ in range(2)])
        a1 = k.tile([128, D], name="a1")
        a2 = k.tile([128, D], name="a2")
        for mt in range(MT):
            dec = decp.next()
            hf = hfp.next()
            hb = hbp.next()
            hs = hsp.next()
            hd = hdp.next()
            self.act(dec[:], dl[:], AF.Exp, r=[dl, negt], w=[dec], scale=negt[:, mt:mt + 1])
            for nb in range(8):
                p = ps.next()
                self.mm(p[:], hT[2][0:64, mt * 128:(mt + 1) * 128], w4[0:64, nb * 512:(nb + 1) * 512], True, True,
                        r=[hT[2], w4], w=[p])
                dst = hf if nb < 4 else hb
                sl = slice((nb % 4) * 512, (nb % 4 + 1) * 512)
                self.tt("dve", dst[:, sl], p[:], dec[:, sl], ALU.mult, r=[p, dec], w=[dst])
            if mt == 0:
                self.memset("pool", hb[0:1, :], 0.0, w=[hb])
            self.tt("pool", hs[:], hf[:], hb[:], ALU.add, r=[hf, hb], w=[hs])
            self.tt("pool", hd[:], hf[:], hb[:], ALU.subtract, r=[hf, hb], w=[hd])
            r0 = row0 + mt * 128
            k.dma("pool", self.HS[r0:r0 + 128, :], hs[:], r=[hs], w=[self.HS])
            k.dma("pool", self.HD[r0:r0 + 128, :], hd[:], r=[hd], w=[self.HD])
            self.act(a1[:], hf[:], AF.Abs, r=[hf], w=[a1])
            self.act(a2[:], hb[:], AF.Abs, r=[hb], w=[a2])
            self.tt("dve", a1[:], a1[:], a2[:], ALU.add, r=[a1, a2], w=[a1])
            self.tt("dve", absum[:], absum[:], a1[:], ALU.add, r=[absum, a1], w=[absum])
        pn = ps.next()
        for dc in range(16):
            self.mm(pn[:, dc:dc + 1], absum[:, dc * 128:(dc + 1) * 128], ones[:, 0:1], True, True, r=[absum, ones], w=[pn])
        rn = k.tile([128, 16], name="rn")
        self.recip(rn[:], pn[:, 0:16], r=[pn], w=[rn])
        k.dma("pool", self.RN[rn_row0:rn_row0 + 128, :], rn[:], r=[rn], w=[self.RN])
        k.end()

    def ph_dft(self, A, a_row0, Ls, tname, OUT, out_row0, scale_wn):
        k = self.k
        k.begin()
        ps = self.psums(8)
        TC = Ls // 128
        FC = TC + 1
        wn = None
        if scale_wn:
            wn = k.tile([128, FC], name="wn")
            self.load("sp", wn[:], self.wn[Ls].ap(), r=[self.wn[Ls]], w=[wn])
        At = k.tile([128, TC, 1024], F32R, name="At")
        tbp = Rot([k.tile([128, TC, 128], F32R, name="tab") for _ in range(3)])
        otp = Rot([k.tile([128, 1024], name="ot") for _ in range(3)])
        Av = A.ap()[a_row0:a_row0 + Ls, :].rearrange("(tc p) d -> p tc d", p=128)
        qn = 0
        for db in range(2):
            self.load("sp", At[:], Av[:, :, db * 1024:(db + 1) * 1024].bitcast(F32R), r=[A], w=[At])
            for fc in range(FC):
                M = 128 if fc < TC else 1
                tab = tbp.next()
                qn += 1
                if Ls == L:
                    tap, tres = self.wsl(tname + "_l", fc)
                else:
                    tc_t = self.cf_c if tname == "cf" else self.sf_c
                    tap, tres = tc_t.ap()[fc * 128:(fc + 1) * 128, :], []
                self.load("sp", tab[:],
                          tap.rearrange("p (tc j) -> p tc j", j=128).bitcast(F32R), r=tres, w=[tab])
                ot = otp.next()
                for half in range(2):
                    p = ps.next()
                    for tc in range(TC):
                        self.mm(p[0:M, :], tab[:, tc, 0:M], At[:, tc, half * 512:(half + 1) * 512], tc == 0, tc == TC - 1,
                                r=[tab, At], w=[p])
                    if wn is not None:
                        self.act(ot[0:M, half * 512:(half + 1) * 512], p[0:M, :], AF.Copy, r=[p, wn], w=[ot],
                                 scale=wn[0:M, fc:fc + 1])
                    else:
                        self.evac(ot[0:M, half * 512:(half + 1) * 512], p[0:M, :], r=[p], w=[ot])
                r0 = out_row0 + fc * 128
                k.dma("pool", OUT[r0:r0 + M, db * 1024:(db + 1) * 1024], ot[0:M, :], r=[ot], w=[OUT])
        k.end()

    def ph_hy_proj(self, j, with_ctx):
        k = self.k
        k.begin()
        ps = self.psums(8)
        ident = k.tile([128, 128], name="ident")
        self.load("sp", ident[:], self.ident.ap(), r=[self.ident], w=[ident])
        cols = k.tile([128, 48, 5], name="cols")
        self.load("sp", cols[:], self.hy_cols[j].rearrange("p (n c) -> p n c", c=5), r=[self.hy_cols], w=[cols])
        hT = k.tile([128, 16, 512], F32R, name="hT")
        wp = Rot([k.tile([128, 16, 256], F32R, name="w") for _ in range(5)])
        utok = k.tile([128, 4, D], name="utok")
        pp = Rot([k.tile([128, 512], name="p") for _ in range(3)])
        yp = Rot([k.tile([128, 512], name="y") for _ in range(6)])
        uu = Rot([k.tile([128, 512], name="uu") for _ in range(2)])
        wap, wres = self.wsl("hy_w_in", j)
        wv = wap.rearrange("(kc p) n -> p kc n", p=128)
        HTv = self.HT.ap().rearrange("(kc p) t -> p kc t", p=128)
        qn = 0
        for (t0, TB) in self.blocks(with_ctx):
            ntl = TB // 128
            grid = t0 < L
            self.load("sp", hT[:, :, 0:TB], HTv[:, :, t0:t0 + TB].bitcast(F32R), r=[self.HT], w=[hT])
            for jb in range(8):
                wts = []
                for sec in range(3):
                    wt = wp.next()
                    qn += 1
                    c0 = sec * D + jb * 256
                    self.load("sp", wt[:], wv[:, :, c0:c0 + 256].bitcast(F32R),
                              r=wres, w=[wt])
                    wts.append(wt)
                for jl in range(2):
                    jc = jb * 2 + jl
                    ys = []
                    for sec in range(3):
                        nc_ = sec * 16 + jc
                        p = ps.next()
                        for kc in range(16):
                            self.mm(p[:, 0:TB], wts[sec][:, kc, jl * 128:(jl + 1) * 128], hT[:, kc, 0:TB], kc == 0,
                                    kc == 15, r=[wts[sec], hT], w=[p])
                        pt = pp.next()
                        y = yp.next()
                        self.act(pt[:, 0:TB], p[:, 0:TB], AF.Identity, r=[p, cols], w=[pt], bias=cols[:, nc_, 0:1])
                        self.act(y[:, 0:TB], pt[:, 0:TB], AF.Identity, r=[pt, cols], w=[y], scale=cols[:, nc_, 2:3],
                                 bias=cols[:, nc_, 4:5])
                        if grid:
                            pv = pt[:, 0:TB].rearrange("p (r w) -> p r w", w=64)
                            yv = y[:, 0:TB].rearrange("p (r w) -> p r w", w=64)
                            self.stt("dve", yv[:, :, 1:64], pv[:, :, 0:63], cols[:, nc_, 1:2], yv[:, :, 1:64], ALU.mult,
                                     ALU.add, r=[pt, y, cols], w=[y])
                            self.stt("dve", yv[:, :, 0:63], pv[:, :, 1:64], cols[:, nc_, 3:4], yv[:, :, 0:63], ALU.mult,
                                     ALU.add, r=[pt, y, cols], w=[y])
                        else:
                            self.stt("dve", y[:, 1:TB], pt[:, 0:TB - 1], cols[:, nc_, 1:2], y[:, 1:TB], ALU.mult,
                                     ALU.add, r=[pt, y, cols], w=[y])
                            self.stt("dve", y[:, 0:TB - 1], pt[:, 1:TB], cols[:, nc_, 3:4], y[:, 0:TB - 1], ALU.mult,
                                     ALU.add, r=[pt, y, cols], w=[y])
                        ys.append(y)
                    k.dma("pool", self.X0T[jc * 128:(jc + 1) * 128, t0:t0 + TB], ys[0][:, 0:TB], r=[ys[0]], w=[self.X0T])
                    u = uu.next()
                    self.tt("pool", u[:, 0:TB], ys[1][:, 0:TB], ys[2][:, 0:TB], ALU.mult, r=[ys[1], ys[2]], w=[u])
                    k.dma("pool", self.UT[jc * 128:(jc + 1) * 128, t0:t0 + TB], u[:, 0:TB], r=[u], w=[self.UT])
                    p = ps.next()
                    for tl in range(ntl):
                        self.tr(p[:, tl * 128:(tl + 1) * 128], u[:, tl * 128:(tl + 1) * 128], ident[:], r=[u, ident], w=[p])
                    self.evac(utok[:, 0:ntl, jc * 128:(jc + 1) * 128],
                              p[:, 0:TB].rearrange("p (a b) -> p a b", b=128), r=[p], w=[utok])
            k.dma("pool", self.UTOK.ap()[t0:t0 + TB, :].rearrange("(a p) d -> p a d", p=128), utok[:, 0:ntl, :],
                  r=[utok], w=[self.UTOK])
        k.end()

    def ph_hy_inv(self, j, Ls, tok0, frow0, rn_row0):
        k = self.k
        k.begin()
        ps = self.psums(8)
        TC = Ls // 128
        FC = TC + 1
        GS = min(4, TC)
        NG = TC // GS
        TBW = min(512, Ls)
        NTB = Ls // TBW
        rn = k.tile([128, 16], name="rn")
        skc = k.tile([128, 16], name="skc")
        self.load("sp", rn[:], self.RN[rn_row0:rn_row0 + 128, :], r=[self.RN], w=[rn])
        self.load("sp", skc[:], self.hy_skipc[j], r=[self.hy_skipc], w=[skc])
        YR = k.tile([128, FC, 512], F32R, name="YR")
        YI = k.tile([128, FC, 512], F32R, name="YI")
        ldp = Rot([k.tile([128, 4, 512], name="ld") for _ in range(2)])
        tmp = Rot([k.tile([128, 4, 512], name="tm") for _ in range(2)])
        cip = Rot([k.tile([128, GS, TBW], F32R, name="ci") for _ in range(2)])
        nsp = Rot([k.tile([128, GS, TBW], F32R, name="nsi") for _ in range(2)])
        cnp = Rot([k.tile([1, TBW], F32R, name="cin") for _ in range(2)])
        e1 = Rot([k.tile([128, TBW], name="e1") for _ in range(2)])
        e2 = Rot([k.tile([128, TBW], name="e2") for _ in range(2)])
        e3 = Rot([k.tile([128, TBW], name="e3") for _ in range(2)])
        e4 = Rot([k.tile([128, TBW], name="e4") for _ in range(2)])
        for db in range(4):
            dsl = slice(db * 512, (db + 1) * 512)
            for fc in range(FC):
                M = 128 if fc < TC else 1
                r0 = frow0 + fc * 128
                ld = ldp.next()
                tm = tmp.next()
                for n_, src in enumerate((self.URE, self.UIM, self.KRE, self.KIM)):
                    self.load("sp", ld[0:M, n_, :], src[r0:r0 + M, dsl], r=[src], w=[ld])
                ur, ui, kr, ki = (ld[0:M, n_, :] for n_ in range(4))
                self.tt("dve", tm[0:M, 0, :], ur, kr, ALU.mult, r=[ld], w=[tm])
                self.tt("pool", tm[0:M, 1, :], ui, ki, ALU.mult, r=[ld], w=[tm])
                self.tt("pool", tm[0:M, 2, :], ur, ki, ALU.mult, r=[ld], w=[tm])
                self.tt("dve", tm[0:M, 3, :], ui, kr, ALU.mult, r=[ld], w=[tm])
                self.tt("dve", YR[0:M, fc, :], tm[0:M, 0, :], tm[0:M, 1, :], ALU.subtract, r=[tm], w=[YR])
                self.tt("pool", YI[0:M, fc, :], tm[0:M, 2, :], tm[0:M, 3, :], ALU.add, r=[tm], w=[YI])
            for tb in range(NTB):
                pd = [ps.next() for _ in range(4)]
                for g in range(NG):
                    ci = cip.next()
                    nsi = nsp.next()
                    if Ls == L:
                        ciap, cires = self.wsl("ci_l", tb * 4 + g)
                        nsap, nsres = self.wsl("nsi_l", tb * 4 + g)
                    else:
                        ciap, cires = self.ci_c.ap(), []
                        nsap, nsres = self.nsi_c.ap(), []
                    self.load("sp", ci[:], ciap.rearrange("p (f t) -> p f t", t=TBW).bitcast(F32R), r=cires, w=[ci])
                    self.load("sp", nsi[:], nsap.rearrange("p (f t) -> p f t", t=TBW).bitcast(F32R), r=nsres, w=[nsi])
                    for dc in range(4):
                        for fl in range(GS):
                            fc = g * GS + fl
                            self.mm(pd[dc][:, 0:TBW], YR[:, fc, dc * 128:(dc + 1) * 128], ci[:, fl, :],
                                    g == 0 and fl == 0, False, r=[YR, ci], w=[pd[dc]])
                            self.mm(pd[dc][:, 0:TBW], YI[:, fc, dc * 128:(dc + 1) * 128], nsi[:, fl, :], False, False,
                                    r=[YI, nsi], w=[pd[dc]])
                cn = cnp.next()
                self.load("sp", cn[:], self.cin[Ls][0:1, tb * TBW:(tb + 1) * TBW].bitcast(F32R), r=[self.cin[Ls]], w=[cn])
                for dc in range(4):
                    self.mm(pd[dc][:, 0:TBW], YR[0:1, TC, dc * 128:(dc + 1) * 128], cn[0:1, :], False, True,
                            r=[YR, cn], w=[pd[dc]])
                    dch = db * 4 + dc
                    c0 = tok0 + tb * TBW
                    x0t = e1.next()
                    ut = e2.next()
                    t1 = e3.next()
                    t2 = e4.next()
                    self.load("sp", x0t[:], self.X0T[dch * 128:(dch + 1) * 128, c0:c0 + TBW], r=[self.X0T], w=[x0t])
                    self.load("sp", ut[:], self.UT[dch * 128:(dch + 1) * 128, c0:c0 + TBW], r=[self.UT], w=[ut])
                    self.act(t1[:], pd[dc][:, 0:TBW], AF.Copy, r=[pd[dc], rn], w=[t1], scale=rn[:, dch:dch + 1])
                    self.stt("dve", t2[:], ut[:], skc[:, dch:dch + 1], t1[:], ALU.mult, ALU.add, r=[ut, skc, t1], w=[t2])
                    self.tt("pool", t2[:], t2[:], x0t[:], ALU.mult, r=[t2, x0t], w=[t2])
                    k.dma("pool", self.YHT[dch * 128:(dch + 1) * 128, c0:c0 + TBW], t2[:], r=[t2], w=[self.YHT])
        k.end()

    def ph_final(self):
        k = self.k
        k.begin()
        gam = k.tile([128, D], name="gam")
        self.bcload(gam, self.norm_final[0:1, :], r=[self.norm_final])
        xp = Rot([k.tile([128, D], name="xt") for _ in range(3)])
        op_ = Rot([k.tile([128, D], name="ot") for _ in range(3)])
        junk = k.tile([128, D], name="junk")
        ssp = Rot([k.tile([128, 2], name="ss") for _ in range(3)])
        for tt_ in range(16):
            r0 = tt_ * 128
            xt = xp.next()
            ot = op_.next()
            ss = ssp.next()
            self.load("sp", xt[:], self.X[r0:r0 + 128, :], r=self.X.rows(r0, r0 + 128), w=[xt])
            self.memset("pool", ss[:], 0.0, w=[ss])
            self.act(junk[:], xt[:], AF.Square, r=[xt], w=[junk, ss], accum=ss[:, 0:1])
            self.rstd(ss[:, 0:1], ss[:, 1:2], D, r=[ss])
            self.stt("dve", ot[:], xt[:], ss[:, 1:2], gam[:], ALU.mult, ALU.mult, r=[xt, ss, gam], w=[ot])
            k.dma("act", self.out[r0:r0 + 128, :], ot[:], r=[ot], w=self.out.rows(r0, r0 + 128))
        k.end()

    def ph_dump(self, src, r0, r1):
        k = self.k
        k.begin()
        k.dma("sp", self.out.ap(), src[r0:r1, :], r=[src], w=[self.out])
        k.end()

    def layer_mixer(self, i):
        need_ctx = i < DEPTH - 1
        j = i // 2
        if i % 2 == 0:
            self.ph_norm(i, "mix", True)
            self.ph_gla_proj(j)
            self.ph_gla_scan(j)
            self.ph_outproj(i, "gla_w_out", j, self.OG, "tok", need_ctx)
        else:
            self.ph_norm(i, "mix", need_ctx)
            streams = [(L, 0, 0, 0)] + ([(CL, L, 17 * 128, 128)] if need_ctx else [])
            for (Ls, tok0, frow0, rn0) in streams:
                self.ph_hy_filter(j, Ls, tok0, rn0)
                self.ph_dft(self.HS, tok0, Ls, "cf", self.KRE, frow0, True)
                self.ph_dft(self.HD, tok0, Ls, "sf", self.KIM, frow0, True)
            self.ph_hy_proj(j, need_ctx)
            for (Ls, tok0, frow0, rn0) in streams:
                self.ph_dft(self.UTOK, tok0, Ls, "cf", self.URE, frow0, False)
                self.ph_dft(self.UTOK, tok0, Ls, "sf", self.UIM, frow0, False)
                self.ph_hy_inv(j, Ls, tok0, frow0, rn0)
            self.ph_outproj(i, "hy_w_out", j, self.YHT, "feat", need_ctx, bias_row=self.hy_b_out[j:j + 1, :])

    def layer_ffn(self, i):
        need_ctx = i < DEPTH - 1
        self.ph_norm(i, "ffn", need_ctx)
        self.ph_experts(i, need_ctx)

    def build(self, stages=None):
        self.ph_init()
        if not self.direct:
            self.ph_gather()
        for i in range(DEPTH):
            if stages is not None and ("mix", i) not in stages and ("ffn", i) not in stages:
                continue
            self.ph_mods(i)
            if stages is None or ("mix", i) in stages:
                self.layer_mixer(i)
            if stages is None or ("ffn", i) in stages:
                self.layer_ffn(i)
        if self.debug_dump is None:
            self.ph_final()
        else:
            src = getattr(self, self.debug_dump[0])
            self.ph_dump(src, self.debug_dump[2], self.debug_dump[2] + self.debug_dump[1][0])
        self.k.finish()
        return self.nc


_CONST = None


def constants():
    global _CONST
    if _CONST is not None:
        return _CONST
    f32 = np.float32
    c = {}
    c["ident"] = np.eye(128, dtype=f32)
    s = np.arange(128)
    tri_f = (s[:, None] <= s[None, :]).astype(f32)
    tri_b = (s[:, None] >= s[None, :]).astype(f32)
    c["tri"] = np.stack([tri_f, tri_b]) * f32(-1.0 / 16.0)
    c["msk"] = np.stack([tri_f, tri_b])
    for Ls, nm in ((L, "l"), (CL, "c")):
        t = np.linspace(0.0, 1.0, Ls, dtype=f32)[:, None]
        w = (2 * np.pi * np.arange(Ls, dtype=f32)[:, None] / Ls).astype(f32)
        f = np.linspace(1e-4, 15, 16, dtype=f32)[None, :]
        fw = (f * w).astype(f32)
        z = np.concatenate([t, np.cos(fw), -np.sin(fw)], axis=-1).astype(f32)
        c["zT_" + nm] = np.ascontiguousarray(z.T)
        tt_ = np.linspace(0.0, 1.0, Ls, dtype=f32)
        c["negt_" + nm] = np.ascontiguousarray((-tt_).reshape(Ls // 128, 128).T)
        N = 2 * Ls
        TC = Ls // 128
        FCn = TC + 1
        tq = np.arange(Ls, dtype=np.float64)
        fq = np.arange(FCn * 128, dtype=np.float64)
        ang = 2 * np.pi * np.outer(tq, fq) / N
        valid = (fq <= Ls)[None, :]
        C = np.where(valid, np.cos(ang), 0.0)
        Sn = np.where(valid, -np.sin(ang), 0.0)
        def fwd(T):
            T4 = T.reshape(TC, 128, FCn, 128)
            return np.ascontiguousarray(T4.transpose(2, 1, 0, 3).reshape(FCn, 128, TC * 128)).astype(f32)
        c["cf_" + nm] = fwd(C)
        c["sf_" + nm] = fwd(Sn)
        TBW = min(512, Ls)
        NTB = Ls // TBW
        GS = min(4, TC)
        NG = TC // GS
        def inv(T):
            T5 = T[:, :TC * 128].reshape(NTB, TBW, NG, GS, 128)
            return np.ascontiguousarray(T5.transpose(0, 2, 4, 3, 1).reshape(NTB, NG, 128, GS * TBW)).astype(f32)
        c["ci_" + nm] = inv(C)
        c["nsi_" + nm] = inv(Sn)
        c["cin_" + nm] = np.cos(np.pi * tq)[None, :].astype(f32)
        wf = np.full(FCn * 128, 2.0 / N)
        wf[0] = 1.0 / N
        wf[Ls] = 1.0 / N
        wf[Ls + 1:] = 0.0
        c["wn_" + nm] = np.ascontiguousarray(wf.reshape(FCn, 128).T).astype(f32)
    max_decay = math.log(1e-2) / 0.3
    min_decay = math.log(1e-2) / 1.5
    c["padf"] = np.ascontiguousarray(np.broadcast_to((NT + np.arange(128)).astype(f32)[None, :], (16, 128)))
    c["delta"] = np.abs(np.linspace(min_decay, max_decay, D, dtype=f32))[None, :].astype(f32)
    _CONST = c
    return c


def host_inputs(inp, ncores=NCORES, units=None, direct=None):
    f32 = np.float32
    g = lambda n: np.ascontiguousarray(np.asarray(inp[n], dtype=f32))
    direct = (ncores == 1) if direct is None else direct
    units = units if units is not None else all_units()
    dcache = {}
    shared = {}
    for n in ("b_ada", "norm_mix", "norm_ffn", "hy_f_w1", "hy_f_w2", "hy_f_w3", "hy_f_w4", "hy_b_out", "moe_router"):
        shared[n] = g(n)
    shared["norm_final"] = g("norm_final").reshape(1, D)
    shared["gla_hn"] = g("gla_head_norm")
    a1 = g("gla_w_a1")
    shared["gla_wa1"] = np.ascontiguousarray(a1.transpose(0, 2, 1, 3).reshape(2, D, 32))
    a2 = g("gla_w_a2")
    ba = g("gla_b_a")
    wa2 = np.zeros((2, 33, 2048), f32)
    for z in range(2):
        wa2[:, z * 16:(z + 1) * 16, z * 1024:(z + 1) * 1024] = a2[:, z]
        wa2[:, 32, z * 1024:(z + 1) * 1024] = ba[:, z]
    shared["gla_wa2"] = wa2
    cw = g("hy_conv_w")
    cols = np.stack([g("hy_b_in"), cw[:, 0], cw[:, 1], cw[:, 2], g("hy_conv_b")], axis=-1)
    shared["hy_cols"] = np.ascontiguousarray(cols.reshape(2, 48, 128, 5).transpose(0, 2, 1, 3).reshape(2, 128, 240))
    shared["hy_fb"] = np.ascontiguousarray(np.stack([g("hy_f_b1"), g("hy_f_b2"), g("hy_f_b3")], axis=-1))
    shared["hy_skipc"] = np.ascontiguousarray(g("hy_skip").reshape(2, 16, 128).transpose(0, 2, 1))
    cst = constants()
    big = {}
    for n in SHARDED:
        C, rpu, nu, CH = SHARDED[n]
        src = cst[n] if n in cst else np.asarray(inp[n], dtype=f32)
        big[n] = src.reshape(nu * rpu, C)
    for n, v in cst.items():
        if n in SHARDED:
            continue
        if n in ("cf_c", "sf_c"):
            v = v.reshape(3 * 128, 2 * 128)
        if n in ("ci_c", "nsi_c"):
            v = v.reshape(128, 2 * 256)
        shared[n] = v
    x = g("x")
    ctx = g("ctx")
    c = g("c")
    cctx = g("c_ctx")
    maps = []
    for b in range(ncores):
        m = dict(shared)
        m["x"] = x[b]
        m["ctx"] = ctx[b]
        cc = np.stack([c[b], cctx], axis=-1)
        m["cc"] = np.ascontiguousarray(cc.reshape(16, 128, 2).transpose(1, 0, 2).reshape(128, 32))
        for n in SHARDED:
            C, rpu, nu, CH = SHARDED[n]
            if direct:
                if n not in dcache:
                    us = units[n] if len(units[n]) else [0]
                    dcache[n] = big[n] if len(us) == nu else np.ascontiguousarray(
                        big[n].reshape(nu, rpu, C)[us].reshape(-1, C))
                m[n] = dcache[n]
            else:
                R = nu * rpu
                m[n] = np.ascontiguousarray(big[n].reshape(R // CH, NCORES, CH // NCORES, C)[:, b].reshape(-1, C))
        maps.append(m)
    return maps


_PROG = {}


REPLICATE = True


def get_prog():
    if "p" not in _PROG:
        p = Prog(direct=REPLICATE)
        p.build()
        _PROG["p"] = p
    return _PROG["p"]


def kernel(**inputs):
    maps = host_inputs(inputs, direct=REPLICATE)
    prog = get_prog()
    res = run_bass_kernel_spmd(prog.nc, maps, core_ids=list(range(NCORES)))
    out = np.stack([np.asarray(r["out"]) for r in res.results], axis=0)
    return out.astype(np.float32)
```

```python
import math
import numpy as np
from contextlib import ExitStack
import concourse.bass as bass
import concourse.mybir as mybir
from concourse.bass_utils import run_bass_kernel_spmd

F32 = mybir.dt.float32
F32R = mybir.dt.float32r
I32 = mybir.dt.int32
U32 = mybir.dt.uint32
AF = mybir.ActivationFunctionType
ALU = mybir.AluOpType
AX = mybir.AxisListType

D = 2048
L = 2048
CL = 256
NT = L + CL
DEPTH = 4
EPS = 1e-6
NE = 16
FF = 1024
HEADS = 4
DK = 256
DV = 512
NCORES = 8

SEM_LIMIT = 16000
NDMA = 8


class Res:
    __slots__ = ("w", "r")

    def __init__(self):
        self.w = {}
        self.r = {}


class Tile:
    def __init__(self, h, nblk=1, bs=None, is_dram=False, persist=False):
        self.h = h
        self.res = [Res() for _ in range(nblk)]
        self.bs = bs
        self.is_dram = is_dram
        self.persist = persist

    def __getitem__(self, key):
        if self.is_dram:
            return self.h.ap()[key]
        return self.h[key]

    def ap(self):
        return self.h.ap() if self.is_dram else self.h[:]

    def rows(self, r0, r1):
        if self.bs is None:
            return list(self.res)
        return self.res[r0 // self.bs:(r1 - 1) // self.bs + 1]


def _flat(lst):
    out = []
    for x in lst:
        if isinstance(x, Res):
            out.append(x)
        elif isinstance(x, Tile):
            out.extend(x.res)
        else:
            out.extend(_flat(x))
    return out


class K:
    COMPUTE = ("pe", "act", "dve", "pool")
    ALL = ("pe", "act", "dve", "pool", "sp")

    def __init__(self, nc):
        self.nc = nc
        self.ges = ExitStack()
        self.sems = {}
        self.cs = {}
        self.nsem = 0
        for e in self.COMPUTE:
            self._new_csem(e)
        self.ds = {q: [[self._new_sem("d_%s%d" % (q, i)), 0] for i in range(NDMA)]
                   for q in ("sp", "pool", "act")}
        self.dn = {q: 0 for q in self.ds}
        self.ops = {e: [] for e in self.ALL}
        self.waited = {e: {} for e in self.ALL}
        self.tiles = []
        self.pes = None
        self.ntile = 0
        self.ninst = 0
        self.ccsem = None

    def _new_sem(self, name):
        name = "%s_%d" % (name, self.nsem)
        self.nsem += 1
        self.sems[name] = self.ges.enter_context(self.nc.semaphore(name))
        return name

    def _new_csem(self, e):
        self.cs[e] = [self._new_sem("c_" + e), 0]

    def dram(self, name, shape, dtype=F32, kind="Internal", bs=None, persist=False):
        h = self.nc.dram_tensor(name, list(shape), dtype, kind=kind)
        nblk = 1 if bs is None else (shape[0] + bs - 1) // bs
        t = Tile(h, nblk=nblk, bs=bs, is_dram=True, persist=persist)
        self.tiles.append(t)
        return t

    def tile(self, shape, dtype=F32, name=None):
        self.ntile += 1
        name = "%s_%d" % (name or "t", self.ntile)
        h = self.pes.enter_context(self.nc.sbuf_tensor(name, list(shape), dtype))
        t = Tile(h)
        self.tiles.append(t)
        return t

    def psum(self, shape=(128, 512), dtype=F32, name=None):
        self.ntile += 1
        name = "%s_%d" % (name or "ps", self.ntile)
        h = self.pes.enter_context(self.nc.psum_tensor(name, list(shape), dtype))
        t = Tile(h)
        self.tiles.append(t)
        return t

    def _deps(self, eng, r, w):
        deps = {}
        for x in _flat(r):
            for s, v in x.w.items():
                if deps.get(s, 0) < v:
                    deps[s] = v
        for x in _flat(w):
            for d in (x.w, x.r):
                for s, v in d.items():
                    if deps.get(s, 0) < v:
                        deps[s] = v
        waits = []
        wd = self.waited[eng]
        for s, v in deps.items():
            if eng == "pe" and s.startswith("c_pe"):
                continue
            if wd.get(s, 0) < v:
                waits.append((s, v))
                wd[s] = v
        return waits

    def _mark(self, r, w, s, v):
        for x in _flat(r):
            if x.r.get(s, 0) < v:
                x.r[s] = v
        for x in _flat(w):
            x.w = {s: v}
            x.r = {}

    def op(self, eng, fn, r=(), w=()):
        waits = self._deps(eng, r, w)
        c = self.cs[eng]
        if c[1] >= SEM_LIMIT:
            self._new_csem(eng)
            c = self.cs[eng]
        c[1] += 1
        self.ops[eng].append((waits, fn, c[0], 1))
        self._mark(r, w, c[0], c[1])
        self.ninst += 1 + len(waits)

    def dma(self, q, out, in_, r=(), w=(), fn=None, **kw):
        waits = self._deps(q, r, w)
        j = self.dn[q] % NDMA
        self.dn[q] += 1
        slot = self.ds[q][j]
        if slot[1] + 16 > SEM_LIMIT:
            slot[0] = self._new_sem("d_%s%d" % (q, j))
            slot[1] = 0
        if slot[1] > 0 and self.waited[q].get(slot[0], 0) < slot[1]:
            waits.append((slot[0], slot[1]))
            self.waited[q][slot[0]] = slot[1]
        slot[1] += 16
        if fn is None:
            def fn(e, out=out, in_=in_, kw=kw):
                return e.dma_start(out=out, in_=in_, **kw)
        self.ops[q].append((waits, fn, slot[0], 16))
        self._mark(r, w, slot[0], slot[1])
        self.ninst += 1 + len(waits)

    def coll(self, fn, r=(), w=()):
        if self.ccsem is None:
            self.ccsem = [self._new_sem("cc"), 0]
        waits = self._deps("pool", r, w)
        self.ccsem[1] += 1
        self.ops["pool"].append((waits, fn, self.ccsem[0], 1))
        self._mark(r, w, self.ccsem[0], self.ccsem[1])

    def barrier(self):
        cur = {}
        for e in self.COMPUTE:
            n, c = self.cs[e]
            if c > 0:
                cur[n] = c
        for q in self.ds:
            for n, v in self.ds[q]:
                if v > 0:
                    cur[n] = v
        for e in self.ALL:
            wd = self.waited[e]
            waits = []
            for s, v in cur.items():
                if wd.get(s, 0) < v:
                    waits.append((s, v))
                    wd[s] = v
            if waits:
                self.ops[e].append((waits, None, None, 0))
        for t in self.tiles:
            if t.persist:
                continue
            for x in t.res:
                x.w = {}
                x.r = {}

    def begin(self):
        self.pes = ExitStack()

    def end(self):
        self.barrier()
        nc = self.nc
        ops = self.ops
        sems = self.sems

        def replay(name, e):
            for waits, fn, s, inc in ops[name]:
                for ws, wv in waits:
                    e.wait_ge(sems[ws], wv)
                if fn is not None:
                    ins = fn(e)
                    ins.then_inc(sems[s], inc)

        with nc.Block() as blk:
            @blk.sync
            def _(e):
                replay("sp", e)

            @blk.scalar
            def _(e):
                replay("act", e)

            @blk.vector
            def _(e):
                replay("dve", e)

            @blk.gpsimd
            def _(e):
                replay("pool", e)

            @blk.tensor
            def _(e):
                replay("pe", e)
        self.ops = {e: [] for e in self.ALL}
        self.pes.close()
        self.pes = None
        self.tiles = [t for t in self.tiles if t.is_dram]

    def finish(self):
        self.ges.close()


class Rot:
    def __init__(self, tiles):
        self.t = tiles
        self.i = 0

    def next(self):
        t = self.t[self.i % len(self.t)]
        self.i += 1
        return t


SHARDED = {
    "w_ada": (6 * D, D, 4, 1024),
    "gla_w_in": (6144, D, 2, 2048),
    "gla_w_out": (D, D, 2, 2048),
    "hy_w_in": (3 * D, D, 2, 2048),
    "hy_w_out": (D, D, 2, 2048),
    "moe_w1": (FF, D, 64, 8192),
    "moe_w3": (FF, D, 64, 8192),
    "moe_w2": (D, FF, 64, 4096),
    "cf_l": (16 * 128, 128, 17, 2176),
    "sf_l": (16 * 128, 128, 17, 2176),
    "ci_l": (4 * 512, 128, 16, 2048),
    "nsi_l": (4 * 512, 128, 16, 2048),
}


GROUP = {"w_ada": 1, "moe_w1": 16, "moe_w3": 16, "moe_w2": 16}


def all_units():
    return {n: list(range(v[2])) for n, v in SHARDED.items()}


class Prog:
    def __init__(self, ncores=NCORES, units=None, debug_dump=None, direct=None):
        self.ncores = ncores
        self.direct = (ncores == 1) if direct is None else direct
        self.units = units if units is not None else all_units()
        self.debug_dump = debug_dump
        nc = bass.Bass("TRN2", target_bir_lowering=False)
        nc.dge_precook = False
        self.nc = nc
        self.k = K(nc)
        self.evn = 0
        self.decl()

    def decl(self):
        k = self.k
        I = lambda name, shape, dt=F32: k.dram(name, shape, dt, kind="ExternalInput")
        S = lambda name, shape, dt=F32, bs=None: k.dram(name, shape, dt, kind="Internal", bs=bs)
        self.x_in = I("x", [L, D])
        self.ctx_in = I("ctx", [CL, D])
        self.cc = I("cc", [128, 32])
        self.b_ada = I("b_ada", [DEPTH, 6 * D])
        self.norm_mix = I("norm_mix", [DEPTH, D])
        self.norm_ffn = I("norm_ffn", [DEPTH, D])
        self.norm_final = I("norm_final", [1, D])
        self.gla_wa1 = I("gla_wa1", [2, D, 32])
        self.gla_wa2 = I("gla_wa2", [2, 33, 2048])
        self.gla_hn = I("gla_hn", [2, DV])
        self.hy_cols = I("hy_cols", [2, 128, 48 * 5])
        self.hy_f_w1 = I("hy_f_w1", [2, 33, 64])
        self.hy_fb = I("hy_fb", [2, 64, 3])
        self.hy_f_w2 = I("hy_f_w2", [2, 64, 64])
        self.hy_f_w3 = I("hy_f_w3", [2, 64, 64])
        self.hy_f_w4 = I("hy_f_w4", [2, 64, 2 * D])
        self.hy_skipc = I("hy_skipc", [2, 128, 16])
        self.hy_b_out = I("hy_b_out", [2, D])
        self.moe_router = I("moe_router", [DEPTH, D, NE])
        self.wts = {}
        self.wsh = {}
        self.wbn = {}
        for n, (C, rpu, nu, CH) in SHARDED.items():
            if self.direct:
                nuse = max(1, len(self.units[n]))
                self.wts[n] = k.dram(n, [nuse * rpu, C], F32, kind="ExternalInput")
            else:
                R = nu * rpu
                gu = GROUP.get(n, nu)
                self.wsh[n] = k.dram(n, [R // NCORES, C], F32, kind="ExternalInput")
                self.wbn[n] = k.dram(n + "_bn", [R // NCORES, C], F32, kind="Internal")
                self.wts[n] = [k.dram("%s_g%d" % (n, gi), [gu * rpu, C], F32, kind="Internal", bs=CH, persist=True)
                               for gi in range(nu // gu)]
        self.ident = I("ident", [128, 128])
        self.padf = I("padf", [16, 128])
        self.tri = I("tri", [2, 128, 128])
        self.msk = I("msk", [2, 128, 128])
        self.zT = {L: I("zT_l", [33, L]), CL: I("zT_c", [33, CL])}
        self.negt = {L: I("negt_l", [128, L // 128]), CL: I("negt_c", [128, CL // 128])}
        self.delta = I("delta", [1, D])
        self.cf_c = I("cf_c", [3 * 128, 2 * 128])
        self.sf_c = I("sf_c", [3 * 128, 2 * 128])
        self.ci_c = I("ci_c", [128, 2 * 256])
        self.nsi_c = I("nsi_c", [128, 2 * 256])
        self.cin = {L: I("cin_l", [1, L]), CL: I("cin_c", [1, CL])}
        self.wn = {L: I("wn_l", [128, 17]), CL: I("wn_c", [128, 3])}
        if self.debug_dump is None:
            self.out = k.dram("out", [L, D], F32, kind="ExternalOutput", bs=128)
        else:
            self.out = k.dram("out", list(self.debug_dump[1]), F32, kind="ExternalOutput")
        self.X = S("X", [NT + 128, D], bs=128)
        self.MOD = S("MOD", [DEPTH * 2, 6 * D])
        self.HT = S("HT", [D, NT])
        self.H = S("H", [NT + 128, D])
        self.QT = S("QT", [1024, NT])
        self.KT = S("KT", [1024, NT])
        self.V = S("V", [NT, D])
        self.G = S("G", [NT, D])
        self.U = S("U", [NT, D])
        self.OG = S("OG", [NT, D])
        self.X0T = S("X0T", [D, NT])
        self.UT = S("UT", [D, NT])
        self.UTOK = S("UTOK", [NT, D])
        self.HS = S("HS", [NT, D])
        self.HD = S("HD", [NT, D])
        FR = 17 * 128 + 3 * 128
        self.KRE = S("KRE", [FR, D])
        self.KIM = S("KIM", [FR, D])
        self.URE = S("URE", [FR, D])
        self.UIM = S("UIM", [FR, D])
        self.YHT = S("YHT", [D, NT])
        self.RN = S("RN", [2 * 128, 16])
        self.IDXD = S("IDXD", [3 * 128, NE], I32)
        self.TSD = S("TSD", [3 * 128, NE])

    def wsl(self, name, unit):
        C, rpu, nu, CH = SHARDED[name]
        if self.direct:
            r0 = self.units[name].index(unit) * rpu
            return self.wts[name].ap()[r0:r0 + rpu, :], []
        gu = GROUP.get(name, nu)
        t = self.wts[name][unit // gu]
        r0 = (unit % gu) * rpu
        return t.ap()[r0:r0 + rpu, :], t.rows(r0, r0 + rpu)

    def ph_gather(self):
        k = self.k
        k.begin()
        order = []
        moe = lambda l: [(n, 4 * l + c) for c in range(4) for n in ("moe_w1", "moe_w3", "moe_w2")]
        order += [("w_ada", 0), ("w_ada", 1), ("gla_w_in", 0), ("gla_w_out", 0)] + moe(0)
        order += [("w_ada", 2), ("w_ada", 3), ("hy_w_in", 0), ("cf_l", 0), ("sf_l", 0), ("ci_l", 0), ("nsi_l", 0),
                  ("hy_w_out", 0)] + moe(1)
        order += [("w_ada", 4), ("w_ada", 5), ("gla_w_in", 1), ("gla_w_out", 1)] + moe(2)
        order += [("w_ada", 6), ("w_ada", 7), ("hy_w_in", 1), ("hy_w_out", 1)] + moe(3)
        qn = 0
        for (n, c) in order:
            C, rpu, nu, CH = SHARDED[n]
            sh, bn = self.wsh[n], self.wbn[n]
            grows = GROUP.get(n, nu) * rpu
            gt = self.wts[n][(c * CH) // grows]
            g0 = (c * CH) % grows
            pr = CH // NCORES
            qn += 1
            bres = Res()
            k.dma("sp", bn[c * pr:(c + 1) * pr, :], sh[c * pr:(c + 1) * pr, :], r=[], w=[bres])
            k.coll(lambda e, bn=bn, gt=gt, c=c, pr=pr, CH=CH, g0=g0: e.collective_compute(
                "AllGather", ALU.bypass, replica_groups=[list(range(NCORES))],
                ins=[bn.ap()[c * pr:(c + 1) * pr, :]], outs=[gt.ap()[g0:g0 + CH, :]]),
                r=[bres], w=gt.rows(g0, g0 + CH))
        k.end()

    def mm(self, ps, lhsT, rhs, start, stop, r, w):
        self.k.op("pe", lambda e: e.matmul(ps, lhsT, rhs, start=start, stop=stop), r=r, w=w)

    def tr(self, ps, in_, ident, r, w):
        self.k.op("pe", lambda e: e.transpose(ps, in_, ident), r=r, w=w)

    def act(self, out, in_, func, r, w, bias=None, scale=None, accum=None):
        kw = {}
        if bias is not None:
            kw["bias"] = bias
        if scale is not None:
            kw["scale"] = scale
        if accum is not None:
            kw["accum_out"] = accum
        self.k.op("act", lambda e: e.activation(out, in_, func, **kw), r=r, w=w)

    def cp(self, eng, out, in_, r, w):
        if eng == "act":
            self.k.op("act", lambda e: e.copy(out, in_), r=r, w=w)
        else:
            self.k.op(eng, lambda e: e.tensor_copy(out, in_), r=r, w=w)

    def evac(self, out, in_, r, w):
        self.evn += 1
        self.cp("act" if self.evn % 2 else "dve", out, in_, r, w)

    def tt(self, eng, out, a, b, op, r, w):
        self.k.op(eng, lambda e: e.tensor_tensor(out=out, in0=a, in1=b, op=op), r=r, w=w)

    def ts(self, eng, out, in0, s1, s2, op0, op1, r, w):
        if s2 is None:
            self.k.op(eng, lambda e: e.tensor_scalar(out, in0, s1, None, op0=op0), r=r, w=w)
        else:
            self.k.op(eng, lambda e: e.tensor_scalar(out, in0, s1, s2, op0=op0, op1=op1), r=r, w=w)

    def stt(self, eng, out, in0, scalar, in1, op0, op1, r, w):
        self.k.op(eng, lambda e: e.scalar_tensor_tensor(out=out, in0=in0, scalar=scalar, in1=in1, op0=op0, op1=op1),
                  r=r, w=w)

    def memset(self, eng, ap, val, w):
        self.k.op(eng, lambda e: e.memset(ap, val), w=w)

    def recip(self, out, in_, r, w):
        self.k.op("dve", lambda e: e.reciprocal(out, in_), r=r, w=w)

    def load(self, q, out, in_, r, w, **kw):
        self.k.dma(q, out, in_, r=r, w=w, **kw)

    def bcload(self, tile_, dram_row_ap, r, npart=128):
        self.k.dma("pool", tile_[0:npart], dram_row_ap.partition_broadcast(npart), r=r, w=[tile_])

    def psums(self, n=8):
        return Rot([self.k.psum(name="ps%d" % i) for i in range(n)])

    def rstd(self, ss, rs, n, r):
        self.act(rs, ss, AF.Sqrt, r=r, w=r, scale=1.0 / n, bias=EPS)
        self.recip(rs, rs, r=r, w=r)

    def ph_init(self):
        k = self.k
        k.begin()
        k.dma("sp", self.X[0:L, :], self.x_in.ap(), r=[self.x_in], w=self.X.rows(0, L))
        k.dma("act", self.X[L:NT, :], self.ctx_in.ap(), r=[self.ctx_in], w=self.X.rows(L, NT))
        k.end()

    def ph_mods(self, i):
        k = self.k
        k.begin()
        ps = self.psums(4)
        cct = k.tile([128, 32], name="cct")
        sc = k.tile([128, 32], F32R, name="sc")
        self.load("sp", cct[:], self.cc.ap(), r=[self.cc], w=[cct])
        self.act(sc[:], cct[:], AF.Silu, r=[cct], w=[sc])
        scv = sc[:].rearrange("p (kc r) -> p kc r", r=2)
        wp = Rot([k.tile([128, 16, 512], F32R, name="wada") for _ in range(3)])
        bp = Rot([k.tile([2, 512], name="bada") for _ in range(2)])
        mp = Rot([k.tile([2, 512], name="modo") for _ in range(2)])
        if True:
            wap, wres = self.wsl("w_ada", i)
            wv = wap.rearrange("(kc p) n -> p kc n", p=128)
            for nb in range(24):
                wt = wp.next()
                bt = bp.next()
                mt = mp.next()
                self.load("sp", wt[:], wv[:, :, nb * 512:(nb + 1) * 512].bitcast(F32R),
                          r=wres, w=[wt])
                self.bcload(bt, self.b_ada[i:i + 1, nb * 512:(nb + 1) * 512], r=[self.b_ada], npart=2)
                p = ps.next()
                for kc in range(16):
                    self.mm(p[0:2, :], scv[:, kc, :], wt[:, kc, :], kc == 0, kc == 15, r=[sc, wt], w=[p])
                self.tt("dve", mt[:], p[0:2, :], bt[:], ALU.add, r=[p, bt], w=[mt])
                k.dma("pool", self.MOD[2 * i:2 * i + 2, nb * 512:(nb + 1) * 512], mt[:], r=[mt], w=[self.MOD])
        k.end()

    def mod_row(self, i, r, chunk):
        return self.MOD[2 * i + r:2 * i + r + 1, chunk * D:(chunk + 1) * D]

    def tok_tiles(self, with_ctx):
        return list(range(18 if with_ctx else 16))

    def blocks(self, with_ctx):
        b = [(t0, 512) for t0 in range(0, L, 512)]
        if with_ctx:
            b.append((L, CL))
        return b

    def ph_norm(self, i, kind, with_ctx):
        k = self.k
        k.begin()
        ps = self.psums(6)
        ident = k.tile([128, 128], name="ident")
        self.load("sp", ident[:], self.ident.ap(), r=[self.ident], w=[ident])
        gsrc = self.norm_mix if kind == "mix" else self.norm_ffn
        shc, scc = (0, 1) if kind == "mix" else (3, 4)
        gam = k.tile([128, D], name="gam")
        self.bcload(gam, gsrc[i:i + 1, :], r=[gsrc])
        gm, sh = [], []
        for r_ in range(2 if with_ctx else 1):
            g_ = k.tile([128, D], name="gm")
            s_ = k.tile([128, D], name="sh")
            self.bcload(g_, self.mod_row(i, r_, scc), r=[self.MOD])
            self.stt("dve", g_[:], g_[:], 1.0, gam[:], ALU.add, ALU.mult, r=[g_, gam], w=[g_])
            self.bcload(s_, self.mod_row(i, r_, shc), r=[self.MOD])
            gm.append(g_)
            sh.append(s_)
        xp = Rot([k.tile([128, D], name="xt") for _ in range(2)])
        hp = Rot([k.tile([128, D], name="ht") for _ in range(2)])
        junk = k.tile([128, D], name="junk")
        ssp = Rot([k.tile([128, 2], name="ss") for _ in range(3)])
        if kind == "mix":
            hTb = Rot([k.tile([128, 16, 512], name="hTb") for _ in range(2)])
        else:
            hTt = Rot([k.tile([128, 16, 128], name="hTt") for _ in range(2)])
            wr = k.tile([128, 16, NE], name="wr")
            self.load("sp", wr[:], self.moe_router[i].rearrange("(kc p) e -> p kc e", p=128),
                      r=[self.moe_router], w=[wr])
            ST = k.tile([16, NT], name="ST")
            smp = Rot([k.tile([128, 40], name="sm") for _ in range(2)])
        tiles = self.tok_tiles(with_ctx)
        cur = None
        for tt_ in tiles:
            r_ = 1 if tt_ >= 16 else 0
            r0 = tt_ * 128
            xt = xp.next()
            ht = hp.next()
            ss = ssp.next()
            self.load("sp", xt[:], self.X[r0:r0 + 128, :], r=self.X.rows(r0, r0 + 128), w=[xt])
            self.memset("pool", ss[:], 0.0, w=[ss])
            self.act(junk[:], xt[:], AF.Square, r=[xt], w=[junk, ss], accum=ss[:, 0:1])
            self.rstd(ss[:, 0:1], ss[:, 1:2], D, r=[ss])
            self.stt("dve", ht[:], xt[:], ss[:, 1:2], gm[r_][:], ALU.mult, ALU.mult, r=[xt, ss, gm[r_]], w=[ht])
            self.tt("pool", ht[:], ht[:], sh[r_][:], ALU.add, r=[ht, sh[r_]], w=[ht])
            if kind == "mix":
                bi = tt_ // 4
                tl = tt_ % 4
                if tl == 0:
                    cur = hTb.next()
                for g in range(4):
                    p = ps.next()
                    for q in range(4):
                        kc = g * 4 + q
                        self.tr(p[:, q * 128:(q + 1) * 128], ht[:, kc * 128:(kc + 1) * 128], ident[:],
                                r=[ht, ident], w=[p])
                    self.evac(cur[:, g * 4:(g + 1) * 4, tl * 128:(tl + 1) * 128],
                              p[:].rearrange("p (a b) -> p a b", b=128), r=[p], w=[cur])
                last_in_block = (tl == 3) or (tt_ == tiles[-1])
                if last_in_block:
                    t0 = bi * 512
                    nt = (tl + 1) * 128
                    k.dma("pool", self.HT.ap().rearrange("(kc p) t -> p kc t", p=128)[:, :, t0:t0 + nt],
                          cur[:, :, 0:nt], r=[cur], w=[self.HT])
            else:
                k.dma("pool", self.H[r0:r0 + 128, :], ht[:], r=[ht], w=[self.H])
                hT = hTt.next()
                for g in range(4):
                    p = ps.next()
                    for q in range(4):
                        kc = g * 4 + q
                        self.tr(p[:, q * 128:(q + 1) * 128], ht[:, kc * 128:(kc + 1) * 128], ident[:],
                                r=[ht, ident], w=[p])
                    self.evac(hT[:, g * 4:(g + 1) * 4, :], p[:].rearrange("p (a b) -> p a b", b=128), r=[p], w=[hT])
                p = ps.next()
                for kc in range(16):
                    self.mm(p[:, 0:NE], hT[:, kc, :], wr[:, kc, :], kc == 0, kc == 15, r=[hT, wr], w=[p])
                sm = smp.next()
                k.op("dve", lambda e, sm=sm, p=p: e.tensor_reduce(out=sm[:, 32:33], in_=p[:, 0:NE], axis=AX.X,
                                                                  op=ALU.max), r=[p], w=[sm])
                self.ts("dve", sm[:, 33:34], sm[:, 32:33], -1.0, None, ALU.mult, None, r=[sm], w=[sm])
                self.memset("pool", sm[:, 34:35], 0.0, w=[sm])
                self.act(sm[:, 0:NE], p[:, 0:NE], AF.Exp, r=[p, sm], w=[sm], bias=sm[:, 33:34], accum=sm[:, 34:35])
                self.recip(sm[:, 35:36], sm[:, 34:35], r=[sm], w=[sm])
                self.ts("dve", sm[:, 16:32], sm[:, 0:NE], sm[:, 35:36], None, ALU.mult, None, r=[sm], w=[sm])
                p2 = ps.next()
                self.tr(p2[0:NE, 0:128], sm[:, 16:32], ident[:], r=[sm, ident], w=[p2])
                self.evac(ST[:, r0:r0 + 128], p2[0:NE, 0:128], r=[p2], w=[ST])
        if kind == "ffn":
            self.topk(ST, ident, ps, with_ctx)
        k.end()

    def topk(self, ST, ident, ps, with_ctx):
        k = self.k
        work = k.tile([16, L], name="work")
        vals = k.tile([16, 384], name="vals")
        idxu = k.tile([16, 384], U32, name="idxu")
        idxf = k.tile([16, 384], name="idxf")
        self.memset("pool", vals[:], 0.0, w=[vals])
        self.load("sp", idxf[:, 256:384], self.padf.ap(), r=[], w=[idxf])
        self.cp("dve", work[:], ST[:, 0:L], r=[ST], w=[work])

        def run(wk, n, off):
            for it in range(n // 8):
                sl = slice(off + it * 8, off + it * 8 + 8)
                k.op("dve", lambda e, sl=sl: e.max(out=vals[:, sl], in_=wk), r=[work], w=[vals])
                k.op("dve", lambda e, sl=sl: e.max_index(out=idxu[:, sl], in_max=vals[:, sl], in_values=wk),
                     r=[work, vals], w=[idxu])
                k.op("dve", lambda e, sl=sl: e.match_replace(out=wk, in_to_replace=vals[:, sl], in_values=wk,
                                                             imm_value=-1.0), r=[work, vals], w=[work])

        run(work[:, 0:L], 256, 0)
        self.cp("dve", idxf[:, 0:256], idxu[:, 0:256], r=[idxu], w=[idxf])
        if with_ctx:
            self.cp("dve", work[:, 0:CL], ST[:, L:NT], r=[ST, work], w=[work])
            run(work[:, 0:CL], 32, 256)
            self.cp("dve", idxf[:, 256:288], idxu[:, 256:288], r=[idxu], w=[idxf])
            self.ts("dve", idxf[:, 256:288], idxf[:, 256:288], float(L), None, ALU.add, None, r=[idxf], w=[idxf])
        for c in range(3):
            p = ps.next()
            self.tr(p[:, 0:NE], idxf[:, c * 128:(c + 1) * 128], ident[0:16, 0:16], r=[idxf, ident], w=[p])
            it_ = k.tile([128, NE], I32, name="idxi")
            self.cp("dve", it_[:], p[:, 0:NE], r=[p], w=[it_])
            k.dma("pool", self.IDXD[c * 128:(c + 1) * 128, :], it_[:], r=[it_], w=[self.IDXD])
            p = ps.next()
            self.tr(p[:, 0:NE], vals[:, c * 128:(c + 1) * 128], ident[0:16, 0:16], r=[vals, ident], w=[p])
            vt = k.tile([128, NE], name="valt")
            self.cp("dve", vt[:], p[:, 0:NE], r=[p], w=[vt])
            k.dma("pool", self.TSD[c * 128:(c + 1) * 128, :], vt[:], r=[vt], w=[self.TSD])

    def ph_experts(self, i, with_ctx):
        k = self.k
        k.begin()
        ps = self.psums(8)
        ident = k.tile([128, 128], name="ident")
        self.load("sp", ident[:], self.ident.ap(), r=[self.ident], w=[ident])
        nct = 3 if with_ctx else 2
        NC_ = 288 if with_ctx else 256
        idx = [k.tile([128, NE], I32, name="idx") for _ in range(nct)]
        tsv = [k.tile([128, NE], name="tsv") for _ in range(nct)]
        for c in range(nct):
            self.load("sp", idx[c][:], self.IDXD[c * 128:(c + 1) * 128, :], r=[self.IDXD], w=[idx[c]])
            self.load("sp", tsv[c][:], self.TSD[c * 128:(c + 1) * 128, :], r=[self.TSD], w=[tsv[c]])
        g2 = []
        for r_ in range(2 if with_ctx else 1):
            g_ = k.tile([128, D], name="g2")
            self.bcload(g_, self.mod_row(i, r_, 5), r=[self.MOD])
            g2.append(g_)
        xs = [k.tile([128, D], name="xs") for _ in range(nct)]
        yo = [k.tile([128, D], name="yo") for _ in range(nct)]
        xsT = k.tile([128, 16, NC_], F32R, name="xsT")
        aT = k.tile([128, 8, NC_], F32R, name="aT")
        tmp = Rot([k.tile([128, NC_], name="tmp") for _ in range(2)])
        wp = Rot([k.tile([128, 4096], F32R, name="wexp") for _ in range(6)])
        csz = [128, 128, 32]
        if with_ctx:
            self.memset("pool", xs[2][:], 0.0, w=[xs[2]])
            self.memset("pool", yo[2][:], 0.0, w=[yo[2]])
        qi = 0

        def gather(e):
            for c in range(nct):
                k.dma("pool", None, None, r=[idx[c], self.H], w=[xs[c]],
                      fn=lambda eng, c=c, e=e: eng.indirect_dma_start(
                          out=xs[c][:], out_offset=None, in_=self.H.ap(),
                          in_offset=bass.IndirectOffsetOnAxis(ap=idx[c][:, e:e + 1], axis=0)))

        gather(0)
        for e in range(NE):
            for kc in range(16):
                p = ps.next()
                for c in range(nct):
                    if c < 2:
                        self.tr(p[:, c * 128:(c + 1) * 128], xs[c][:, kc * 128:(kc + 1) * 128], ident[:],
                                r=[xs[c], ident], w=[p])
                    else:
                        self.tr(p[:, 256:288], xs[c][0:32, kc * 128:(kc + 1) * 128], ident[0:32, 0:32],
                                r=[xs[c], ident], w=[p])
                self.evac(xsT[:, kc, :], p[:, 0:NC_], r=[p], w=[xsT])
            if e + 1 < NE:
                gather(e + 1)
            w1ap, w1res = self.wsl("moe_w1", i * NE + e)
            w3ap, w3res = self.wsl("moe_w3", i * NE + e)
            w2ap, w2res = self.wsl("moe_w2", i * NE + e)
            w1v = w1ap.rearrange("(kc p) f -> p kc f", p=128)
            w3v = w3ap.rearrange("(kc p) f -> p kc f", p=128)
            w2v = w2ap.rearrange("(fc p) d -> p fc d", p=128)
            for fq in range(4):
                w1q = wp.next()
                w3q = wp.next()
                w1a = w1q[:].rearrange("p (kc f) -> p kc f", f=256)
                w3a = w3q[:].rearrange("p (kc f) -> p kc f", f=256)
                qi += 1
                self.load("sp", w1a, w1v[:, :, fq * 256:(fq + 1) * 256].bitcast(F32R),
                          r=w1res, w=[w1q])
                self.load("sp", w3a, w3v[:, :, fq * 256:(fq + 1) * 256].bitcast(F32R),
                          r=w3res, w=[w3q])
                for fl in range(2):
                    fc = fq * 2 + fl
                    p1 = ps.next()
                    p3 = ps.next()
                    for kc in range(16):
                        self.mm(p1[:, 0:NC_], w1a[:, kc, fl * 128:(fl + 1) * 128], xsT[:, kc, :], kc == 0, kc == 15,
                                r=[w1q, xsT], w=[p1])
                    for kc in range(16):
                        self.mm(p3[:, 0:NC_], w3a[:, kc, fl * 128:(fl + 1) * 128], xsT[:, kc, :], kc == 0, kc == 15,
                                r=[w3q, xsT], w=[p3])
                    t_ = tmp.next()
                    self.act(t_[:], p1[:, 0:NC_], AF.Silu, r=[p1], w=[t_])
                    self.tt("dve", aT[:, fc, :], t_[:], p3[:, 0:NC_], ALU.mult, r=[t_, p3], w=[aT])
            for dq in range(4):
                w2q = wp.next()
                w2a = w2q[:].rearrange("p (fc d) -> p fc d", d=512)
                qi += 1
                self.load("sp", w2a, w2v[:, :, dq * 512:(dq + 1) * 512].bitcast(F32R),
                          r=w2res, w=[w2q])
                for c in range(nct):
                    cs_ = csz[c]
                    p = ps.next()
                    for fc in range(8):
                        self.mm(p[0:cs_, :], aT[:, fc, c * 128:c * 128 + cs_], w2a[:, fc, :], fc == 0, fc == 7,
                                r=[aT, w2q], w=[p])
                    g_ = g2[1 if c == 2 else 0]
                    self.stt("dve", yo[c][0:cs_, dq * 512:(dq + 1) * 512], p[0:cs_, :], tsv[c][0:cs_, e:e + 1],
                             g_[0:cs_, dq * 512:(dq + 1) * 512], ALU.mult, ALU.mult, r=[p, tsv[c], g_], w=[yo[c]])
            for c in range(nct):
                k.dma("pool", None, None, r=[idx[c], yo[c]], w=[self.X],
                      fn=lambda eng, c=c, e=e: eng.indirect_dma_start(
                          out=self.X.ap(), out_offset=bass.IndirectOffsetOnAxis(ap=idx[c][:, e:e + 1], axis=0),
                          in_=yo[c][:], in_offset=None, compute_op=ALU.add))
        k.end()

    def ph_gla_proj(self, j, with_ctx=True):
        k = self.k
        k.begin()
        ps = self.psums(8)
        hTp = Rot([k.tile([128, 16, 512], F32R, name="hT") for _ in range(2)])
        wp = Rot([k.tile([128, 16, 512], F32R, name="w") for _ in range(3)])
        op_ = Rot([k.tile([128, 512], name="o") for _ in range(6)])
        wa1 = k.tile([128, 16, 32], F32R, name="wa1")
        wa2 = k.tile([33, 2048], name="wa2")
        lowT = k.tile([33, 512], name="lowT")
        self.load("sp", wa1[:], self.gla_wa1[j].rearrange("(kc p) r -> p kc r", p=128).bitcast(F32R),
                  r=[self.gla_wa1], w=[wa1])
        self.load("sp", wa2[:], self.gla_wa2[j], r=[self.gla_wa2], w=[wa2])
        self.memset("pool", lowT[:], 1.0, w=[lowT])
        wap, wres = self.wsl("gla_w_in", j)
        wv = wap.rearrange("(kc p) n -> p kc n", p=128)
        HTv = self.HT.ap().rearrange("(kc p) t -> p kc t", p=128)
        qn = 0
        for (t0, TB) in self.blocks(with_ctx):
            hT = hTp.next()
            self.load("sp", hT[:, :, 0:TB], HTv[:, :, t0:t0 + TB].bitcast(F32R), r=[self.HT], w=[hT])
            for nb in range(12):
                wt = wp.next()
                qn += 1
                self.load("sp", wt[:], wv[:, :, nb * 512:(nb + 1) * 512].bitcast(F32R),
                          r=wres, w=[wt])
                if nb < 4:
                    dst = self.QT if nb < 2 else self.KT
                    for nl in range(4):
                        p = ps.next()
                        for kc in range(16):
                            self.mm(p[:, 0:TB], wt[:, kc, nl * 128:(nl + 1) * 128], hT[:, kc, 0:TB], kc == 0, kc == 15,
                                    r=[wt, hT], w=[p])
                        o = op_.next()
                        if nb < 2:
                            self.act(o[:, 0:TB], p[:, 0:TB], AF.Copy, r=[p], w=[o], scale=DK ** -0.5)
                        else:
                            self.evac(o[:, 0:TB], p[:, 0:TB], r=[p], w=[o])
                        f0 = ((nb % 2) * 4 + nl) * 128
                        k.dma("pool", dst[f0:f0 + 128, t0:t0 + TB], o[:, 0:TB], r=[o], w=[dst])
                else:
                    dst = self.V if nb < 8 else self.G
                    c0 = ((nb - 4) % 4) * 512
                    for tl in range(TB // 128):
                        p = ps.next()
                        for kc in range(16):
                            self.mm(p[:], hT[:, kc, tl * 128:(tl + 1) * 128], wt[:, kc, :], kc == 0, kc == 15,
                                    r=[wt, hT], w=[p])
                        o = op_.next()
                        if nb < 8:
                            self.evac(o[:], p[:], r=[p], w=[o])
                        else:
                            self.act(o[:], p[:], AF.Silu, r=[p], w=[o])
                        r0 = t0 + tl * 128
                        k.dma("pool", dst[r0:r0 + 128, c0:c0 + 512], o[:], r=[o], w=[dst])
            p = ps.next()
            for kc in range(16):
                self.mm(p[0:32, 0:TB], wa1[:, kc, :], hT[:, kc, 0:TB], kc == 0, kc == 15, r=[wa1, hT], w=[p])
            self.evac(lowT[0:32, 0:TB], p[0:32, 0:TB], r=[p], w=[lowT])
            for tl in range(TB // 128):
                for cb in range(4):
                    p = ps.next()
                    self.mm(p[:], lowT[0:33, tl * 128:(tl + 1) * 128], wa2[0:33, cb * 512:(cb + 1) * 512], True, True,
                            r=[lowT, wa2], w=[p])
                    o = op_.next()
                    self.act(o[:], p[:], AF.Exp, r=[p], w=[o], scale=-1.0)
                    self.act(o[:], o[:], AF.Ln, r=[o], w=[o], bias=1.0)
                    r0 = t0 + tl * 128
                    k.dma("pool", self.U[r0:r0 + 128, cb * 512:(cb + 1) * 512], o[:], r=[o], w=[self.U])
        k.end()

    def ph_gla_scan(self, j):
        k = self.k
        k.begin()
        ps = self.psums(8)
        ident = k.tile([128, 128], name="ident")
        self.load("sp", ident[:], self.ident.ap(), r=[self.ident], w=[ident])
        tri = [k.tile([128, 128], name="tri") for _ in range(2)]
        msk = [k.tile([128, 128], name="msk") for _ in range(2)]
        for z in range(2):
            self.load("sp", tri[z][:], self.tri[z], r=[self.tri], w=[tri[z]])
            self.load("sp", msk[z][:], self.msk[z], r=[self.msk], w=[msk[z]])
        hn = k.tile([128, DV], name="hn")
        self.bcload(hn, self.gla_hn[j:j + 1, :], r=[self.gla_hn])
        oacc = k.tile([128, 18, DV], name="oacc")
        S = [k.tile([128, DV], F32R, name="S") for _ in range(2)]
        zer = k.tile([128, DV], name="zer")
        self.memset("pool", zer[:], 0.0, w=[zer])
        qp = Rot([k.tile([128, 2, 128], name="q") for _ in range(3)])
        kp = Rot([k.tile([128, 2, 128], name="k") for _ in range(3)])
        vp = Rot([k.tile([128, DV], F32R, name="v") for _ in range(3)])
        up = Rot([k.tile([128, 256], name="u") for _ in range(3)])
        ebp = Rot([k.tile([128, 256], name="eb") for _ in range(2)])
        enp = Rot([k.tile([128, 256], name="en") for _ in range(2)])
        qsp = Rot([k.tile([128, 2, 128], F32R, name="qs") for _ in range(2)])
        ksp = Rot([k.tile([128, 2, 128], F32R, name="ks") for _ in range(2)])
        ktp = Rot([k.tile([128, 256], F32R, name="kt") for _ in range(2)])
        atp = Rot([k.tile([128, 128], F32R, name="at") for _ in range(2)])
        tsp = Rot([k.tile([128, DV], name="tS") for _ in range(2)])
        gp = Rot([k.tile([128, DV], name="g") for _ in range(2)])
        onp = Rot([k.tile([128, DV], name="on") for _ in range(2)])
        junk = k.tile([128, DV], name="junk")
        ssp = Rot([k.tile([128, 2], name="ss") for _ in range(3)])
        QTv = self.QT.ap().rearrange("(c p) t -> p c t", p=128)
        KTv = self.KT.ap().rearrange("(c p) t -> p c t", p=128)
        for h in range(HEADS):
            for z in range(2):
                order = [16, 17] + list(range(16)) if z == 0 else [17, 16] + list(range(15, -1, -1))
                last = 127 if z == 0 else 0
                for dc in range(2):
                    self.cp("dve", S[dc][:], zer[:], r=[zer], w=[S[dc]])
                for ti in order:
                    r0 = ti * 128
                    q_ = qp.next()
                    k_ = kp.next()
                    v_ = vp.next()
                    u_ = up.next()
                    self.load("sp", q_[:], QTv[:, 2 * h:2 * h + 2, r0:r0 + 128], r=[self.QT], w=[q_])
                    self.load("sp", k_[:], KTv[:, 2 * h:2 * h + 2, r0:r0 + 128], r=[self.KT], w=[k_])
                    self.load("sp", v_[:], self.V[r0:r0 + 128, h * DV:(h + 1) * DV].bitcast(F32R), r=[self.V], w=[v_])
                    c0 = z * 1024 + h * DK
                    self.load("sp", u_[:], self.U[r0:r0 + 128, c0:c0 + DK], r=[self.U], w=[u_])
                    pb = ps.next()
                    for dc in range(2):
                        self.mm(pb[:, dc * 128:(dc + 1) * 128], u_[:, dc * 128:(dc + 1) * 128], tri[z][:], True, True,
                                r=[u_, tri[z]], w=[pb])
                    eb = ebp.next()
                    en = enp.next()
                    self.act(eb[:], pb[:, 0:256], AF.Exp, r=[pb], w=[eb])
                    self.act(en[:], pb[:, 0:256], AF.Exp, r=[pb], w=[en], scale=-1.0)
                    qs = qsp.next()
                    ks = ksp.next()
                    self.tt("dve", qs[:], q_[:], eb[:].rearrange("p (c t) -> p c t", t=128), ALU.mult, r=[q_, eb], w=[qs])
                    self.tt("pool", ks[:], k_[:], en[:].rearrange("p (c t) -> p c t", t=128), ALU.mult, r=[k_, en], w=[ks])
                    pt = ps.next()
                    for dc in range(2):
                        self.tr(pt[:, dc * 128:(dc + 1) * 128], ks[:, dc, :].bitcast(F32), ident[:], r=[ks, ident], w=[pt])
                    kt = ktp.next()
                    self.evac(kt[:], pt[:, 0:256], r=[pt], w=[kt])
                    pa = ps.next()
                    for dc in range(2):
                        self.mm(pa[:, 0:128], ks[:, dc, :], qs[:, dc, :], dc == 0, dc == 1, r=[ks, qs], w=[pa])
                    at = atp.next()
                    self.tt("dve", at[:], pa[:, 0:128], msk[z][:], ALU.mult, r=[pa, msk[z]], w=[at])
                    po = ps.next()
                    self.mm(po[:], at[:], v_[:], True, False, r=[at, v_], w=[po])
                    for dc in range(2):
                        self.mm(po[:], qs[:, dc, :], S[dc][:], False, dc == 1, r=[qs, S[dc]], w=[po])
                    if z == 0:
                        self.evac(oacc[:, ti, :], po[:], r=[po], w=[oacc])
                    else:
                        self.tt("dve", oacc[:, ti, :], oacc[:, ti, :], po[:], ALU.add, r=[oacc, po], w=[oacc])
                    for dc in range(2):
                        p_s = ps.next()
                        self.mm(p_s[:], kt[:, dc * 128:(dc + 1) * 128], v_[:], True, True, r=[kt, v_], w=[p_s])
                        tS = tsp.next()
                        self.tt("dve", tS[:], S[dc][:].bitcast(F32), p_s[:], ALU.add, r=[S[dc], p_s], w=[tS])
                        col = dc * 128 + last
                        self.act(S[dc][:], tS[:], AF.Copy, r=[tS, eb], w=[S[dc]], scale=eb[:, col:col + 1])
            for ti in range(18):
                r0 = ti * 128
                ss = ssp.next()
                g_ = gp.next()
                on = onp.next()
                self.load("sp", g_[:], self.G[r0:r0 + 128, h * DV:(h + 1) * DV], r=[self.G], w=[g_])
                self.memset("pool", ss[:], 0.0, w=[ss])
                self.act(junk[:], oacc[:, ti, :], AF.Square, r=[oacc], w=[junk, ss], accum=ss[:, 0:1])
                self.rstd(ss[:, 0:1], ss[:, 1:2], DV, r=[ss])
                self.stt("dve", on[:], oacc[:, ti, :], ss[:, 1:2], hn[:], ALU.mult, ALU.mult, r=[oacc, ss, hn], w=[on])
                self.tt("pool", on[:], on[:], g_[:], ALU.mult, r=[on, g_], w=[on])
                k.dma("pool", self.OG[r0:r0 + 128, h * DV:(h + 1) * DV], on[:], r=[on], w=[self.OG])
        k.end()

    def ph_outproj(self, i, wname, wunit, src, src_major, with_ctx, bias_row=None):
        k = self.k
        k.begin()
        ps = self.psums(8)
        ident = k.tile([128, 128], name="ident")
        self.load("sp", ident[:], self.ident.ap(), r=[self.ident], w=[ident])
        g1 = []
        for r_ in range(2 if with_ctx else 1):
            g_ = k.tile([128, D], name="g1")
            self.bcload(g_, self.mod_row(i, r_, 2), r=[self.MOD])
            g1.append(g_)
        bo = None
        if bias_row is not None:
            bo = k.tile([128, D], name="bo")
            self.bcload(bo, bias_row, r=[self.hy_b_out])
        sT = k.tile([128, 16, 512], F32R, name="sT")
        wp = Rot([k.tile([128, 16, 512], F32R, name="w") for _ in range(2)])
        xt = [k.tile([128, D], name="xt") for _ in range(4)]
        sp_ = Rot([k.tile([128, D], name="st") for _ in range(2)])
        tp = Rot([k.tile([128, 512], name="tp") for _ in range(3)])
        wap, wres = self.wsl(wname, wunit)
        wv = wap.rearrange("(kc p) n -> p kc n", p=128)
        qn = 0
        for (t0, TB) in self.blocks(with_ctx):
            r_ = 1 if t0 >= L else 0
            ntl = TB // 128
            if src_major == "tok":
                for tl in range(ntl):
                    st = sp_.next()
                    r0 = t0 + tl * 128
                    self.load("sp", st[:], src[r0:r0 + 128, :], r=[src], w=[st])
                    for g in range(4):
                        p = ps.next()
                        for q in range(4):
                            kc = g * 4 + q
                            self.tr(p[:, q * 128:(q + 1) * 128], st[:, kc * 128:(kc + 1) * 128], ident[:],
                                    r=[st, ident], w=[p])
                        self.evac(sT[:, g * 4:(g + 1) * 4, tl * 128:(tl + 1) * 128],
                                  p[:].rearrange("p (a b) -> p a b", b=128), r=[p], w=[sT])
            else:
                self.load("sp", sT[:, :, 0:TB],
                          src.ap().rearrange("(kc p) t -> p kc t", p=128)[:, :, t0:t0 + TB].bitcast(F32R),
                          r=[src], w=[sT])
            for tl in range(ntl):
                r0 = t0 + tl * 128
                self.load("sp", xt[tl][:], self.X[r0:r0 + 128, :], r=self.X.rows(r0, r0 + 128), w=[xt[tl]])
            for nb in range(4):
                wt = wp.next()
                qn += 1
                self.load("sp", wt[:], wv[:, :, nb * 512:(nb + 1) * 512].bitcast(F32R),
                          r=wres, w=[wt])
                for tl in range(ntl):
                    p = ps.next()
                    for kc in range(16):
                        self.mm(p[:], sT[:, kc, tl * 128:(tl + 1) * 128], wt[:, kc, :], kc == 0, kc == 15,
                                r=[sT, wt], w=[p])
                    t_ = tp.next()
                    sl = slice(nb * 512, (nb + 1) * 512)
                    if bo is not None:
                        self.tt("dve", t_[:], p[:], bo[:, sl], ALU.add, r=[p, bo], w=[t_])
                        self.tt("pool", t_[:], t_[:], g1[r_][:, sl], ALU.mult, r=[t_, g1[r_]], w=[t_])
                    else:
                        self.tt("dve", t_[:], p[:], g1[r_][:, sl], ALU.mult, r=[p, g1[r_]], w=[t_])
                    self.tt("pool", xt[tl][:, sl], xt[tl][:, sl], t_[:], ALU.add, r=[xt[tl], t_], w=[xt[tl]])
            for tl in range(ntl):
                r0 = t0 + tl * 128
                k.dma("pool", self.X[r0:r0 + 128, :], xt[tl][:], r=[xt[tl]], w=self.X.rows(r0, r0 + 128))
        k.end()

    def ph_hy_filter(self, j, Ls, row0, rn_row0):
        k = self.k
        k.begin()
        ps = self.psums(6)
        MT = Ls // 128
        zT = k.tile([33, Ls], name="zT")
        self.load("sp", zT[:], self.zT[Ls].ap(), r=[self.zT[Ls]], w=[zT])
        w1 = k.tile([33, 64], name="fw1")
        w2 = k.tile([64, 64], name="fw2")
        w3 = k.tile([64, 64], name="fw3")
        w4 = k.tile([64, 2 * D], name="fw4")
        fb = k.tile([64, 3], name="fb")
        fb2 = k.tile([64, 3], name="fb2")
        self.load("sp", w1[:], self.hy_f_w1[j], r=[self.hy_f_w1], w=[w1])
        self.load("sp", w2[:], self.hy_f_w2[j], r=[self.hy_f_w2], w=[w2])
        self.load("sp", w3[:], self.hy_f_w3[j], r=[self.hy_f_w3], w=[w3])
        self.load("sp", w4[:], self.hy_f_w4[j], r=[self.hy_f_w4], w=[w4])
        self.load("sp", fb[:], self.hy_fb[j], r=[self.hy_fb], w=[fb])
        OFF = 16.0
        self.ts("dve", fb2[:], fb[:], 1.0 / (2 * math.pi), 0.5 + OFF, ALU.mult, ALU.add, r=[fb], w=[fb2])
        hT = [k.tile([64, Ls], name="hid") for _ in range(3)]
        up = Rot([k.tile([64, 512], name="u") for _ in range(2)])
        uip = Rot([k.tile([64, 512], I32, name="ui") for _ in range(2)])
        ufp = Rot([k.tile([64, 512], name="uf") for _ in range(2)])
        SE = 1e-6
        srcs = [(zT, 33, w1), (hT[0], 64, w2), (hT[1], 64, w3)]
        for li in range(3):
            src, K_, w_ = srcs[li]
            for lb in range((Ls + 511) // 512):
                wdt = min(512, Ls - lb * 512)
                sl = slice(lb * 512, lb * 512 + wdt)
                p = ps.next()
                self.mm(p[0:64, 0:wdt], w_[0:K_, :], src[0:K_, sl], True, True, r=[w_, src], w=[p])
                u = up.next()
                ui = uip.next()
                uf = ufp.next()
                self.act(u[:, 0:wdt], p[0:64, 0:wdt], AF.Identity, r=[p, fb2], w=[u], scale=1.0 / (2 * math.pi),
                         bias=fb2[:, li:li + 1])
                self.cp("dve", ui[:, 0:wdt], u[:, 0:wdt], r=[u], w=[ui])
                self.cp("dve", uf[:, 0:wdt], ui[:, 0:wdt], r=[ui], w=[uf])
                self.tt("dve", u[:, 0:wdt], u[:, 0:wdt], uf[:, 0:wdt], ALU.subtract, r=[u, uf], w=[u])
                self.stt("dve", uf[:, 0:wdt], u[:, 0:wdt], 0.0, u[:, 0:wdt], ALU.is_lt, ALU.add, r=[u], w=[uf])
                self.act(hT[li][:, sl], uf[:, 0:wdt], AF.Sin, r=[uf], w=[hT[li]], scale=2 * math.pi * (1 - SE),
                         bias=-math.pi * (1 - SE))
        negt = k.tile([128, MT], name="negt")
        self.load("sp", negt[:], self.negt[Ls].ap(), r=[self.negt[Ls]], w=[negt])
        dl = k.tile([128, D], name="delta")
        self.bcload(dl, self.delta[0:1, :], r=[self.delta])
        ones = k.tile([128, 1], name="ones")
        self.memset("pool", ones[:], 1.0, w=[ones])
        absum = k.tile([128, D], name="absum")
        self.memset("pool", absum[:], 0.0, w=[absum])
        decp = Rot([k.tile([128, D], name="dec") for _ in range(2)])
        hfp = Rot([k.tile([128, D], name="hf") for _ in range(2)])
        hbp = Rot([k.tile([128, D], name="hb") for _ in range(2)])
        hsp = Rot([k.tile([128, D], name="hs") for _ in range(2)])
        hdp = Rot([k.tile([128, D], name="hd") for _ in range(2)])
        a1 = k.tile([128, D], name="a1")
        a2 = k.tile([128, D], name="a2")
        for mt in range(MT):
            dec = decp.next()
            hf = hfp.next()
            hb = hbp.next()
            hs = hsp.next()
            hd = hdp.next()
            self.act(dec[:], dl[:], AF.Exp, r=[dl, negt], w=[dec], scale=negt[:, mt:mt + 1])
            for nb in range(8):
                p = ps.next()
                self.mm(p[:], hT[2][0:64, mt * 128:(mt + 1) * 128], w4[0:64, nb * 512:(nb + 1) * 512], True, True,
                        r=[hT[2], w4], w=[p])
                dst = hf if nb < 4 else hb
                sl = slice((nb % 4) * 512, (nb % 4 + 1) * 512)
                self.tt("dve", dst[:, sl], p[:], dec[:, sl], ALU.mult, r=[p, dec], w=[dst])
            if mt == 0:
                self.memset("pool", hb[0:1, :], 0.0, w=[hb])
            self.tt("pool", hs[:], hf[:], hb[:], ALU.add, r=[hf, hb], w=[hs])
            self.tt("pool", hd[:], hf[:], hb[:], ALU.subtract, r=[hf, hb], w=[hd])
            r0 = row0 + mt * 128
            k.dma("pool", self.HS[r0:r0 + 128, :], hs[:], r=[hs], w=[self.HS])
            k.dma("pool", self.HD[r0:r0 + 128, :], hd[:], r=[hd], w=[self.HD])
            self.act(a1[:], hf[:], AF.Abs, r=[hf], w=[a1])
            self.act(a2[:], hb[:], AF.Abs, r=[hb], w=[a2])
            self.tt("dve", a1[:], a1[:], a2[:], ALU.add, r=[a1, a2], w=[a1])
            self.tt("dve", absum[:], absum[:], a1[:], ALU.add, r=[absum, a1], w=[absum])
        pn = ps.next()
        for dc in range(16):
            self.mm(pn[:, dc:dc + 1], absum[:, dc * 128:(dc + 1) * 128], ones[:, 0:1], True, True, r=[absum, ones], w=[pn])
        rn = k.tile([128, 16], name="rn")
        self.recip(rn[:], pn[:, 0:16], r=[pn], w=[rn])
        k.dma("pool", self.RN[rn_row0:rn_row0 + 128, :], rn[:], r=[rn], w=[self.RN])
        k.end()

    def ph_dft(self, A, a_row0, Ls, tname, OUT, out_row0, scale_wn):
        k = self.k
        k.begin()
        ps = self.psums(8)
        TC = Ls // 128
        FC = TC + 1
        wn = None
        if scale_wn:
            wn = k.tile([128, FC], name="wn")
            self.load("sp", wn[:], self.wn[Ls].ap(), r=[self.wn[Ls]], w=[wn])
        At = k.tile([128, TC, 1024], F32R, name="At")
        tbp = Rot([k.tile([128, TC, 128], F32R, name="tab") for _ in range(5)])
        otp = Rot([k.tile([128, 1024], name="ot") for _ in range(4)])
        Av = A.ap()[a_row0:a_row0 + Ls, :].rearrange("(tc p) d -> p tc d", p=128)
        qn = 0
        for db in range(2):
            self.load("sp", At[:], Av[:, :, db * 1024:(db + 1) * 1024].bitcast(F32R), r=[A], w=[At])
            for fc in range(FC):
                M = 128 if fc < TC else 1
                tab = tbp.next()
                qn += 1
                if Ls == L:
                    tap, tres = self.wsl(tname + "_l", fc)
                else:
                    tc_t = self.cf_c if tname == "cf" else self.sf_c
                    tap, tres = tc_t.ap()[fc * 128:(fc + 1) * 128, :], []
                self.load("sp", tab[:],
                          tap.rearrange("p (tc j) -> p tc j", j=128).bitcast(F32R), r=tres, w=[tab])
                ot = otp.next()
                for half in range(2):
                    p = ps.next()
                    for tc in range(TC):
                        self.mm(p[0:M, :], tab[:, tc, 0:M], At[:, tc, half * 512:(half + 1) * 512], tc == 0, tc == TC - 1,
                                r=[tab, At], w=[p])
                    if wn is not None:
                        self.act(ot[0:M, half * 512:(half + 1) * 512], p[0:M, :], AF.Copy, r=[p, wn], w=[ot],
                                 scale=wn[0:M, fc:fc + 1])
                    else:
                        self.evac(ot[0:M, half * 512:(half + 1) * 512], p[0:M, :], r=[p], w=[ot])
                r0 = out_row0 + fc * 128
                k.dma("pool", OUT[r0:r0 + M, db * 1024:(db + 1) * 1024], ot[0:M, :], r=[ot], w=[OUT])
        k.end()

    def ph_hy_proj(self, j, with_ctx):
        k = self.k
        k.begin()
        ps = self.psums(8)
        ident = k.tile([128, 128], name="ident")
        self.load("sp", ident[:], self.ident.ap(), r=[self.ident], w=[ident])
        cols = k.tile([128, 48, 5], name="cols")
        self.load("sp", cols[:], self.hy_cols[j].rearrange("p (n c) -> p n c", c=5), r=[self.hy_cols], w=[cols])
        hT = k.tile([128, 16, 512], F32R, name="hT")
        wp = Rot([k.tile([128, 16, 256], F32R, name="w") for _ in range(7)])
        utok = k.tile([128, 4, D], name="utok")
        pp = Rot([k.tile([128, 512], name="p") for _ in range(3)])
        yp = Rot([k.tile([128, 512], name="y") for _ in range(6)])
        uu = Rot([k.tile([128, 512], name="uu") for _ in range(2)])
        wap, wres = self.wsl("hy_w_in", j)
        wv = wap.rearrange("(kc p) n -> p kc n", p=128)
        HTv = self.HT.ap().rearrange("(kc p) t -> p kc t", p=128)
        qn = 0
        for (t0, TB) in self.blocks(with_ctx):
            ntl = TB // 128
            grid = t0 < L
            self.load("sp", hT[:, :, 0:TB], HTv[:, :, t0:t0 + TB].bitcast(F32R), r=[self.HT], w=[hT])
            for jb in range(8):
                wts = []
                for sec in range(3):
                    wt = wp.next()
                    qn += 1
                    c0 = sec * D + jb * 256
                    self.load("sp", wt[:], wv[:, :, c0:c0 + 256].bitcast(F32R),
                              r=wres, w=[wt])
                    wts.append(wt)
                for jl in range(2):
                    jc = jb * 2 + jl
                    ys = []
                    for sec in range(3):
                        nc_ = sec * 16 + jc
                        p = ps.next()
                        for kc in range(16):
                            self.mm(p[:, 0:TB], wts[sec][:, kc, jl * 128:(jl + 1) * 128], hT[:, kc, 0:TB], kc == 0,
                                    kc == 15, r=[wts[sec], hT], w=[p])
                        pt = pp.next()
                        y = yp.next()
                        self.act(pt[:, 0:TB], p[:, 0:TB], AF.Identity, r=[p, cols], w=[pt], bias=cols[:, nc_, 0:1])
                        self.act(y[:, 0:TB], pt[:, 0:TB], AF.Identity, r=[pt, cols], w=[y], scale=cols[:, nc_, 2:3],
                                 bias=cols[:, nc_, 4:5])
                        if grid:
                            pv = pt[:, 0:TB].rearrange("p (r w) -> p r w", w=64)
                            yv = y[:, 0:TB].rearrange("p (r w) -> p r w", w=64)
                            self.stt("dve", yv[:, :, 1:64], pv[:, :, 0:63], cols[:, nc_, 1:2], yv[:, :, 1:64], ALU.mult,
                                     ALU.add, r=[pt, y, cols], w=[y])
                            self.stt("dve", yv[:, :, 0:63], pv[:, :, 1:64], cols[:, nc_, 3:4], yv[:, :, 0:63], ALU.mult,
                                     ALU.add, r=[pt, y, cols], w=[y])
                        else:
                            self.stt("dve", y[:, 1:TB], pt[:, 0:TB - 1], cols[:, nc_, 1:2], y[:, 1:TB], ALU.mult,
                                     ALU.add, r=[pt, y, cols], w=[y])
                            self.stt("dve", y[:, 0:TB - 1], pt[:, 1:TB], cols[:, nc_, 3:4], y[:, 0:TB - 1], ALU.mult,
                                     ALU.add, r=[pt, y, cols], w=[y])
                        ys.append(y)
                    k.dma("pool", self.X0T[jc * 128:(jc + 1) * 128, t0:t0 + TB], ys[0][:, 0:TB], r=[ys[0]], w=[self.X0T])
                    u = uu.next()
                    self.tt("pool", u[:, 0:TB], ys[1][:, 0:TB], ys[2][:, 0:TB], ALU.mult, r=[ys[1], ys[2]], w=[u])
                    k.dma("pool", self.UT[jc * 128:(jc + 1) * 128, t0:t0 + TB], u[:, 0:TB], r=[u], w=[self.UT])
                    p = ps.next()
                    for tl in range(ntl):
                        self.tr(p[:, tl * 128:(tl + 1) * 128], u[:, tl * 128:(tl + 1) * 128], ident[:], r=[u, ident], w=[p])
                    self.evac(utok[:, 0:ntl, jc * 128:(jc + 1) * 128],
                              p[:, 0:TB].rearrange("p (a b) -> p a b", b=128), r=[p], w=[utok])
            k.dma("pool", self.UTOK.ap()[t0:t0 + TB, :].rearrange("(a p) d -> p a d", p=128), utok[:, 0:ntl, :],
                  r=[utok], w=[self.UTOK])
        k.end()

    def ph_hy_inv(self, j, Ls, tok0, frow0, rn_row0):
        k = self.k
        k.begin()
        ps = self.psums(8)
        TC = Ls // 128
        FC = TC + 1
        GS = min(4, TC)
        NG = TC // GS
        TBW = min(512, Ls)
        NTB = Ls // TBW
        rn = k.tile([128, 16], name="rn")
        skc = k.tile([128, 16], name="skc")
        self.load("sp", rn[:], self.RN[rn_row0:rn_row0 + 128, :], r=[self.RN], w=[rn])
        self.load("sp", skc[:], self.hy_skipc[j], r=[self.hy_skipc], w=[skc])
        YR = k.tile([128, FC, 512], F32R, name="YR")
        YI = k.tile([128, FC, 512], F32R, name="YI")
        ldp = Rot([k.tile([128, 4, 512], name="ld") for _ in range(2)])
        tmp = Rot([k.tile([128, 4, 512], name="tm") for _ in range(2)])
        cip = Rot([k.tile([128, GS, TBW], F32R, name="ci") for _ in range(2)])
        nsp = Rot([k.tile([128, GS, TBW], F32R, name="nsi") for _ in range(2)])
        cnp = Rot([k.tile([1, TBW], F32R, name="cin") for _ in range(2)])
        e1 = Rot([k.tile([128, TBW], name="e1") for _ in range(2)])
        e2 = Rot([k.tile([128, TBW], name="e2") for _ in range(2)])
        e3 = Rot([k.tile([128, TBW], name="e3") for _ in range(2)])
        e4 = Rot([k.tile([128, TBW], name="e4") for _ in range(2)])
        for db in range(4):
            dsl = slice(db * 512, (db + 1) * 512)
            for fc in range(FC):
                M = 128 if fc < TC else 1
                r0 = frow0 + fc * 128
                ld = ldp.next()
                tm = tmp.next()
                for n_, src in enumerate((self.URE, self.UIM, self.KRE, self.KIM)):
                    self.load("sp", ld[0:M, n_, :], src[r0:r0 + M, dsl], r=[src], w=[ld])
                ur, ui, kr, ki = (ld[0:M, n_, :] for n_ in range(4))
                self.tt("dve", tm[0:M, 0, :], ur, kr, ALU.mult, r=[ld], w=[tm])
                self.tt("pool", tm[0:M, 1, :], ui, ki, ALU.mult, r=[ld], w=[tm])
                self.tt("pool", tm[0:M, 2, :], ur, ki, ALU.mult, r=[ld], w=[tm])
                self.tt("dve", tm[0:M, 3, :], ui, kr, ALU.mult, r=[ld], w=[tm])
                self.tt("dve", YR[0:M, fc, :], tm[0:M, 0, :], tm[0:M, 1, :], ALU.subtract, r=[tm], w=[YR])
                self.tt("pool", YI[0:M, fc, :], tm[0:M, 2, :], tm[0:M, 3, :], ALU.add, r=[tm], w=[YI])
            for tb in range(NTB):
                pd = [ps.next() for _ in range(4)]
                for g in range(NG):
                    ci = cip.next()
                    nsi = nsp.next()
                    if Ls == L:
                        ciap, cires = self.wsl("ci_l", tb * 4 + g)
                        nsap, nsres = self.wsl("nsi_l", tb * 4 + g)
                    else:
                        ciap, cires = self.ci_c.ap(), []
                        nsap, nsres = self.nsi_c.ap(), []
                    self.load("sp", ci[:], ciap.rearrange("p (f t) -> p f t", t=TBW).bitcast(F32R), r=cires, w=[ci])
                    self.load("sp", nsi[:], nsap.rearrange("p (f t) -> p f t", t=TBW).bitcast(F32R), r=nsres, w=[nsi])
                    for dc in range(4):
                        for fl in range(GS):
                            fc = g * GS + fl
                            self.mm(pd[dc][:, 0:TBW], YR[:, fc, dc * 128:(dc + 1) * 128], ci[:, fl, :],
                                    g == 0 and fl == 0, False, r=[YR, ci], w=[pd[dc]])
                            self.mm(pd[dc][:, 0:TBW], YI[:, fc, dc * 128:(dc + 1) * 128], nsi[:, fl, :], False, False,
                                    r=[YI, nsi], w=[pd[dc]])
                cn = cnp.next()
                self.load("sp", cn[:], self.cin[Ls][0:1, tb * TBW:(tb + 1) * TBW].bitcast(F32R), r=[self.cin[Ls]], w=[cn])
                for dc in range(4):
                    self.mm(pd[dc][:, 0:TBW], YR[0:1, TC, dc * 128:(dc + 1) * 128], cn[0:1, :], False, True,
                            r=[YR, cn], w=[pd[dc]])
                    dch = db * 4 + dc
                    c0 = tok0 + tb * TBW
                    x0t = e1.next()
                    ut = e2.next()
                    t1 = e3.next()
                    t2 = e4.next()
                    self.load("sp", x0t[:], self.X0T[dch * 128:(dch + 1) * 128, c0:c0 + TBW], r=[self.X0T], w=[x0t])
                    self.load("sp", ut[:], self.UT[dch * 128:(dch + 1) * 128, c0:c0 + TBW], r=[self.UT], w=[ut])
                    self.act(t1[:], pd[dc][:, 0:TBW], AF.Copy, r=[pd[dc], rn], w=[t1], scale=rn[:, dch:dch + 1])
                    self.stt("dve", t2[:], ut[:], skc[:, dch:dch + 1], t1[:], ALU.mult, ALU.add, r=[ut, skc, t1], w=[t2])
                    self.tt("pool", t2[:], t2[:], x0t[:], ALU.mult, r=[t2, x0t], w=[t2])
                    k.dma("pool", self.YHT[dch * 128:(dch + 1) * 128, c0:c0 + TBW], t2[:], r=[t2], w=[self.YHT])
        k.end()

    def ph_final(self):
        k = self.k
        k.begin()
        gam = k.tile([128, D], name="gam")
        self.bcload(gam, self.norm_final[0:1, :], r=[self.norm_final])
        xp = Rot([k.tile([128, D], name="xt") for _ in range(3)])
        op_ = Rot([k.tile([128, D], name="ot") for _ in range(3)])
        junk = k.tile([128, D], name="junk")
        ssp = Rot([k.tile([128, 2], name="ss") for _ in range(3)])
        for tt_ in range(16):
            r0 = tt_ * 128
            xt = xp.next()
            ot = op_.next()
            ss = ssp.next()
            self.load("sp", xt[:], self.X[r0:r0 + 128, :], r=self.X.rows(r0, r0 + 128), w=[xt])
            self.memset("pool", ss[:], 0.0, w=[ss])
            self.act(junk[:], xt[:], AF.Square, r=[xt], w=[junk, ss], accum=ss[:, 0:1])
            self.rstd(ss[:, 0:1], ss[:, 1:2], D, r=[ss])
            self.stt("dve", ot[:], xt[:], ss[:, 1:2], gam[:], ALU.mult, ALU.mult, r=[xt, ss, gam], w=[ot])
            k.dma("act", self.out[r0:r0 + 128, :], ot[:], r=[ot], w=self.out.rows(r0, r0 + 128))
        k.end()

    def ph_dump(self, src, r0, r1):
        k = self.k
        k.begin()
        k.dma("sp", self.out.ap(), src[r0:r1, :], r=[src], w=[self.out])
        k.end()

    def layer_mixer(self, i):
        need_ctx = i < DEPTH - 1
        j = i // 2
        if i % 2 == 0:
            self.ph_norm(i, "mix", True)
            self.ph_gla_proj(j)
            self.ph_gla_scan(j)
            self.ph_outproj(i, "gla_w_out", j, self.OG, "tok", need_ctx)
        else:
            self.ph_norm(i, "mix", need_ctx)
            streams = [(L, 0, 0, 0)] + ([(CL, L, 17 * 128, 128)] if need_ctx else [])
            for (Ls, tok0, frow0, rn0) in streams:
                self.ph_hy_filter(j, Ls, tok0, rn0)
                self.ph_dft(self.HS, tok0, Ls, "cf", self.KRE, frow0, True)
                self.ph_dft(self.HD, tok0, Ls, "sf", self.KIM, frow0, True)
            self.ph_hy_proj(j, need_ctx)
            for (Ls, tok0, frow0, rn0) in streams:
                self.ph_dft(self.UTOK, tok0, Ls, "cf", self.URE, frow0, False)
                self.ph_dft(self.UTOK, tok0, Ls, "sf", self.UIM, frow0, False)
                self.ph_hy_inv(j, Ls, tok0, frow0, rn0)
            self.ph_outproj(i, "hy_w_out", j, self.YHT, "feat", need_ctx, bias_row=self.hy_b_out[j:j + 1, :])

    def layer_ffn(self, i):
        need_ctx = i < DEPTH - 1
        self.ph_norm(i, "ffn", need_ctx)
        self.ph_experts(i, need_ctx)

    def build(self, stages=None):
        self.ph_init()
        if not self.direct:
            self.ph_gather()
        for i in range(DEPTH):
            if stages is not None and ("mix", i) not in stages and ("ffn", i) not in stages:
                continue
            self.ph_mods(i)
            if stages is None or ("mix", i) in stages:
                self.layer_mixer(i)
            if stages is None or ("ffn", i) in stages:
                self.layer_ffn(i)
        if self.debug_dump is None:
            self.ph_final()
        else:
            src = getattr(self, self.debug_dump[0])
            self.ph_dump(src, self.debug_dump[2], self.debug_dump[2] + self.debug_dump[1][0])
        self.k.finish()
        return self.nc


_CONST = None


def constants():
    global _CONST
    if _CONST is not None:
        return _CONST
    f32 = np.float32
    c = {}
    c["ident"] = np.eye(128, dtype=f32)
    s = np.arange(128)
    tri_f = (s[:, None] <= s[None, :]).astype(f32)
    tri_b = (s[:, None] >= s[None, :]).astype(f32)
    c["tri"] = np.stack([tri_f, tri_b]) * f32(-1.0 / 16.0)
    c["msk"] = np.stack([tri_f, tri_b])
    for Ls, nm in ((L, "l"), (CL, "c")):
        t = np.linspace(0.0, 1.0, Ls, dtype=f32)[:, None]
        w = (2 * np.pi * np.arange(Ls, dtype=f32)[:, None] / Ls).astype(f32)
        f = np.linspace(1e-4, 15, 16, dtype=f32)[None, :]
        fw = (f * w).astype(f32)
        z = np.concatenate([t, np.cos(fw), -np.sin(fw)], axis=-1).astype(f32)
        c["zT_" + nm] = np.ascontiguousarray(z.T)
        tt_ = np.linspace(0.0, 1.0, Ls, dtype=f32)
        c["negt_" + nm] = np.ascontiguousarray((-tt_).reshape(Ls // 128, 128).T)
        N = 2 * Ls
        TC = Ls // 128
        FCn = TC + 1
        tq = np.arange(Ls, dtype=np.float64)
        fq = np.arange(FCn * 128, dtype=np.float64)
        ang = 2 * np.pi * np.outer(tq, fq) / N
        valid = (fq <= Ls)[None, :]
        C = np.where(valid, np.cos(ang), 0.0)
        Sn = np.where(valid, -np.sin(ang), 0.0)
        def fwd(T):
            T4 = T.reshape(TC, 128, FCn, 128)
            return np.ascontiguousarray(T4.transpose(2, 1, 0, 3).reshape(FCn, 128, TC * 128)).astype(f32)
        c["cf_" + nm] = fwd(C)
        c["sf_" + nm] = fwd(Sn)
        TBW = min(512, Ls)
        NTB = Ls // TBW
        GS = min(4, TC)
        NG = TC // GS
        def inv(T):
            T5 = T[:, :TC * 128].reshape(NTB, TBW, NG, GS, 128)
            return np.ascontiguousarray(T5.transpose(0, 2, 4, 3, 1).reshape(NTB, NG, 128, GS * TBW)).astype(f32)
        c["ci_" + nm] = inv(C)
        c["nsi_" + nm] = inv(Sn)
        c["cin_" + nm] = np.cos(np.pi * tq)[None, :].astype(f32)
        wf = np.full(FCn * 128, 2.0 / N)
        wf[0] = 1.0 / N
        wf[Ls] = 1.0 / N
        wf[Ls + 1:] = 0.0
        c["wn_" + nm] = np.ascontiguousarray(wf.reshape(FCn, 128).T).astype(f32)
    max_decay = math.log(1e-2) / 0.3
    min_decay = math.log(1e-2) / 1.5
    c["padf"] = np.ascontiguousarray(np.broadcast_to((NT + np.arange(128)).astype(f32)[None, :], (16, 128)))
    c["delta"] = np.abs(np.linspace(min_decay, max_decay, D, dtype=f32))[None, :].astype(f32)
    _CONST = c
    return c


def host_inputs(inp, ncores=NCORES, units=None, direct=None):
    f32 = np.float32
    g = lambda n: np.ascontiguousarray(np.asarray(inp[n], dtype=f32))
    direct = (ncores == 1) if direct is None else direct
    units = units if units is not None else all_units()
    dcache = {}
    shared = {}
    for n in ("b_ada", "norm_mix", "norm_ffn", "hy_f_w1", "hy_f_w2", "hy_f_w3", "hy_f_w4", "hy_b_out", "moe_router"):
        shared[n] = g(n)
    shared["norm_final"] = g("norm_final").reshape(1, D)
    shared["gla_hn"] = g("gla_head_norm")
    a1 = g("gla_w_a1")
    shared["gla_wa1"] = np.ascontiguousarray(a1.transpose(0, 2, 1, 3).reshape(2, D, 32))
    a2 = g("gla_w_a2")
    ba = g("gla_b_a")
    wa2 = np.zeros((2, 33, 2048), f32)
    for z in range(2):
        wa2[:, z * 16:(z + 1) * 16, z * 1024:(z + 1) * 1024] = a2[:, z]
        wa2[:, 32, z * 1024:(z + 1) * 1024] = ba[:, z]
    shared["gla_wa2"] = wa2
    cw = g("hy_conv_w")
    cols = np.stack([g("hy_b_in"), cw[:, 0], cw[:, 1], cw[:, 2], g("hy_conv_b")], axis=-1)
    shared["hy_cols"] = np.ascontiguousarray(cols.reshape(2, 48, 128, 5).transpose(0, 2, 1, 3).reshape(2, 128, 240))
    shared["hy_fb"] = np.ascontiguousarray(np.stack([g("hy_f_b1"), g("hy_f_b2"), g("hy_f_b3")], axis=-1))
    shared["hy_skipc"] = np.ascontiguousarray(g("hy_skip").reshape(2, 16, 128).transpose(0, 2, 1))
    cst = constants()
    big = {}
    for n in SHARDED:
        C, rpu, nu, CH = SHARDED[n]
        src = cst[n] if n in cst else np.asarray(inp[n], dtype=f32)
        big[n] = src.reshape(nu * rpu, C)
    for n, v in cst.items():
        if n in SHARDED:
            continue
        if n in ("cf_c", "sf_c"):
            v = v.reshape(3 * 128, 2 * 128)
        if n in ("ci_c", "nsi_c"):
            v = v.reshape(128, 2 * 256)
        shared[n] = v
    x = g("x")
    ctx = g("ctx")
    c = g("c")
    cctx = g("c_ctx")
    maps = []
    for b in range(ncores):
        m = dict(shared)
        m["x"] = x[b]
        m["ctx"] = ctx[b]
        cc = np.stack([c[b], cctx], axis=-1)
        m["cc"] = np.ascontiguousarray(cc.reshape(16, 128, 2).transpose(1, 0, 2).reshape(128, 32))
        for n in SHARDED:
            C, rpu, nu, CH = SHARDED[n]
            if direct:
                if n not in dcache:
                    us = units[n] if len(units[n]) else [0]
                    dcache[n] = big[n] if len(us) == nu else np.ascontiguousarray(
                        big[n].reshape(nu, rpu, C)[us].reshape(-1, C))
                m[n] = dcache[n]
            else:
                R = nu * rpu
                m[n] = np.ascontiguousarray(big[n].reshape(R // CH, NCORES, CH // NCORES, C)[:, b].reshape(-1, C))
        maps.append(m)
    return maps


_PROG = {}


REPLICATE = True


def get_prog():
    if "p" not in _PROG:
        p = Prog(direct=REPLICATE)
        p.build()
        _PROG["p"] = p
    return _PROG["p"]


def kernel(**inputs):
    maps = host_inputs(inputs, direct=REPLICATE)
    prog = get_prog()
    res = run_bass_kernel_spmd(prog.nc, maps, core_ids=list(range(NCORES)))
    out = np.stack([np.asarray(r["out"]) for r in res.results], axis=0)
    return out.astype(np.float32)
```
